# Optimizing a Trainium2 kernel written in Bass

```python
import math
import jax, jax.numpy as jnp
from jax import lax
import numpy as np

D_MODEL = 2048
BATCH = 8
SEQ = 2048
DEPTH = 1

ROPE_THETA = 500000.0
Q_BLOCK = 128
NORM_EPS = 1e-6

DIFF_HEADS = 8
DIFF_HEAD_DIM = 64
DIFF_V_DIM = 2 * DIFF_HEAD_DIM
DIFF_ROT = DIFF_HEAD_DIM // 4
DIFF_SUBLN_EPS = 1e-5

MLA_HEADS = 8
MLA_Q_RANK = 512
MLA_KV_RANK = 256
MLA_NOPE_DIM = 128
MLA_ROPE_DIM = 64
MLA_V_DIM = 128

IN_SPLITS = (DIFF_HEADS * 2 * DIFF_HEAD_DIM,
             DIFF_HEADS * 2 * DIFF_HEAD_DIM,
             DIFF_HEADS * DIFF_V_DIM,
             MLA_Q_RANK,
             MLA_KV_RANK,
             MLA_ROPE_DIM,
             D_MODEL,
             D_MODEL)
D_IN = sum(IN_SPLITS)

MEM_LEN = 256
CROSS_HEADS = 4
CROSS_HEAD_DIM = 128

N_GROUPS = 4
EXPERTS_PER_GROUP = 8
N_EXPERTS = N_GROUPS * EXPERTS_PER_GROUP
TOP_K_IN_GROUP = 2
D_EXPERT = 512
MOE_BLOCK = 128

kernel_name = 'hybrid_diffattn_mla_hiermoe_encoder'


def rmsnorm(x, g, eps=NORM_EPS):
    xf = x.astype(jnp.float32)
    y = xf * lax.rsqrt(jnp.mean(xf * xf, axis=-1, keepdims=True) + eps)
    return (y * g.astype(jnp.float32)).astype(x.dtype)


def rope(x, positions, rot_dim):
    half = rot_dim // 2
    inv_freq = jnp.float32(ROPE_THETA) ** (-jnp.arange(half, dtype=jnp.float32) * 2.0 / rot_dim)
    ang = positions.astype(jnp.float32)[:, :, None] * inv_freq
    cos = jnp.cos(ang)[:, :, None, :]
    sin = jnp.sin(ang)[:, :, None, :]
    xr = x[..., :rot_dim].astype(jnp.float32)
    x1, x2 = xr[..., :half], xr[..., half:]
    rot = jnp.concatenate([x1 * cos - x2 * sin, x2 * cos + x1 * sin], axis=-1)
    return jnp.concatenate([rot.astype(x.dtype), x[..., rot_dim:]], axis=-1)


def sweep_query_blocks(fn, *q_arrays):
    b, s = q_arrays[0].shape[:2]
    nb = s // Q_BLOCK
    blocks = tuple(jnp.moveaxis(q.reshape(b, nb, Q_BLOCK, *q.shape[2:]), 1, 0) for q in q_arrays)
    out = lax.map(lambda blk: fn(*blk), blocks)
    out = jnp.moveaxis(out, 0, 1)
    return out.reshape(b, s, *out.shape[3:])


def differential_attention(q, k, v, lam, subln_g, lam_init):
    scale = DIFF_HEAD_DIM ** -0.5
    k1, k2 = k[..., 0, :], k[..., 1, :]

    def block(qb):
        s1 = jnp.einsum('bqhd,bkhd->bhqk', qb[..., 0, :], k1).astype(jnp.float32) * scale
        s2 = jnp.einsum('bqhd,bkhd->bhqk', qb[..., 1, :], k2).astype(jnp.float32) * scale
        a = jax.nn.softmax(s1, axis=-1) - lam * jax.nn.softmax(s2, axis=-1)
        return jnp.einsum('bhqk,bkhe->bqhe', a.astype(v.dtype), v)

    o = sweep_query_blocks(block, q)
    return rmsnorm(o, subln_g, DIFF_SUBLN_EPS) * (1.0 - lam_init)


def latent_attention(c_q, c_kv, k_pe_raw, positions, q_norm_g, w_uq, kv_norm_g, w_ukv):
    b, s = c_q.shape[:2]
    q = (rmsnorm(c_q, q_norm_g) @ w_uq).reshape(b, s, MLA_HEADS, MLA_NOPE_DIM + MLA_ROPE_DIM)
    q_nope = q[..., :MLA_NOPE_DIM]
    q_pe = rope(q[..., MLA_NOPE_DIM:], positions, MLA_ROPE_DIM)
    kv = (rmsnorm(c_kv, kv_norm_g) @ w_ukv).reshape(b, s, MLA_HEADS, MLA_NOPE_DIM + MLA_V_DIM)
    k_nope, v = kv[..., :MLA_NOPE_DIM], kv[..., MLA_NOPE_DIM:]
    k_pe = rope(k_pe_raw[:, :, None, :], positions, MLA_ROPE_DIM)[:, :, 0, :]
    scale = (MLA_NOPE_DIM + MLA_ROPE_DIM) ** -0.5

    def block(qn, qp):
        sc = (jnp.einsum('bqhd,bkhd->bhqk', qn, k_nope)
              + jnp.einsum('bqhr,bkr->bhqk', qp, k_pe)).astype(jnp.float32) * scale
        p = jax.nn.softmax(sc, axis=-1)
        return jnp.einsum('bhqk,bkhe->bqhe', p.astype(v.dtype), v)

    return sweep_query_blocks(block, q_nope, q_pe)


def memory_cross_attention(hn, mem, mem_norm_g, w_cq, w_ckv, w_co):
    b, s, _ = hn.shape
    m = mem.shape[1]
    q = (hn @ w_cq).reshape(b, s, CROSS_HEADS, CROSS_HEAD_DIM)
    kv = (rmsnorm(mem, mem_norm_g) @ w_ckv).reshape(b, m, 2, CROSS_HEADS, CROSS_HEAD_DIM)
    k, v = kv[:, :, 0], kv[:, :, 1]
    sc = jnp.einsum('bqhd,bmhd->bhqm', q, k).astype(jnp.float32) * CROSS_HEAD_DIM ** -0.5
    p = jax.nn.softmax(sc, axis=-1)
    o = jnp.einsum('bhqm,bmhd->bqhd', p.astype(v.dtype), v).reshape(b, s, CROSS_HEADS * CROSS_HEAD_DIM)
    return o @ w_co


def hierarchical_moe(t, w_rg, b_rg, w_re, b_re, w_gate, w_up, w_down):
    n, d = t.shape
    tf = t.astype(jnp.float32)
    g_prob = jax.nn.softmax(tf @ w_rg.astype(jnp.float32) + b_rg.astype(jnp.float32), axis=-1)
    g_p, g_idx = lax.top_k(g_prob, 1)
    e_logits = (tf @ w_re.astype(jnp.float32) + b_re.astype(jnp.float32)).reshape(n, N_GROUPS, EXPERTS_PER_GROUP)
    e_logits = jnp.take_along_axis(e_logits, g_idx[:, :, None], axis=1)[:, 0]
    e_p, e_local = lax.top_k(jax.nn.softmax(e_logits, axis=-1), TOP_K_IN_GROUP)
    e_p = e_p / jnp.sum(e_p, axis=-1, keepdims=True)
    weights = (g_p * e_p).reshape(-1)
    expert_ids = (g_idx * EXPERTS_PER_GROUP + e_local).reshape(-1).astype(jnp.int32)
    tok_ids = jnp.repeat(jnp.arange(n, dtype=jnp.int32), TOP_K_IN_GROUP)
    a = n * TOP_K_IN_GROUP

    order = jnp.argsort(expert_ids)
    e_sorted, tok_sorted, w_sorted = expert_ids[order], tok_ids[order], weights[order]
    counts = jnp.zeros((N_EXPERTS,), jnp.int32).at[expert_ids].add(1)
    offsets = jnp.cumsum(counts) - counts
    padded = ((counts + MOE_BLOCK - 1) // MOE_BLOCK) * MOE_BLOCK
    padded_end = jnp.cumsum(padded)
    padded_off = padded_end - padded
    dest = padded_off[e_sorted] + (jnp.arange(a, dtype=jnp.int32) - offsets[e_sorted])
    p_rows = ((a + MOE_BLOCK - 1) // MOE_BLOCK) * MOE_BLOCK + N_EXPERTS * MOE_BLOCK
    n_blocks = p_rows // MOE_BLOCK
    row_tok = jnp.full((p_rows,), n, jnp.int32).at[dest].set(tok_sorted)
    row_w = jnp.zeros((p_rows,), jnp.float32).at[dest].set(w_sorted)
    block_start = jnp.arange(n_blocks, dtype=jnp.int32) * MOE_BLOCK
    block_expert = jnp.clip(jnp.searchsorted(padded_end, block_start, side='right'), 0, N_EXPERTS - 1)

    xs = jnp.concatenate([t, jnp.zeros((1, d), t.dtype)], axis=0)
    xb = xs[row_tok].reshape(n_blocks, MOE_BLOCK, d)

    def expert_block(args):
        xblk, e = args
        hid = jax.nn.silu(xblk @ w_gate[e]) * (xblk @ w_up[e])
        return hid @ w_down[e]

    y = lax.map(expert_block, (xb, block_expert)).reshape(p_rows, d)
    out = jax.ops.segment_sum(y.astype(jnp.float32) * row_w[:, None], row_tok, num_segments=n + 1)[:n]
    return out.astype(t.dtype)


def setup_inputs(seed: int = 0) -> dict:
    key = jax.random.key(seed)
    ks = iter(jax.random.split(key, 40))
    L = DEPTH

    def w(shape, fan_in):
        return jax.random.normal(next(ks), shape, jnp.float32) * fan_in ** -0.5

    def gain(shape):
        return 1.0 + 0.01 * jax.random.normal(next(ks), shape, jnp.float32)

    def small(shape, s):
        return s * jax.random.normal(next(ks), shape, jnp.float32)

    return {
        'x': jax.random.normal(next(ks), (BATCH, SEQ, D_MODEL), jnp.float32),
        'mem': jax.random.normal(next(ks), (BATCH, MEM_LEN, D_MODEL), jnp.float32),
        'positions': jnp.broadcast_to(jnp.arange(SEQ, dtype=jnp.int32)[None, :], (BATCH, SEQ)),
        'attn_norm_g': gain((L, D_MODEL)),
        'w_in': w((L, D_MODEL, D_IN), D_MODEL),
        'diff_lambda_q1': small((L, DIFF_HEAD_DIM), 0.1),
        'diff_lambda_k1': small((L, DIFF_HEAD_DIM), 0.1),
        'diff_lambda_q2': small((L, DIFF_HEAD_DIM), 0.1),
        'diff_lambda_k2': small((L, DIFF_HEAD_DIM), 0.1),
        'diff_subln_g': gain((L, DIFF_V_DIM)),
        'w_o_diff': w((L, DIFF_HEADS * DIFF_V_DIM, D_MODEL), DIFF_HEADS * DIFF_V_DIM),
        'mla_q_norm_g': gain((L, MLA_Q_RANK)),
        'w_uq': w((L, MLA_Q_RANK, MLA_HEADS * (MLA_NOPE_DIM + MLA_ROPE_DIM)), MLA_Q_RANK),
        'mla_kv_norm_g': gain((L, MLA_KV_RANK)),
        'w_ukv': w((L, MLA_KV_RANK, MLA_HEADS * (MLA_NOPE_DIM + MLA_V_DIM)), MLA_KV_RANK),
        'w_o_mla': w((L, MLA_HEADS * MLA_V_DIM, D_MODEL), MLA_HEADS * MLA_V_DIM),
        'w_out': w((L, D_MODEL, D_MODEL), D_MODEL),
        'cross_norm_g': gain((L, D_MODEL)),
        'mem_norm_g': gain((L, D_MODEL)),
        'w_cq': w((L, D_MODEL, CROSS_HEADS * CROSS_HEAD_DIM), D_MODEL),
        'w_ckv': w((L, D_MODEL, 2 * CROSS_HEADS * CROSS_HEAD_DIM), D_MODEL),
        'w_co': w((L, CROSS_HEADS * CROSS_HEAD_DIM, D_MODEL), CROSS_HEADS * CROSS_HEAD_DIM),
        'ffn_norm_g': gain((L, D_MODEL)),
        'w_router_group': w((L, D_MODEL, N_GROUPS), D_MODEL),
        'b_router_group': small((L, N_GROUPS), 0.01),
        'w_router_expert': w((L, D_MODEL, N_EXPERTS), D_MODEL),
        'b_router_expert': small((L, N_EXPERTS), 0.01),
        'w_expert_gate': w((L, N_EXPERTS, D_MODEL, D_EXPERT), D_MODEL),
        'w_expert_up': w((L, N_EXPERTS, D_MODEL, D_EXPERT), D_MODEL),
        'w_expert_down': w((L, N_EXPERTS, D_EXPERT, D_MODEL), D_EXPERT),
        'final_norm_g': gain((D_MODEL,)),
    }


def reference(x, mem, positions, attn_norm_g, w_in,
              diff_lambda_q1, diff_lambda_k1, diff_lambda_q2, diff_lambda_k2, diff_subln_g, w_o_diff,
              mla_q_norm_g, w_uq, mla_kv_norm_g, w_ukv, w_o_mla, w_out,
              cross_norm_g, mem_norm_g, w_cq, w_ckv, w_co,
              ffn_norm_g, w_router_group, b_router_group, w_router_expert, b_router_expert,
              w_expert_gate, w_expert_up, w_expert_down, final_norm_g):
    b, s, d = x.shape
    split_points = [int(i) for i in np.cumsum(IN_SPLITS)[:-1]]
    h = x
    for l in range(DEPTH):
        lam_init = 0.8 - 0.6 * math.exp(-0.3 * l)

        xn = rmsnorm(h, attn_norm_g[l])
        proj = xn @ w_in[l]
        dq, dk, dv, c_q, c_kv, k_pe, g_a, g_b = jnp.split(proj, split_points, axis=-1)

        dq = rope(dq.reshape(b, s, 2 * DIFF_HEADS, DIFF_HEAD_DIM), positions, DIFF_ROT)
        dk = rope(dk.reshape(b, s, 2 * DIFF_HEADS, DIFF_HEAD_DIM), positions, DIFF_ROT)
        dq = dq.reshape(b, s, DIFF_HEADS, 2, DIFF_HEAD_DIM)
        dk = dk.reshape(b, s, DIFF_HEADS, 2, DIFF_HEAD_DIM)
        dv = dv.reshape(b, s, DIFF_HEADS, DIFF_V_DIM)
        lam = (jnp.exp(jnp.sum(diff_lambda_q1[l].astype(jnp.float32) * diff_lambda_k1[l].astype(jnp.float32)))
               - jnp.exp(jnp.sum(diff_lambda_q2[l].astype(jnp.float32) * diff_lambda_k2[l].astype(jnp.float32)))
               + lam_init)
        o_a = differential_attention(dq, dk, dv, lam, diff_subln_g[l], lam_init)
        y_a = o_a.reshape(b, s, DIFF_HEADS * DIFF_V_DIM) @ w_o_diff[l]

        o_b = latent_attention(c_q, c_kv, k_pe, positions, mla_q_norm_g[l], w_uq[l], mla_kv_norm_g[l], w_ukv[l])
        y_b = o_b.reshape(b, s, MLA_HEADS * MLA_V_DIM) @ w_o_mla[l]

        merged = jax.nn.sigmoid(g_a) * y_a + jax.nn.sigmoid(g_b) * y_b
        h = h + merged @ w_out[l]

        h = h + memory_cross_attention(rmsnorm(h, cross_norm_g[l]), mem, mem_norm_g[l], w_cq[l], w_ckv[l], w_co[l])

        t = rmsnorm(h, ffn_norm_g[l]).reshape(b * s, d)
        h = h + hierarchical_moe(t, w_router_group[l], b_router_group[l], w_router_expert[l], b_router_expert[l],
                                 w_expert_gate[l], w_expert_up[l], w_expert_down[l]).reshape(b, s, d)
    return rmsnorm(h, final_norm_g)
```

```python
import math
from contextlib import ExitStack

import numpy as np
import ml_dtypes

import concourse.bass as bass
import concourse.mybir as mybir
from concourse.bass_utils import run_bass_kernel_spmd

F32 = mybir.dt.float32
BF16 = mybir.dt.bfloat16
I32 = mybir.dt.int32
ALU = mybir.AluOpType
AF = mybir.ActivationFunctionType
AX = mybir.AxisListType

NCORES = 8
S = 2048
D = 2048
NT = S // 128
DC = D // 128
D_IN = 8000
EPS = 1e-6


class Buf:
    __slots__ = ("name", "last_w", "readers")

    def __init__(self, name):
        self.name = name
        self.last_w = None
        self.readers = {}


class Prog:
    ENGINES = ("pe", "act", "dve", "pool", "sp")

    def __init__(self, nc, stack, n_lanes=12):
        self.nc = nc
        self.handles = {"pe": nc.tensor, "act": nc.scalar, "dve": nc.vector,
                        "pool": nc.gpsimd, "sp": nc.sync}
        self.thunks = {e: [] for e in self.ENGINES}
        self.sems = {}
        self.count = {}
        for e in self.ENGINES:
            self.sems[e] = stack.enter_context(nc.semaphore("c_" + e))
            self.count[e] = 0
        self.lanes = {}
        for q in ("sp", "pool"):
            ls = []
            for i in range(n_lanes):
                key = "l_%s%d" % (q, i)
                self.sems[key] = stack.enter_context(nc.semaphore(key))
                self.count[key] = 0
                ls.append(key)
            self.lanes[q] = ls
        self.lane_rr = {"sp": 0, "pool": 0}
        self.known = {e: {} for e in self.ENGINES}
        self.n_waits = 0
        self.events = {e: [] for e in self.ENGINES}

    def _collect(self, eng, reads, writes):
        need = {}

        def add(tok, same_ok):
            if tok is None:
                return
            k, v = tok
            if k == eng and same_ok:
                return
            if need.get(k, 0) < v:
                need[k] = v

        for b in reads:
            add(b.last_w, same_ok=(eng == "pe"))
        for b in writes:
            add(b.last_w, same_ok=True)
            for k, v in b.readers.items():
                add((k, v), same_ok=True)
        out = []
        kn = self.known[eng]
        for k, v in need.items():
            if kn.get(k, 0) >= v:
                continue
            kn[k] = v
            out.append((k, v))
        return out

    def _emit_waits(self, eng, waits):
        h = self.handles[eng]
        sems = self.sems
        for k, v in waits:
            self.n_waits += 1
            self.events[eng].append(("w", k, v))
            self.thunks[eng].append(lambda h=h, s=sems[k], v=v: h.wait_ge(s, v))

    def _update(self, tok, reads, writes):
        k, v = tok
        for b in reads:
            if b.readers.get(k, 0) < v:
                b.readers[k] = v
        for b in writes:
            b.last_w = tok
            b.readers = {}

    def op(self, eng, fn, reads=(), writes=(), sig=True):
        waits = self._collect(eng, reads, writes)
        self._emit_waits(eng, waits)
        h = self.handles[eng]
        sem = self.sems[eng]
        if sig:
            self.count[eng] += 1
            tok = (eng, self.count[eng])
            self.events[eng].append(("s", eng, 1))
            self.thunks[eng].append(lambda: fn(h).then_inc(sem, 1))
        else:
            tok = (eng, self.count[eng] + 1)
            self.thunks[eng].append(lambda: fn(h))
        self._update(tok, reads, writes)
        return tok

    def dma(self, q, fn, reads=(), writes=()):
        lanes = self.lanes[q]
        lane = lanes[self.lane_rr[q] % len(lanes)]
        self.lane_rr[q] += 1
        waits = self._collect(q, reads, writes)
        prev = self.count[lane]
        if prev > 0 and self.known[q].get(lane, 0) < prev:
            self.known[q][lane] = prev
            waits.append((lane, prev))
        self._emit_waits(q, waits)
        h = self.handles[q]
        sem = self.sems[lane]
        self.count[lane] += 16
        tok = (lane, self.count[lane])
        self.events[q].append(("s", lane, 16))
        self.thunks[q].append(lambda: fn(h).then_inc(sem, 16))
        self._update(tok, reads, writes)
        return tok

    def wait_all(self, eng, bufs):
        waits = self._collect(eng, bufs, ())
        self._emit_waits(eng, waits)

    def check_deadlock(self):
        val = {}
        pos = {e: 0 for e in self.ENGINES}
        progress = True
        while progress:
            progress = False
            for e in self.ENGINES:
                ev = self.events[e]
                i = pos[e]
                while i < len(ev):
                    kind, k, v = ev[i]
                    if kind == "w":
                        if val.get(k, 0) < v:
                            break
                    else:
                        val[k] = val.get(k, 0) + v
                    i += 1
                if i != pos[e]:
                    progress = True
                    pos[e] = i
        stuck = {e: (pos[e], len(self.events[e]), self.events[e][pos[e]]) for e in self.ENGINES
                 if pos[e] < len(self.events[e])}
        if stuck:
            raise RuntimeError("build-time deadlock check failed: %r" % (stuck,))

    def emit(self):
        self.check_deadlock()
        nc = self.nc
        with nc.Block() as block:
            @block.tensor
            def _(e):
                for t in self.thunks["pe"]:
                    t()

            @block.scalar
            def _(e):
                for t in self.thunks["act"]:
                    t()

            @block.vector
            def _(e):
                for t in self.thunks["dve"]:
                    t()

            @block.gpsimd
            def _(e):
                for t in self.thunks["pool"]:
                    t()

            @block.sync
            def _(e):
                for t in self.thunks["sp"]:
                    t()


class Ring:
    def __init__(self, tiles):
        self.tiles = tiles
        self.i = 0

    def next(self):
        t = self.tiles[self.i % len(self.tiles)]
        self.i += 1
        return t


class Tile:
    __slots__ = ("t", "b")

    def __init__(self, t, name):
        self.t = t
        self.b = Buf(name)

    def __getitem__(self, k):
        return self.t[k]


class Ctx:
    def __init__(self, nc, stack):
        self.nc = nc
        self.stack = stack
        self.p = Prog(nc, stack)
        self._n = 0

    def sb(self, name, shape, dt):
        self._n += 1
        return Tile(self.stack.enter_context(self.nc.sbuf_tensor("s%d_%s" % (self._n, name), list(shape), dt)), name)

    def ps(self, name, shape, dt=F32):
        self._n += 1
        return Tile(self.stack.enter_context(self.nc.psum_tensor("p%d_%s" % (self._n, name), list(shape), dt)), name)

    def dram(self, name, shape, dt, kind="Internal"):
        return Tile(self.nc.dram_tensor(name, list(shape), dt, kind=kind), name)

    def sb_ring(self, name, n, shape, dt):
        return Ring([self.sb("%s%d" % (name, i), shape, dt) for i in range(n)])

    def ps_ring(self, name, n, shape, dt=F32):
        return Ring([self.ps("%s%d" % (name, i), shape, dt) for i in range(n)])


H = 8
ROPE_THETA = 500000.0
CAP = 384
NE = 32
NSLOT = NE * CAP
DFF = 512
LAM_INIT = 0.8 - 0.6 * math.exp(-0.3 * 0)
PI = math.pi
NO_SCATTER = False


def bs(tiles):
    return [t.b for t in tiles]


class K(Ctx):
    def mm(self, out, lhsT, rhs, start, stop, R, W, sig=None):
        if sig is None:
            sig = stop
        self.p.op("pe", lambda h: h.matmul(out, lhsT, rhs, start=start, stop=stop), bs(R), bs(W), sig=sig)

    def tr(self, out, in_, ident, R, W, sig=True):
        self.p.op("pe", lambda h: h.transpose(out, in_, ident), bs(R), bs(W), sig=sig)

    def act(self, out, in_, func, R, W, bias=None, scale=None, accum=None):
        kw = {}
        if bias is not None:
            kw["bias"] = bias
        if scale is not None:
            kw["scale"] = scale
        if accum is not None:
            kw["accum_out"] = accum
        self.p.op("act", lambda h: h.activation(out, in_, func, **kw), bs(R), bs(W))

    def ts(self, eng, out, in0, s1, s2, op0, op1, R, W):
        if op1 is None:
            self.p.op(eng, lambda h: h.tensor_scalar(out, in0, s1, None, op0), bs(R), bs(W))
        else:
            self.p.op(eng, lambda h: h.tensor_scalar(out, in0, s1, s2, op0, op1), bs(R), bs(W))

    def tt(self, eng, out, in0, in1, op, R, W):
        self.p.op(eng, lambda h: h.tensor_tensor(out, in0, in1, op), bs(R), bs(W))

    def stt(self, out, in0, scalar, in1, op0, op1, R, W, accum=None):
        if accum is None:
            self.p.op("dve", lambda h: h.scalar_tensor_tensor(out, in0, scalar, in1, op0, op1), bs(R), bs(W))
        else:
            self.p.op("dve", lambda h: h.scalar_tensor_tensor(out, in0, scalar, in1, op0, op1, accum_out=accum),
                      bs(R), bs(W))

    def copy(self, eng, out, in_, R, W):
        if eng == "act":
            self.p.op("act", lambda h: h.copy(out, in_), bs(R), bs(W))
        else:
            self.p.op(eng, lambda h: h.tensor_copy(out, in_), bs(R), bs(W))

    def recip(self, out, in_, R, W):
        self.p.op("dve", lambda h: h.reciprocal(out, in_), bs(R), bs(W))

    def memset(self, eng, out, val, W):
        self.p.op(eng, lambda h: h.memset(out, val), [], bs(W))

    def dma(self, q, out, in_, R, W):
        self.p.dma(q, lambda h: h.dma_start(out=out, in_=in_), bs(R), bs(W))

    def barrier(self):
        p = self.p
        toks = [(e, p.count[e]) for e in p.ENGINES if p.count[e] > 0]
        for q in ("sp", "pool"):
            for lane in p.lanes[q]:
                if p.count[lane] > 0:
                    toks.append((lane, p.count[lane]))
        for e in p.ENGINES:
            w = []
            for k, v in toks:
                if k == e:
                    continue
                if p.known[e].get(k, 0) < v:
                    p.known[e][k] = v
                    w.append((k, v))
            p._emit_waits(e, w)


def rmsnorm_rstd(c, x_t, d, ss, junk, eps):
    c.act(junk[:, :d], x_t[:, :d], AF.Square, [x_t], [junk, ss], accum=ss[:, 0:1])
    c.ts("dve", ss[:, 1:2], ss[:, 0:1], 1.0 / d, eps, ALU.mult, ALU.add, [ss], [ss])
    c.p.op("act", lambda h: h.sqrt(ss[:, 3:4], ss[:, 1:2]), bs([ss]), bs([ss]))
    c.recip(ss[:, 2:3], ss[:, 3:4], [ss], [ss])


def norm_phase1(c, x_t, rg):
    junk = rg["junk"].next()
    ss = rg["ss"].next()
    c.act(junk[:, :D], x_t[:, :D], AF.Square, [x_t], [junk, ss], accum=ss[:, 0:1])
    return ss


def norm_phase2(c, x_t, ss, gT, goff, dstT, tcol, rg, ident):
    xs = rg["xs"].next()
    c.ts("dve", ss[:, 1:2], ss[:, 0:1], 1.0 / D, EPS, ALU.mult, ALU.add, [ss], [ss])
    c.p.op("act", lambda h: h.sqrt(ss[:, 3:4], ss[:, 1:2]), bs([ss]), bs([ss]))
    c.recip(ss[:, 2:3], ss[:, 3:4], [ss], [ss])
    half = D // 2
    c.ts("dve", xs[:, :half], x_t[:, :half], ss[:, 2:3], None, ALU.mult, None, [x_t, ss], [xs])
    c.act(xs[:, half:], x_t[:, half:], AF.Copy, [x_t, ss], [xs], scale=ss[:, 2:3])
    for g4 in range(DC // 4):
        pt = rg["pst"].next()
        for j in range(4):
            ch = g4 * 4 + j
            c.tr(pt[:, j * 128:(j + 1) * 128], xs[:, ch * 128:(ch + 1) * 128], ident[:, :], [xs, ident], [pt],
                 sig=(j == 3))
        for j in range(4):
            ch = g4 * 4 + j
            if j % 2 == 0:
                c.ts("dve", dstT[:, ch, tcol:tcol + 128], pt[:, j * 128:(j + 1) * 128],
                     gT[:, goff + ch:goff + ch + 1], None, ALU.mult, None, [pt, gT], [dstT])
            else:
                c.act(dstT[:, ch, tcol:tcol + 128], pt[:, j * 128:(j + 1) * 128], AF.Copy, [pt, gT], [dstT],
                      scale=gT[:, goff + ch:goff + ch + 1])


def norm_transpose_stream(c, n_tiles, load_fn, gT, goff, dstT, rg, ident, dst_fn=None, after_fn=None):
    cur = load_fn(0)
    ss = norm_phase1(c, cur, rg)
    for t in range(n_tiles):
        nxt = nss = None
        if t + 1 < n_tiles:
            nxt = load_fn(t + 1)
            nss = norm_phase1(c, nxt, rg)
        if dst_fn is None:
            norm_phase2(c, cur, ss, gT, goff, dstT, t * 128, rg, ident)
        else:
            dt_, tcol = dst_fn(t)
            norm_phase2(c, cur, ss, gT, goff, dt_, tcol, rg, ident)
        if after_fn is not None:
            after_fn(t)
        cur, ss = nxt, nss


def norm_transpose_tile(c, x_t, gT, goff, dstT, tcol, rg, ident):
    ss = norm_phase1(c, x_t, rg)
    norm_phase2(c, x_t, ss, gT, goff, dstT, tcol, rg, ident)


def rope_tables(c, pos_d, invf, col, nparts, Ct, St, tmp_i, tmp_f):
    n = nparts
    c.dma("sp", tmp_i[:n, :], pos_d[0:1, :].partition_broadcast(n), [pos_d], [tmp_i])
    c.copy("dve", tmp_f[:n, :], tmp_i[:n, :], [tmp_i], [tmp_f])
    for dst, phase in ((St, 0.0), (Ct, 0.25)):
        c.ts("dve", dst[:n, :], tmp_f[:n, :], invf[:n, col:col + 1], phase, ALU.mult, ALU.add, [tmp_f, invf], [dst])
        c.copy("dve", tmp_i[:n, :], dst[:n, :], [dst], [tmp_i])
        c.copy("dve", Ct[:n, :] if dst is St else tmp_f[:n, :], tmp_i[:n, :], [tmp_i], [Ct if dst is St else tmp_f])
        kf = Ct if dst is St else tmp_f
        c.tt("dve", dst[:n, :], dst[:n, :], kf[:n, :], ALU.subtract, [dst, kf], [dst])
        for _ in range(2):
            c.stt(dst[:n, :], dst[:n, :], 0.5, dst[:n, :], ALU.is_gt, ALU.subtract, [dst], [dst])
        c.act(dst[:n, :], dst[:n, :], AF.Sin, [dst], [dst], scale=2 * PI)


def rope_evac(c, ps, npart, blk, Rm, Ct, St, rg, dst_ap, dst_tile, defer=None):
    qs = rg["qs"].next()
    c.copy("act", qs[:npart, :], ps[:npart, :], [ps], [qs])
    if defer is not None:
        defer.append(lambda: _rope_rest(c, qs, npart, blk, Rm, Ct, St, rg, dst_ap, dst_tile))
        return
    _rope_rest(c, qs, npart, blk, Rm, Ct, St, rg, dst_ap, dst_tile)


def _rope_rest(c, qs, npart, blk, Rm, Ct, St, rg, dst_ap, dst_tile):
    pr = rg["psr"].next()
    c.mm(pr[:npart, :], Rm[:npart, :npart], qs[:npart, :], True, True, [Rm, qs], [pr])
    t1 = rg["t1"].next()
    t2 = rg["t2"].next()
    cs = slice(blk * 512, (blk + 1) * 512)
    c.tt("dve", t1[:npart, :], qs[:npart, :], Ct[:npart, cs], ALU.mult, [qs, Ct], [t1])
    c.tt("dve", t2[:npart, :], pr[:npart, :], St[:npart, cs], ALU.mult, [pr, St], [t2])
    c.tt("pool", dst_ap, t1[:npart, :], t2[:npart, :], ALU.add, [t1, t2], [dst_tile])


def build_program(stop_after=None, dump=()):
    nc = bass.Bass("TRN2", target_bir_lowering=False)
    outer = ExitStack()
    with outer:
        c = K(nc, outer)
        p = c.p

        def dram(name, shape, dt, kind=None):
            if kind is None:
                kind = "ExternalOutput" if name in dump else "Internal"
            return c.dram(name, shape, dt, kind=kind)

        x_d = dram("x", [S, D], F32, "ExternalInput")
        mem_d = dram("mem", [256, D], F32, "ExternalInput")
        pos_d = dram("pos", [1, S], I32, "ExternalInput")
        w_in_d = dram("w_in", [D, D_IN], F32, "ExternalInput")
        w_od_d = dram("w_o_diff", [1024, D], F32, "ExternalInput")
        w_uq_d = dram("w_uq", [512, 1536], F32, "ExternalInput")
        w_ukv_d = dram("w_ukv", [256, 2048], F32, "ExternalInput")
        w_om_d = dram("w_o_mla", [1024, D], F32, "ExternalInput")
        w_out_d = dram("w_out", [D, D], F32, "ExternalInput")
        w_cq_d = dram("w_cq", [D, 512], F32, "ExternalInput")
        w_ckv_d = dram("w_ckv", [D, 1024], F32, "ExternalInput")
        w_co_d = dram("w_co", [512, D], F32, "ExternalInput")
        w_r_d = dram("w_router", [D, 36], F32, "ExternalInput")
        b_r_d = dram("b_router", [1, 36], F32, "ExternalInput")
        w_eg_d = dram("w_expert_gate", [NE, D, DFF], F32, "ExternalInput")
        w_eu_d = dram("w_expert_up", [NE, D, DFF], F32, "ExternalInput")
        w_ed_d = dram("w_expert_down", [NE, DFF, D], F32, "ExternalInput")
        gT_d = dram("gT", [128, 4 * DC + 8], F32, "ExternalInput")
        grow_d = dram("grow", [2, D], F32, "ExternalInput")
        lam_d = dram("lam", [1, 256], F32, "ExternalInput")
        cb_d = dram("cb", [128, 4 * 128], BF16, "ExternalInput")
        rm_d = dram("rm", [64, 64], BF16, "ExternalInput")
        cf_d = dram("cf", [128, 2 + NE], F32, "ExternalInput")
        out_d = dram("out", [S, D], F32, "ExternalOutput")

        qT_d = dram("qT_s", [H, 128, S], BF16)
        kT_d = dram("kT_s", [H, 128, S], BF16)
        v_d = dram("v_s", [S, 1024], BF16)
        cq_d = dram("cq_s", [4, 128, S], F32)
        ckv_d = dram("ckv_s", [2, 128, S], F32)
        kpe_d = dram("kpe_s", [64, S], BF16)
        ga_d = dram("ga_s", [DC, 128, S], BF16)
        gb_d = dram("gb_s", [DC, 128, S], BF16)
        oa_d = dram("oa_s", [H, 128, S], BF16)
        ob_d = dram("ob_s", [H, 128, S], BF16)
        h1_d = dram("h1_s", [S, D], F32)
        h2_d = dram("h2_s", [S, D], F32)
        xg_d = dram("xg_s", [NSLOT + 128, D], BF16)
        y_d = dram("y_s", [NSLOT + 128, D], BF16)

        cb = c.sb("cb", [128, 4 * 128], BF16)
        rm = c.sb("rm", [64, 64], BF16)
        cf = c.sb("cf", [128, 2 + NE], F32)
        gT = c.sb("gTs", [128, 4 * DC + 8], F32)
        c.dma("sp", cb[:, :], cb_d[:, :], [cb_d], [cb])
        c.dma("sp", rm[:, :], rm_d[:, :], [rm_d], [rm])
        c.dma("sp", cf[:, :], cf_d[:, :], [cf_d], [cf])
        c.dma("sp", gT[:, :], gT_d[:, :], [gT_d], [gT])
        ident = Tile(cb.t[:, 0:128], "ident"); ident.b = cb.b
        ones = Tile(cb.t[:, 128:256], "ones"); ones.b = cb.b
        Rd = Tile(cb.t[:, 256:384], "Rd"); Rd.b = cb.b
        Utri = Tile(cb.t[:, 384:512], "Utri"); Utri.b = cb.b
        slot_i = c.sb("slot_i", [128, NT, 2], I32)
        wts = c.sb("wts", [128, NT, 2], F32)
        zrow = c.sb("zrow", [128, D], BF16)
        c.memset("pool", zrow[:, :], 0.0, [zrow])

        final_outs = []
        bc_reg = nc.gpsimd.alloc_register("bc_reg")
        p.thunks["pool"].insert(0, lambda: nc.gpsimd.reg_mov(bc_reg, NSLOT + 127))

        def finish():
            p.wait_all("sp", [b for b in final_outs])
            p.emit()

        with ExitStack() as sa:
            c.stack = sa
            xnT = c.sb("xnT", [128, DC, S], BF16)
            with ExitStack() as sa1:
                c.stack = sa1
                rg = {"junk": c.sb_ring("junk", 1, [128, D], BF16), "ss": c.sb_ring("ss", 4, [128, 4], F32),
                      "xs": c.sb_ring("xs", 2, [128, D], BF16), "pst": c.ps_ring("pst", 2, [128, 512], BF16)}
                xin = c.sb_ring("xin", 3, [128, D], F32)

                def load_x(t):
                    xt = xin.next()
                    c.dma("sp", xt[:, :], x_d[t * 128:(t + 1) * 128, :], [x_d], [xt])
                    return xt

                norm_transpose_stream(c, NT, load_x, gT, 0, xnT, rg, ident)
                c.barrier()
            c.stack = sa
            if "xnT_dbg" in dump:
                dbg = dram("xnT_dbg", [128, DC, S], BF16)
                c.dma("sp", dbg[:, :, :], xnT[:, :, :], [xnT], [dbg])
                final_outs.append(dbg.b)
            Cd = c.sb("Cd", [128, S], F32)
            Sd = c.sb("Sd", [128, S], F32)
            Cm = c.sb("Cm", [64, S], F32)
            Sm = c.sb("Sm", [64, S], F32)
            with ExitStack() as sa2:
                c.stack = sa2
                tmp_i = c.sb("tmp_i", [128, S], I32)
                tmp_f = c.sb("tmp_f", [128, S], F32)
                rope_tables(c, pos_d, cf, 0, 128, Cd, Sd, tmp_i, tmp_f)
                rope_tables(c, pos_d, cf, 1, 64, Cm, Sm, tmp_i, tmp_f)
                c.barrier()
            c.stack = sa
            slabs = c.sb_ring("wslab", 3, [128, DC, 512], BF16)
            rg = {"qs": c.sb_ring("qs", 3, [128, 512], BF16), "psr": c.ps_ring("psr", 2, [128, 512], F32),
                  "t1": c.sb_ring("t1", 2, [128, 512], F32), "t2": c.sb_ring("t2", 2, [128, 512], F32)}
            psA = c.ps_ring("psA", 4, [128, 512], F32)
            stg = c.sb_ring("stgA", 4, [128, 512], BF16)
            stgf = c.sb_ring("stgAf", 3, [128, 512], F32)

            def load_slab(c0, ncols):
                sl = slabs.next()
                c.dma("pool", sl[:, :, :ncols], w_in_d[:, c0:c0 + ncols].rearrange("(kc p) n -> p kc n", p=128),
                      [w_in_d], [sl])
                return sl

            def proj_fm(sl, off, width, blk):
                ps = psA.next()
                for kc in range(DC):
                    c.mm(ps[:width, :], sl[:, kc, off:off + width], xnT[:, kc, blk * 512:(blk + 1) * 512],
                         kc == 0, kc == DC - 1, [sl, xnT], [ps])
                return ps

            for sidx in range(2):
                sl = load_slab(2048 + sidx * 512, 512)
                for t in range(NT):
                    ps = psA.next()
                    for kc in range(DC):
                        c.mm(ps[:, :], xnT[:, kc, t * 128:(t + 1) * 128], sl[:, kc, :], kc == 0, kc == DC - 1,
                             [sl, xnT], [ps])
                    st = stg.next()
                    c.copy("act" if t % 2 == 0 else "dve", st[:, :], ps[:, :], [ps], [st])
                    c.dma("sp", v_d[t * 128:(t + 1) * 128, sidx * 512:(sidx + 1) * 512], st[:, :], [st], [v_d])
            pend = []

            def flush_pending():
                while pend:
                    pend.pop(0)()

            for which, dst_d in ((0, qT_d), (1, kT_d)):
                for sidx in range(2):
                    sl = load_slab(which * 1024 + sidx * 512, 512)
                    for j in range(4):
                        hh = sidx * 4 + j
                        for blk in range(4):
                            ps = proj_fm(sl, j * 128, 128, blk)
                            flush_pending()
                            st = stg.next()
                            todo = []
                            rope_evac(c, ps, 128, blk, Rd, Cd, Sd, rg, st[:, :], st, defer=todo)

                            def tail(todo=todo, st=st, dst_d=dst_d, hh=hh, blk=blk):
                                todo[0]()
                                c.dma("sp", dst_d[hh, :, blk * 512:(blk + 1) * 512], st[:, :], [st], [dst_d])
                            pend.append(tail)
            flush_pending()
            sl = load_slab(3072, 512)
            for j in range(4):
                for blk in range(4):
                    ps = proj_fm(sl, j * 128, 128, blk)
                    st = stgf.next()
                    c.copy("act" if blk % 2 == 0 else "dve", st[:, :], ps[:, :], [ps], [st])
                    c.dma("sp", cq_d[j, :, blk * 512:(blk + 1) * 512], st[:, :], [st], [cq_d])
            sl = load_slab(3584, 320)
            for j in range(2):
                for blk in range(4):
                    ps = proj_fm(sl, j * 128, 128, blk)
                    st = stgf.next()
                    c.copy("act" if blk % 2 == 0 else "dve", st[:, :], ps[:, :], [ps], [st])
                    c.dma("sp", ckv_d[j, :, blk * 512:(blk + 1) * 512], st[:, :], [st], [ckv_d])
            for blk in range(4):
                ps = proj_fm(sl, 256, 64, blk)
                st = stg.next()
                rope_evac(c, ps, 64, blk, rm, Cm, Sm, rg, st[:64, :], st)
                c.dma("sp", kpe_d[:, blk * 512:(blk + 1) * 512], st[:64, :], [st], [kpe_d])
            for which, dst_d in ((0, ga_d), (1, gb_d)):
                for sidx in range(4):
                    sl = load_slab(3904 + which * 2048 + sidx * 512, 512)
                    for j in range(4):
                        ch = sidx * 4 + j
                        for blk in range(4):
                            ps = proj_fm(sl, j * 128, 128, blk)
                            st = stg.next()
                            c.act(st[:, :], ps[:, :], AF.Sigmoid, [ps], [st])
                            c.dma("sp", dst_d[ch, :, blk * 512:(blk + 1) * 512], st[:, :], [st], [dst_d])
            c.barrier()
        c.stack = outer
        for nm, tl in (("qT_s", qT_d), ("kT_s", kT_d), ("v_s", v_d), ("cq_s", cq_d), ("ckv_s", ckv_d),
                       ("kpe_s", kpe_d), ("ga_s", ga_d), ("gb_s", gb_d)):
            if nm in dump:
                final_outs.append(tl.b)
        if stop_after == "A":
            final_outs.append(out_d.b)
            c.dma("sp", out_d[0:128, :], x_d[0:128, :], [x_d], [out_d])
            finish()
            return nc

        def stop_here():
            final_outs.append(out_d.b)
            c.dma("sp", out_d[0:128, :], x_d[0:128, :], [x_d], [out_d])
            finish()

        def attn_core(nk_tiles, qblk, score_fn, v_fn, ps_s, e_ring, acc_o, acc_d, scale):
            Es = {}

            def score(kt):
                ps = ps_s.next()
                score_fn(ps, kt)
                E = e_ring.next()
                c.act(E[:, :], ps[:, :], AF.Exp, [ps], [E], scale=scale)
                Es[kt] = E

            score(0)
            for kt in range(nk_tiles):
                if kt + 1 < nk_tiles:
                    score(kt + 1)
                E = Es.pop(kt)
                vap, vt = v_fn(kt)
                last = kt == nk_tiles - 1
                c.mm(acc_o[:, :], vap, E[:, :], kt == 0, last, [vt, E], [acc_o], sig=last)
                c.mm(acc_d[:, :], ones[:, :], E[:, :], kt == 0, last, [ones, E], [acc_d], sig=last)

        def attn_stream(calls, ps_s, e_ring, ps_o, ps_d, scale, look):
            flat = [(ci, kt) for ci, cl in enumerate(calls) for kt in range(0, cl["nk"], 2)]
            Es = {}
            state = {"nxt": 0}

            def score(i):
                ci, kt = flat[i]
                cl = calls[ci]
                if kt == 0 and cl.get("pre_fn") is not None:
                    cl["pre_fn"]()
                ps = ps_s.next()
                cl["score_fn"](ps, 0, kt)
                cl["score_fn"](ps, 512, kt + 1)
                E = e_ring.next()
                c.act(E[:, :], ps[:, :], AF.Exp, [ps], [E], scale=scale)
                Es[i] = E

            def fill():
                if state["nxt"] < len(flat):
                    score(state["nxt"])
                    state["nxt"] += 1

            deferred = []
            for _ in range(look):
                fill()
            for i, (ci, kt) in enumerate(flat):
                fill()
                for dfr in list(deferred):
                    dfr[0] -= 1
                    if dfr[0] <= 0:
                        deferred.remove(dfr)
                        dfr[1]()
                cl = calls[ci]
                if kt == 0:
                    cl["acc_o"] = ps_o.next()
                    cl["acc_d"] = cl["acc_d_tile"] if cl.get("acc_d_tile") is not None else ps_d.next()
                acc_o, acc_d = cl["acc_o"], cl["acc_d"]
                E = Es.pop(i)
                for j in range(2):
                    vap, vt = cl["v_fn"](kt + j)
                    first = (kt + j == 0)
                    last = (kt + j == cl["nk"] - 1)
                    c.mm(acc_o[:, :], vap, E[:, j * 512:(j + 1) * 512], first, last, [vt, E], [acc_o], sig=last)
                    c.mm(acc_d[:, :], ones[:, :], E[:, j * 512:(j + 1) * 512], first, last, [ones, E], [acc_d], sig=last)
                if kt + 2 >= cl["nk"]:
                    tail = cl["post_fn"](acc_o, acc_d)
                    if tail is not None:
                        deferred.append([3, tail])
            for dfr in deferred:
                dfr[1]()

        with ExitStack() as sb_:
            c.stack = sb_
            lamt = c.sb("lamt", [128, 256], F32)
            lams = c.sb("lams", [128, 8], F32)
            ljunk = c.sb("ljunk", [128, 64], F32)
            c.dma("sp", lamt[:, :], lam_d[0:1, :].partition_broadcast(128), [lam_d], [lamt])
            for i in range(2):
                c.tt("dve", ljunk[:, :], lamt[:, i * 128:i * 128 + 64], lamt[:, i * 128 + 64:i * 128 + 128], ALU.mult,
                     [lamt], [ljunk])
                c.p.op("dve", lambda h, i=i: h.reduce_sum(lams[:, i:i + 1], ljunk[:, :], axis=AX.X), bs([ljunk]), bs([lams]))
                c.act(lams[:, 2 + i:3 + i], lams[:, i:i + 1], AF.Exp, [lams], [lams])
            c.tt("dve", lams[:, 4:5], lams[:, 3:4], lams[:, 2:3], ALU.subtract, [lams], [lams])
            c.ts("dve", lams[:, 5:6], lams[:, 4:5], -LAM_INIT, None, ALU.add, None, [lams], [lams])
            neglam = lams[:, 5:6]

            zf = {"i": 0}

            def zero_fill(n):
                for _ in range(n):
                    i = zf["i"]
                    if i < NSLOT // 128:
                        c.dma("sp", xg_d[i * 128:(i + 1) * 128, :], zrow[:, :], [zrow], [xg_d])
                    elif i == NSLOT // 128:
                        c.dma("sp", y_d[NSLOT:NSLOT + 128, :], zrow[:, :], [zrow], [y_d])
                    zf["i"] = i + 1

            qh_r = c.sb_ring("qh", 2, [128, S], BF16)
            kh_r = c.sb_ring("kh", 2, [128, 2, S], BF16)
            for _kt in kh_r.tiles:
                c.memset("pool", _kt[:, :, :], 0.0, [_kt])
            vh_r = c.sb_ring("vh", 2, [128, NT, 128], BF16)
            e_ring = c.sb_ring("E", 4, [128, 1024], BF16)
            ps_s = c.ps_ring("ps_s", 2, [128, 1024], F32)
            ps_o = c.ps_ring("ps_o", 2, [128, 512], F32)
            ps_d = c.ps_ring("ps_d", 2, [128, 512], F32)
            rec_r = c.sb_ring("rec", 3, [128, 512], F32)
            oc_r = c.sb_ring("oc", 6, [128, 512], F32)
            sq_r = c.sb_ring("sq", 3, [128, 512], BF16)
            ost_r = c.sb_ring("ost", 3, [128, 512], BF16)
            heads = {}

            def load_head(hh):
                if hh >= H or hh in heads:
                    return
                qh, kh, vh = qh_r.next(), kh_r.next(), vh_r.next()
                c.dma("sp", qh[:, :], qT_d[hh, :, :], [qT_d], [qh])
                c.dma("sp", kh[0:64, 0, :], kT_d[hh, 0:64, :], [kT_d], [kh])
                c.dma("sp", kh[64:128, 1, :], kT_d[hh, 64:128, :], [kT_d], [kh])
                c.dma("sp", vh[:, :, :], v_d[:, hh * 128:(hh + 1) * 128].rearrange("(kt p) d -> p kt d", p=128),
                      [v_d], [vh])
                heads[hh] = (qh, kh, vh)

            calls = []
            for hh in range(H):
                for qb in range(4):
                    qs_ = slice(qb * 512, (qb + 1) * 512)
                    pair = {}
                    for comp in range(2):
                        r0 = comp * 64

                        def pre_fn(hh=hh, qb=qb, comp=comp):
                            if comp == 0 and qb == 0:
                                load_head(hh)
                            if comp == 0 and qb == 1:
                                load_head(hh + 1)
                            if comp == 0:
                                zero_fill(4)

                        def score_fn(ps, off, kt, comp=comp, hh=hh, qs_=qs_):
                            qh, kh, vh = heads[hh]
                            c.mm(ps[:, off:off + 512], kh[:, comp, kt * 128:(kt + 1) * 128], qh[:, qs_], True, True,
                                 [kh, qh], [ps], sig=(off == 512))

                        def v_fn(kt, hh=hh):
                            vh = heads[hh][2]
                            return vh[:, kt, :], vh

                        def post_fn(acc_o, acc_d, hh=hh, qs_=qs_, comp=comp, pair=pair):
                            rec = rec_r.next()
                            c.recip(rec[:, :], acc_d[:, :], [acc_d], [rec])
                            oc = oc_r.next()
                            c.tt("dve", oc[:, :], acc_o[:, :], rec[:, :], ALU.mult, [acc_o, rec], [oc])
                            pair[comp] = oc
                            if comp == 0:
                                return
                            o = oc_r.next()
                            c.stt(o[:, :], pair[1][:, :], neglam, pair[0][:, :], ALU.mult, ALU.add,
                                  [pair[0], pair[1], lams], [o])
                            sq = sq_r.next()
                            c.tt("pool", sq[:, :], o[:, :], o[:, :], ALU.mult, [o], [sq])
                            return lambda: subln_tail(o, sq, hh, qs_)

                        def subln_tail(o, sq, hh, qs_):
                            pn = ps_d.tiles[1]
                            c.mm(pn[:, :], ones[:, :], sq[:, :], True, True, [ones, sq], [pn])
                            rec = rec_r.next()
                            c.ts("dve", rec[:, :], pn[:, :], 1.0 / 128.0, 1e-5, ALU.mult, ALU.add, [pn], [rec])
                            c.act(rec[:, :], rec[:, :], AF.Ln, [rec], [rec])
                            c.act(rec[:, :], rec[:, :], AF.Exp, [rec], [rec], scale=-0.5)
                            c.tt("pool", o[:, :], o[:, :], rec[:, :], ALU.mult, [o, rec], [o])
                            ost = ost_r.next()
                            c.ts("pool", ost[:, :], o[:, :], gT[:, 54:55], 1.0 - LAM_INIT, ALU.mult, ALU.mult, [o, gT], [ost])
                            c.dma("sp", oa_d[hh, :, qs_], ost[:, :], [ost], [oa_d])

                        calls.append({"nk": NT, "score_fn": score_fn, "v_fn": v_fn, "post_fn": post_fn, "pre_fn": pre_fn,
                                      "acc_d_tile": ps_d.tiles[comp]})
            attn_stream(calls, ps_s, e_ring, ps_o, ps_d, 0.125, 1)
            c.barrier()
        c.stack = outer
        if "oa_s" in dump:
            final_outs.append(oa_d.b)
        if stop_after == "B":
            stop_here()
            return nc

        with ExitStack() as sc_:
            c.stack = sc_
            cqn = c.sb("cqn", [128, 4, S], BF16)
            ckvn = c.sb("ckvn", [128, 2, S], BF16)
            kpe = c.sb("kpe", [128, S], BF16)
            c.memset("pool", kpe[:, :], 0.0, [kpe])
            Cm = c.sb("Cm2", [64, S], F32)
            Sm = c.sb("Sm2", [64, S], F32)
            c.dma("sp", kpe[0:64, :], kpe_d[:, :], [kpe_d], [kpe])
            with ExitStack() as sc1:
                c.stack = sc1
                tmp_i = c.sb("tmp_i2", [64, S], I32)
                tmp_f = c.sb("tmp_f2", [64, S], F32)
                rope_tables(c, pos_d, cf, 1, 64, Cm, Sm, tmp_i, tmp_f)
                c.barrier()
            with ExitStack() as sc2:
                c.stack = sc2
                cf32 = c.sb("cf32", [128, 4, S], F32)
                sq_r = c.sb_ring("sqc", 2, [128, 512], BF16)
                ps_n = c.ps_ring("ps_nc", 2, [128, 512], F32)
                rec_r = c.sb_ring("recc", 2, [128, 512], F32)
                for src_d, nch, dst, goff in ((cq_d, 4, cqn, 48), (ckv_d, 2, ckvn, 52)):
                    for ch in range(nch):
                        c.dma("sp", cf32[:, ch, :], src_d[ch, :, :], [src_d], [cf32])
                    for blk in range(4):
                        cs = slice(blk * 512, (blk + 1) * 512)
                        pn = ps_n.next()
                        for ch in range(nch):
                            sq = sq_r.next()
                            c.act(sq[:, :], cf32[:, ch, cs], AF.Square, [cf32], [sq])
                            c.mm(pn[:, :], ones[:, :], sq[:, :], ch == 0, ch == nch - 1, [ones, sq], [pn], sig=True)
                        rec = rec_r.next()
                        c.ts("dve", rec[:, :], pn[:, :], 1.0 / (nch * 128), EPS, ALU.mult, ALU.add, [pn], [rec])
                        c.p.op("act", lambda h, rec=rec: h.sqrt(rec[:, :], rec[:, :]), bs([rec]), bs([rec]))
                        c.recip(rec[:, :], rec[:, :], [rec], [rec])
                        for ch in range(nch):
                            c.stt(dst[:, ch, cs], cf32[:, ch, cs], gT[:, goff + ch:goff + ch + 1], rec[:, :],
                                  ALU.mult, ALU.mult, [cf32, gT, rec], [dst])
                c.barrier()
            if stop_after == "C1":
                dbg = dram("cqn_dbg", [128, 4, S], BF16)
                c.dma("sp", dbg[:, :, :], cqn[:, :, :], [cqn], [dbg])
                final_outs.append(dbg.b)
                stop_here()
                return nc
            c.stack = sc_
            psA = c.ps_ring("psAC", 2, [128, 512], F32)
            rg = {"qs": c.sb_ring("qsC", 2, [128, 512], BF16), "psr": psA,
                  "t1": c.sb_ring("t1C", 2, [128, 512], F32), "t2": c.sb_ring("t2C", 2, [128, 512], F32)}
            slq_r = c.sb_ring("slq", 2, [128, 4, 192], BF16)
            slkv_r = c.sb_ring("slkv", 2, [128, 2, 256], BF16)
            qn_r = c.sb_ring("qn", 2, [128, S], BF16)
            qp_r = c.sb_ring("qp", 2, [128, S], BF16)
            for _qt in qp_r.tiles:
                c.memset("pool", _qt[:, :], 0.0, [_qt])
            kn_r = c.sb_ring("kn", 2, [128, S], BF16)
            vh_r = c.sb_ring("vhC", 2, [128, NT, 128], BF16)
            ps_s = c.ps_ring("ps_sC", 2, [128, 1024], F32)
            ps_o = c.ps_ring("ps_oC", 1, [128, 512], F32)
            ps_d = c.ps_ring("ps_dC", 1, [128, 512], F32)
            rec_r = c.sb_ring("recC", 2, [128, 512], F32)
            ost_r = c.sb_ring("ostC", 3, [128, 512], BF16)
            e_ring = c.sb_ring("EC", 4, [128, 1024], BF16)
            heads = {}

            def prep_head(hh):
                if hh >= H or hh in heads:
                    return
                slq, slkv = slq_r.next(), slkv_r.next()
                c.dma("pool", slq[:, :, :], w_uq_d[:, hh * 192:(hh + 1) * 192].rearrange("(kc p) n -> p kc n", p=128),
                      [w_uq_d], [slq])
                c.dma("pool", slkv[:, :, :], w_ukv_d[:, hh * 256:(hh + 1) * 256].rearrange("(kc p) n -> p kc n", p=128),
                      [w_ukv_d], [slkv])
                qn, qp, kn, vh = qn_r.next(), qp_r.next(), kn_r.next(), vh_r.next()
                heads[hh] = (qn, qp, kn, vh)
                for blk in range(4):
                    cs = slice(blk * 512, (blk + 1) * 512)
                    ps = psA.next()
                    for kc in range(4):
                        c.mm(ps[:, :], slq[:, kc, 0:128], cqn[:, kc, cs], kc == 0, kc == 3, [slq, cqn], [ps])
                    c.copy("dve", qn[:, cs], ps[:, :], [ps], [qn])
                    ps = psA.next()
                    for kc in range(4):
                        c.mm(ps[:64, :], slq[:, kc, 128:192], cqn[:, kc, cs], kc == 0, kc == 3, [slq, cqn], [ps])
                    rope_evac(c, ps, 64, blk, rm, Cm, Sm, rg, qp[:64, cs], qp)
                    ps = psA.next()
                    for kc in range(2):
                        c.mm(ps[:, :], slkv[:, kc, 0:128], ckvn[:, kc, cs], kc == 0, kc == 1, [slkv, ckvn], [ps])
                    c.copy("dve", kn[:, cs], ps[:, :], [ps], [kn])
                for t4 in range(NT // 4):
                    ps = psA.next()
                    for j in range(4):
                        t = t4 * 4 + j
                        for kc in range(2):
                            c.mm(ps[:, j * 128:(j + 1) * 128], ckvn[:, kc, t * 128:(t + 1) * 128], slkv[:, kc, 128:256],
                                 kc == 0, kc == 1, [slkv, ckvn], [ps], sig=(kc == 1 and j == 3))
                    c.copy("dve", vh[:, t4 * 4:(t4 + 1) * 4, :],
                           ps[:, :].rearrange("p (a b) -> p a b", a=4), [ps], [vh])

            calls = []
            for hh in range(H):
                for qb in range(4):
                    qs_ = slice(qb * 512, (qb + 1) * 512)

                    def pre_fn(hh=hh, qb=qb):
                        if qb == 0:
                            prep_head(hh)
                        if qb == 2:
                            prep_head(hh + 1)

                    def score_fn(ps, off, kt, hh=hh, qs_=qs_):
                        qn, qp, kn, vh = heads[hh]
                        ks = slice(kt * 128, (kt + 1) * 128)
                        c.mm(ps[:, off:off + 512], kn[:, ks], qn[:, qs_], True, False, [kn, qn], [ps], sig=False)
                        c.mm(ps[:, off:off + 512], kpe[:, ks], qp[:, qs_], False, True, [kpe, qp], [ps], sig=(off == 512))

                    def v_fn(kt, hh=hh):
                        vh = heads[hh][3]
                        return vh[:, kt, :], vh

                    def post_fn(acc_o, acc_d, hh=hh, qs_=qs_):
                        rec = rec_r.next()
                        c.recip(rec[:, :], acc_d[:, :], [acc_d], [rec])
                        ost = ost_r.next()
                        c.tt("dve", ost[:, :], acc_o[:, :], rec[:, :], ALU.mult, [acc_o, rec], [ost])
                        c.dma("sp", ob_d[hh, :, qs_], ost[:, :], [ost], [ob_d])

                    calls.append({"nk": NT, "score_fn": score_fn, "v_fn": v_fn, "post_fn": post_fn, "pre_fn": pre_fn})
            attn_stream(calls, ps_s, e_ring, ps_o, ps_d, 192.0 ** -0.5, 1)
            c.barrier()
        c.stack = outer
        if "ob_s" in dump:
            final_outs.append(ob_d.b)
        if stop_after == "C":
            stop_here()
            return nc

        with ExitStack() as sd_:
            c.stack = sd_
            mergedT = c.sb("mergedT", [128, DC, S], BF16)
            with ExitStack() as sd1:
                c.stack = sd1
                oaT = c.sb("oaT", [128, H, S], BF16)
                obT = c.sb("obT", [128, H, S], BF16)
                for hh in range(H):
                    c.dma("sp", oaT[:, hh, :], oa_d[hh, :, :], [oa_d], [oaT])
                    c.dma("sp", obT[:, hh, :], ob_d[hh, :, :], [ob_d], [obT])
                sla_r = c.sb_ring("sla", 2, [128, H, 512], BF16)
                slb_r = c.sb_ring("slb", 2, [128, H, 512], BF16)
                ga_r = c.sb_ring("gaT", 2, [128, S], BF16)
                gb_r = c.sb_ring("gbT", 2, [128, S], BF16)
                ps_a = c.ps_ring("ps_a", 3, [128, 512], F32)
                ps_b = c.ps_ring("ps_b", 3, [128, 512], F32)
                t1_r = c.sb_ring("t1D", 2, [128, 512], F32)
                t2_r = c.sb_ring("t2D", 2, [128, 512], F32)
                for c4 in range(4):
                    sla, slb = sla_r.next(), slb_r.next()
                    c.dma("pool", sla[:, :, :], w_od_d[:, c4 * 512:(c4 + 1) * 512].rearrange("(kc p) n -> p kc n", p=128),
                          [w_od_d], [sla])
                    c.dma("pool", slb[:, :, :], w_om_d[:, c4 * 512:(c4 + 1) * 512].rearrange("(kc p) n -> p kc n", p=128),
                          [w_om_d], [slb])
                    for j in range(4):
                        ch = c4 * 4 + j
                        ga, gb = ga_r.next(), gb_r.next()
                        c.dma("sp", ga[:, :], ga_d[ch, :, :], [ga_d], [ga])
                        c.dma("sp", gb[:, :], gb_d[ch, :, :], [gb_d], [gb])
                        for blk in range(4):
                            cs = slice(blk * 512, (blk + 1) * 512)
                            pa, pb = ps_a.next(), ps_b.next()
                            for kc in range(H):
                                c.mm(pa[:, :], sla[:, kc, j * 128:(j + 1) * 128], oaT[:, kc, cs], kc == 0, kc == H - 1,
                                     [sla, oaT], [pa])
                            for kc in range(H):
                                c.mm(pb[:, :], slb[:, kc, j * 128:(j + 1) * 128], obT[:, kc, cs], kc == 0, kc == H - 1,
                                     [slb, obT], [pb])
                            t1, t2 = t1_r.next(), t2_r.next()
                            c.tt("dve", t1[:, :], pa[:, :], ga[:, cs], ALU.mult, [pa, ga], [t1])
                            c.tt("dve", t2[:, :], pb[:, :], gb[:, cs], ALU.mult, [pb, gb], [t2])
                            c.tt("pool", mergedT[:, ch, cs], t1[:, :], t2[:, :], ALU.add, [t1, t2], [mergedT])
                c.barrier()
            c.stack = sd_
            wout = c.sb("wout", [128, DC, D], BF16)
            for nb in range(4):
                c.dma("pool", wout[:, :, nb * 512:(nb + 1) * 512],
                      w_out_d[:, nb * 512:(nb + 1) * 512].rearrange("(kc p) n -> p kc n", p=128), [w_out_d], [wout])
            xin = c.sb_ring("xinD", 2, [128, D], F32)
            hout = c.sb_ring("houtD", 2, [128, D], F32)
            ps_h = c.ps_ring("ps_h", 4, [128, 512], F32)
            for t in range(NT):
                ts_ = slice(t * 128, (t + 1) * 128)
                xt, ht = xin.next(), hout.next()
                c.dma("sp", xt[:, :], x_d[ts_, :], [x_d], [xt])
                for nb in range(4):
                    ns = slice(nb * 512, (nb + 1) * 512)
                    ps = ps_h.next()
                    for kc in range(DC):
                        c.mm(ps[:, :], mergedT[:, kc, ts_], wout[:, kc, ns], kc == 0, kc == DC - 1, [mergedT, wout], [ps])
                    c.tt("dve", ht[:, ns], ps[:, :], xt[:, ns], ALU.add, [ps, xt], [ht])
                c.dma("sp", h1_d[ts_, :], ht[:, :], [ht], [h1_d])
            c.barrier()
        c.stack = outer
        if "h1_s" in dump:
            final_outs.append(h1_d.b)
        if stop_after == "D":
            stop_here()
            return nc

        with ExitStack() as se_:
            c.stack = se_
            qcT = c.sb("qcT", [128, 4, S], BF16)
            kcT = c.sb("kcT", [128, 4, 256], BF16)
            vc = c.sb("vc", [128, 2, 512], BF16)
            ocT = c.sb("ocT", [128, 4, S], BF16)
            with ExitStack() as se1:
                c.stack = se1
                hn1T_b = [c.sb("hn1T%d" % i, [128, DC, 512], BF16) for i in range(4)]
                memnT = c.sb("memnT", [128, DC, 256], BF16)
                rg = {"junk": c.sb_ring("junkE", 1, [128, D], BF16), "ss": c.sb_ring("ssE", 4, [128, 4], F32),
                      "xs": c.sb_ring("xsE", 2, [128, D], BF16), "pst": c.ps_ring("pstE", 2, [128, 512], BF16)}
                xin = c.sb_ring("xinE", 3, [128, D], F32)
                slab_r = c.sb_ring("slabE", 2, [128, DC, 512], BF16)
                psA = c.ps_ring("psAE", 4, [128, 512], F32)
                for mt in range(2):
                    xt = xin.next()
                    c.dma("sp", xt[:, :], mem_d[mt * 128:(mt + 1) * 128, :], [mem_d], [xt])
                    norm_transpose_tile(c, xt, gT, 32, memnT, mt * 128, rg, ident)
                slq_ = slab_r.next()
                c.dma("pool", slq_[:, :, :], w_cq_d[:, :].rearrange("(kc p) n -> p kc n", p=128), [w_cq_d], [slq_])

                def load_h1(t):
                    xt = xin.next()
                    c.dma("sp", xt[:, :], h1_d[t * 128:(t + 1) * 128, :], [h1_d], [xt])
                    return xt

                def q_proj_block(t):
                    if t % 4 != 3:
                        return
                    blk = t // 4
                    cs = slice(blk * 512, (blk + 1) * 512)
                    for hh in range(4):
                        ps = psA.next()
                        for kc in range(DC):
                            c.mm(ps[:, :], slq_[:, kc, hh * 128:(hh + 1) * 128], hn1T_b[blk][:, kc, :], kc == 0, kc == DC - 1,
                                 [slq_, hn1T_b[blk]], [ps])
                        c.copy("act" if hh % 2 == 0 else "dve", qcT[:, hh, cs], ps[:, :], [ps], [qcT])

                norm_transpose_stream(c, NT, load_h1, gT, 16, None, rg, ident,
                                      dst_fn=lambda t: (hn1T_b[t // 4], (t % 4) * 128), after_fn=q_proj_block)
                sl = slab_r.next()
                c.dma("pool", sl[:, :, :], w_ckv_d[:, 0:512].rearrange("(kc p) n -> p kc n", p=128), [w_ckv_d], [sl])
                for hh in range(4):
                    ps = psA.next()
                    for kc in range(DC):
                        c.mm(ps[:, 0:256], sl[:, kc, hh * 128:(hh + 1) * 128], memnT[:, kc, :], kc == 0, kc == DC - 1,
                             [sl, memnT], [ps])
                    c.copy("act", kcT[:, hh, :], ps[:, 0:256], [ps], [kcT])
                sl = slab_r.next()
                c.dma("pool", sl[:, :, :], w_ckv_d[:, 512:1024].rearrange("(kc p) n -> p kc n", p=128), [w_ckv_d], [sl])
                for mt in range(2):
                    ps = psA.next()
                    for kc in range(DC):
                        c.mm(ps[:, :], memnT[:, kc, mt * 128:(mt + 1) * 128], sl[:, kc, :], kc == 0, kc == DC - 1,
                             [sl, memnT], [ps])
                    c.copy("dve", vc[:, mt, :], ps[:, :], [ps], [vc])
                c.barrier()
            with ExitStack() as se2:
                c.stack = se2
                e_ring = c.sb_ring("EE", 4, [128, 512], BF16)
                ps_s = c.ps_ring("ps_sE", 3, [128, 512], F32)
                ps_o = c.ps_ring("ps_oE", 2, [128, 512], F32)
                ps_d = c.ps_ring("ps_dE", 2, [128, 512], F32)
                rec_r = c.sb_ring("recE", 2, [128, 512], F32)
                for hh in range(4):
                    for qb in range(4):
                        qs_ = slice(qb * 512, (qb + 1) * 512)
                        acc_o, acc_d = ps_o.next(), ps_d.next()

                        def score_fn(ps, kt, hh=hh, qs_=qs_):
                            c.mm(ps[:, :], kcT[:, hh, kt * 128:(kt + 1) * 128], qcT[:, hh, qs_], True, True,
                                 [kcT, qcT], [ps])

                        def v_fn(kt, hh=hh):
                            return vc[:, kt, hh * 128:(hh + 1) * 128], vc

                        attn_core(2, qb, score_fn, v_fn, ps_s, e_ring, acc_o, acc_d, 128.0 ** -0.5)
                        rec = rec_r.next()
                        c.recip(rec[:, :], acc_d[:, :], [acc_d], [rec])
                        c.tt("dve", ocT[:, hh, qs_], acc_o[:, :], rec[:, :], ALU.mult, [acc_o, rec], [ocT])
                c.barrier()
            with ExitStack() as se3:
                c.stack = se3
                wco = c.sb("wco", [128, 4, D], BF16)
                c.dma("pool", wco[:, :, :], w_co_d[:, :].rearrange("(kc p) n -> p kc n", p=128), [w_co_d], [wco])
                gff = c.sb("gff", [128, D], F32)
                c.dma("sp", gff[:, :], grow_d[0:1, :].partition_broadcast(128), [grow_d], [gff])
                wr = c.sb("wr", [128, DC, 36], BF16)
                c.dma("pool", wr[:, :, :], w_r_d[:, :].rearrange("(kc p) n -> p kc n", p=128), [w_r_d], [wr])
                br = c.sb("br", [128, 36], F32)
                c.dma("sp", br[:, :], b_r_d[0:1, :].partition_broadcast(128), [b_r_d], [br])
                A_all = c.sb("A_all", [128, NT, 32], BF16)
                hin = c.sb_ring("hinE", 2, [128, D], F32)
                hout = c.sb_ring("houtE", 2, [128, D], F32)
                ttok_r = c.sb_ring("ttok", 3, [128, D], BF16)
                tTs_r = c.sb_ring("tTs", 2, [128, DC, 128], BF16)
                junk_r = c.sb_ring("junkE3", 1, [128, D], BF16)
                ss_r = c.sb_ring("ssE3", 4, [128, 4], F32)
                rt_r = c.sb_ring("rt", 3, [128, 320], F32)
                ps_h = c.ps_ring("ps_hE", 3, [128, 512], F32)
                pst_r = c.ps_ring("pstE3", 2, [128, 512], BF16)
                ps_l = c.ps_ring("ps_l", 2, [128, 64], F32)
                ps_r = c.ps_ring("ps_r", 1, [128, 64], F32)
                pend_tail = []
                for t in range(NT):
                    ts_ = slice(t * 128, (t + 1) * 128)
                    xt, ht = hin.next(), hout.next()
                    c.dma("sp", xt[:, :], h1_d[ts_, :], [h1_d], [xt])
                    for nb in range(4):
                        ns = slice(nb * 512, (nb + 1) * 512)
                        ps = ps_h.next()
                        for kc in range(4):
                            c.mm(ps[:, :], ocT[:, kc, ts_], wco[:, kc, ns], kc == 0, kc == 3, [ocT, wco], [ps])
                        c.tt("dve", ht[:, ns], ps[:, :], xt[:, ns], ALU.add, [ps, xt], [ht])
                    c.dma("sp", h2_d[ts_, :], ht[:, :], [ht], [h2_d])
                    ss, junk, ttok = ss_r.next(), junk_r.next(), ttok_r.next()
                    rmsnorm_rstd(c, ht, D, ss, junk, EPS)
                    c.stt(ttok[:, :], ht[:, :], ss[:, 2:3], gff[:, :], ALU.mult, ALU.mult, [ht, ss, gff], [ttok])
                    tTs = tTs_r.next()
                    for g4 in range(DC // 4):
                        pt = pst_r.next()
                        for j in range(4):
                            ch = g4 * 4 + j
                            c.tr(pt[:, j * 128:(j + 1) * 128], ttok[:, ch * 128:(ch + 1) * 128], ident[:, :],
                                 [ttok, ident], [pt], sig=(j == 3))
                        c.copy("act" if g4 % 2 == 0 else "dve", tTs[:, g4 * 4:(g4 + 1) * 4, :],
                               pt[:, :].rearrange("p (a b) -> p a b", a=4), [pt], [tTs])
                    pl = ps_l.next()
                    for kc in range(DC):
                        c.mm(pl[:, 0:36], tTs[:, kc, :], wr[:, kc, :], kc == 0, kc == DC - 1, [tTs, wr], [pl])
                    rt = rt_r.next()
                    R_ = [rt]
                    L = rt[:, 0:36]
                    c.tt("dve", L, pl[:, 0:36], br[:, :], ALU.add, [pl, br], R_)
                    def tail(t=t, rt=rt, R_=R_, ttok=ttok):
                        gmax, ngmax, gsum, gp = rt[:, 40:41], rt[:, 41:42], rt[:, 42:43], rt[:, 43:44]
                        c.p.op("dve", lambda h, rt=rt: h.reduce_max(rt[:, 40:41], rt[:, 0:4], axis=AX.X), bs(R_), bs(R_))
                        c.ts("pool", ngmax, gmax, -1.0, None, ALU.mult, None, R_, R_)
                        c.act(rt[:, 44:48], rt[:, 0:4], AF.Exp, R_, R_, bias=ngmax, scale=1.0, accum=gsum)
                        c.recip(gp, gsum, R_, R_)
                        c.ts("pool", rt[:, 48:52], rt[:, 0:4], gmax, None, ALU.is_ge, None, R_, R_)
                        c.ts("pool", rt[:, 52:56], rt[:, 48:52], -1.0, 1e30, ALU.add, ALU.mult, R_, R_)
                        for g in range(4):
                            c.ts("pool", rt[:, 64 + g * 8:72 + g * 8], rt[:, 4 + g * 8:12 + g * 8], rt[:, 52 + g:53 + g], None,
                                 ALU.add, None, R_, R_)
                        Lm = rt[:, 64:96]
                        c.p.op("dve", lambda h, rt=rt: h.max(rt[:, 96:104], rt[:, 64:96]), bs(R_), bs(R_))
                        c.ts("pool", rt[:, 104:136], Lm, rt[:, 96:97], None, ALU.is_equal, None, R_, R_)
                        c.ts("pool", rt[:, 136:168], Lm, rt[:, 97:98], None, ALU.is_equal, None, R_, R_)
                        c.tt("pool", rt[:, 56:57], rt[:, 97:98], rt[:, 96:97], ALU.subtract, R_, R_)
                        c.act(rt[:, 57:58], rt[:, 56:57], AF.Exp, R_, R_)
                        c.ts("pool", rt[:, 58:59], rt[:, 57:58], 1.0, None, ALU.add, None, R_, R_)
                        c.recip(rt[:, 58:59], rt[:, 58:59], R_, R_)
                        c.tt("pool", wts[:, t, 0:1], rt[:, 58:59], gp, ALU.mult, R_, [wts])
                        c.stt(wts[:, t, 1:2], rt[:, 57:58], rt[:, 58:59], gp, ALU.mult, ALU.mult, R_, [wts])
                        c.tt("pool", A_all[:, t, :], rt[:, 104:136], rt[:, 136:168], ALU.add, R_, [A_all])
                        pr = ps_r.next()
                        c.mm(pr[:, 0:32], Utri[:, :], A_all[:, t, :], True, t == 0, [Utri, A_all], [pr], sig=(t == 0))
                        for tp in range(t):
                            c.mm(pr[:, 0:32], ones[:, :], A_all[:, tp, :], False, tp == t - 1, [ones, A_all], [pr],
                                 sig=(tp == t - 1))
                        c.ts("dve", rt[:, 200:232], pr[:, 0:32], float(CAP), 1e6, ALU.is_ge, ALU.mult, [pr], R_)
                        c.tt("dve", rt[:, 168:200], pr[:, 0:32], cf[:, 2:2 + NE], ALU.add, [pr, cf], R_)
                        c.tt("pool", rt[:, 168:200], rt[:, 168:200], rt[:, 200:232], ALU.add, R_, R_)
                        for k, oh0 in ((0, 104), (1, 136)):
                            c.tt("pool", rt[:, 232 + 32 * k:264 + 32 * k], rt[:, oh0:oh0 + 32], rt[:, 168:200], ALU.mult, R_, R_)
                            c.p.op("dve", lambda h, rt=rt, k=k: h.reduce_sum(rt[:, 59 + k:60 + k], rt[:, 232 + 32 * k:264 + 32 * k],
                                                                           axis=AX.X), bs(R_), bs(R_))
                            c.ts("pool", rt[:, 59 + k:60 + k], rt[:, 59 + k:60 + k], float(NSLOT), None, ALU.min, None, R_, R_)
                            c.copy("pool", slot_i[:, t, k:k + 1], rt[:, 59 + k:60 + k], R_, [slot_i])
                        for k in range(0 if NO_SCATTER else 2):
                            c.p.dma("pool", lambda h, ttok=ttok, t=t, k=k: h.indirect_dma_start(
                                out=xg_d[:, :], out_offset=bass.IndirectOffsetOnAxis(ap=slot_i[:, t, k:k + 1], axis=0),
                                in_=ttok[:, :], in_offset=None, bounds_check=bc_reg, oob_is_err=False),
                                bs([ttok, slot_i]), bs([xg_d]))

                    if pend_tail:
                        pend_tail.pop(0)()
                    pend_tail.append(tail)
                while pend_tail:
                    pend_tail.pop(0)()
                c.barrier()
        c.stack = outer
        for nm, tl in (("h2_s", h2_d), ("xg_s", xg_d)):
            if nm in dump:
                final_outs.append(tl.b)
        if "route" in dump:
            rdump = dram("route", [128, NT, 4], F32)
            c.dma("sp", rdump[:, :, 2:4], wts[:, :, :], [wts], [rdump])
            sfl = c.sb("sfl", [128, NT, 2], F32)
            c.copy("dve", sfl[:, :, :], slot_i[:, :, :], [slot_i], [sfl])
            c.dma("sp", rdump[:, :, 0:2], sfl[:, :, :], [sfl], [rdump])
            final_outs.append(rdump.b)
        if stop_after == "E":
            stop_here()
            return nc

        with ExitStack() as sf_:
            c.stack = sf_
            wg_r = c.sb_ring("wg", 2, [128, DC, DFF], BF16)
            wu_r = c.sb_ring("wu", 2, [128, DC, DFF], BF16)
            wd_r = c.sb_ring("wd", 2, [128, 4, D], BF16)
            xgT_r = c.sb_ring("xgT", 2, [128, DC, CAP], BF16)
            hid_r = c.sb_ring("hid", 2, [128, 4, CAP], BF16)
            sg_r = c.sb_ring("sg", 2, [128, CAP], F32)
            y_r = c.sb_ring("yt", 3, [128, D], BF16)
            pst_r = c.ps_ring("pstF", 2, [128, 512], BF16)
            ps_g = c.ps_ring("ps_g", 2, [128, CAP], F32)
            ps_u = c.ps_ring("ps_u", 2, [128, CAP], F32)
            ps_y = c.ps_ring("ps_y", 2, [128, 512], F32)
            NSB = CAP // 128
            xg_r6 = c.sb_ring("xgt6", 2 * NSB, [128, D], BF16)
            W = {}
            XT = {}
            TG = {}

            def load_weights(e):
                if e >= NE:
                    return
                wg, wu, wd = wg_r.next(), wu_r.next(), wd_r.next()
                c.dma("pool", wg[:, :, :], w_eg_d[e, :, :].rearrange("(p kc) n -> p kc n", p=128), [w_eg_d], [wg])
                c.dma("pool", wu[:, :, :], w_eu_d[e, :, :].rearrange("(p kc) n -> p kc n", p=128), [w_eu_d], [wu])
                c.dma("pool", wd[:, :, :], w_ed_d[e, :, :].rearrange("(p kc) n -> p kc n", p=128), [w_ed_d], [wd])
                W[e] = (wg, wu, wd)

            def load_xg(e):
                if e >= NE:
                    return
                xgT = xgT_r.next()
                XT[e] = xgT
                groups = []
                for sb in range(NSB):
                    xg = xg_r6.next()
                    r0 = e * CAP + sb * 128
                    c.dma("sp", xg[:, :], xg_d[r0:r0 + 128, :], [xg_d], [xg])
                    for g4 in range(DC // 4):
                        groups.append((xg, sb, g4))
                TG[e] = groups

            def transpose_groups(e, n):
                if e >= NE:
                    return
                xgT = XT[e]
                for _ in range(n):
                    if not TG[e]:
                        return
                    xg, sb, g4 = TG[e].pop(0)
                    pt = pst_r.next()
                    for j in range(4):
                        ch = g4 * 4 + j
                        c.tr(pt[:, j * 128:(j + 1) * 128], xg[:, ch:D:DC], ident[:, :], [xg, ident], [pt], sig=(j == 3))
                    c.copy("act" if g4 % 2 == 0 else "dve", xgT[:, g4 * 4:(g4 + 1) * 4, sb * 128:(sb + 1) * 128],
                           pt[:, :].rearrange("p (a b) -> p a b", a=4), [pt], [xgT])

            load_xg(0)
            load_xg(1)
            load_weights(0)
            transpose_groups(0, 4 * NSB)
            for e in range(NE):
                load_xg(e + 2)
                load_weights(e + 1)
                wg, wu, wd = W.pop(e)
                xgT = XT.pop(e)
                hid = hid_r.next()
                for dc in range(4):
                    pg, pu = ps_g.next(), ps_u.next()
                    for kc in range(DC):
                        c.mm(pg[:, :], wg[:, kc, dc:DFF:4], xgT[:, kc, :], kc == 0, kc == DC - 1,
                             [wg, xgT], [pg])
                    for kc in range(DC):
                        c.mm(pu[:, :], wu[:, kc, dc:DFF:4], xgT[:, kc, :], kc == 0, kc == DC - 1,
                             [wu, xgT], [pu])
                    transpose_groups(e + 1, 2)
                    sg = sg_r.next()
                    c.act(sg[:, :], pg[:, :], AF.Silu, [pg], [sg])
                    c.tt("dve", hid[:, dc, :], sg[:, :], pu[:, :], ALU.mult, [sg, pu], [hid])
                for sb in range(NSB):
                    yt = y_r.next()
                    for nb in range(4):
                        ns = slice(nb * 512, (nb + 1) * 512)
                        py = ps_y.next()
                        for dc in range(4):
                            c.mm(py[:, :], hid[:, dc, sb * 128:(sb + 1) * 128], wd[:, dc, ns], dc == 0, dc == 3,
                                 [hid, wd], [py])
                        c.copy("act" if nb % 2 == 0 else "dve", yt[:, ns], py[:, :], [py], [yt])
                    transpose_groups(e + 1, 2)
                    r0 = e * CAP + sb * 128
                    c.dma("sp", y_d[r0:r0 + 128, :], yt[:, :], [yt], [y_d])
                transpose_groups(e + 1, 4 * NSB)
            c.barrier()
        c.stack = outer
        if "y_s" in dump:
            final_outs.append(y_d.b)
        if stop_after == "F":
            stop_here()
            return nc

        with ExitStack() as sg_:
            c.stack = sg_
            gfin = c.sb("gfin", [128, D], F32)
            c.dma("sp", gfin[:, :], grow_d[1:2, :].partition_broadcast(128), [grow_d], [gfin])
            hin = c.sb_ring("hinG", 3, [128, D], F32)
            y1_r = c.sb_ring("y1", 3, [128, D], BF16)
            y2_r = c.sb_ring("y2", 3, [128, D], BF16)
            h3_r = c.sb_ring("h3", 3, [128, D], F32)
            o_r = c.sb_ring("og", 2, [128, D], F32)
            junk_r = c.sb_ring("junkG", 1, [128, D], BF16)
            ss_r = c.sb_ring("ssG", 4, [128, 4], F32)
            def g_phase1(t):
                ts_ = slice(t * 128, (t + 1) * 128)
                ht, y1, y2, h3 = hin.next(), y1_r.next(), y2_r.next(), h3_r.next()
                c.dma("sp", ht[:, :], h2_d[ts_, :], [h2_d], [ht])
                for k, yk in ((0, y1), (1, y2)):
                    c.memset("pool", yk[:, :], 0.0, [yk])
                    c.p.dma("pool", lambda h, yk=yk, t=t, k=k: h.indirect_dma_start(
                        out=yk[:, :], out_offset=None, in_=y_d[:, :],
                        in_offset=bass.IndirectOffsetOnAxis(ap=slot_i[:, t, k:k + 1], axis=0),
                        bounds_check=bc_reg, oob_is_err=False), bs([y_d, slot_i]), bs([yk]))
                c.stt(h3[:, :], y1[:, :], wts[:, t, 0:1], ht[:, :], ALU.mult, ALU.add, [y1, wts, ht], [h3])
                c.stt(h3[:, :], y2[:, :], wts[:, t, 1:2], h3[:, :], ALU.mult, ALU.add, [y2, wts, h3], [h3])
                ss, junk = ss_r.next(), junk_r.next()
                c.act(junk[:, :D], h3[:, :D], AF.Square, [h3], [junk, ss], accum=ss[:, 0:1])
                return h3, ss

            def g_phase2(t, h3, ss):
                ts_ = slice(t * 128, (t + 1) * 128)
                og = o_r.next()
                c.ts("dve", ss[:, 1:2], ss[:, 0:1], 1.0 / D, EPS, ALU.mult, ALU.add, [ss], [ss])
                c.p.op("act", lambda h: h.sqrt(ss[:, 3:4], ss[:, 1:2]), bs([ss]), bs([ss]))
                c.recip(ss[:, 2:3], ss[:, 3:4], [ss], [ss])
                c.stt(og[:, :], h3[:, :], ss[:, 2:3], gfin[:, :], ALU.mult, ALU.mult, [h3, ss, gfin], [og])
                c.dma("sp", out_d[ts_, :], og[:, :], [og], [out_d])

            cur = g_phase1(0)
            for t in range(NT):
                nxt = g_phase1(t + 1) if t + 1 < NT else None
                g_phase2(t, *cur)
                cur = nxt
            final_outs.append(out_d.b)
            finish()
        c.stack = outer
    return nc


def _fm(g):
    g = np.asarray(g, np.float32).reshape(-1)
    return np.ascontiguousarray(g.reshape(-1, 128).T)


def host_shared(inputs):
    f32 = np.float32
    bf = ml_dtypes.bfloat16
    m = {}
    for k in ("w_in", "w_o_diff", "w_uq", "w_ukv", "w_o_mla", "w_out", "w_cq", "w_ckv", "w_co",
              "w_expert_gate", "w_expert_up", "w_expert_down"):
        m[k] = np.ascontiguousarray(np.asarray(inputs[k], f32)[0])
    m["w_router"] = np.ascontiguousarray(np.concatenate(
        [np.asarray(inputs["w_router_group"], f32)[0], np.asarray(inputs["w_router_expert"], f32)[0]], axis=1))
    m["b_router"] = np.ascontiguousarray(np.concatenate(
        [np.asarray(inputs["b_router_group"], f32)[0], np.asarray(inputs["b_router_expert"], f32)[0]])[None, :])
    gT = np.concatenate([_fm(inputs["attn_norm_g"][0]), _fm(inputs["cross_norm_g"][0]), _fm(inputs["mem_norm_g"][0]),
                         _fm(inputs["mla_q_norm_g"][0]), _fm(inputs["mla_kv_norm_g"][0]),
                         _fm(inputs["diff_subln_g"][0]), np.zeros((128, 1), f32),
                         _fm(inputs["ffn_norm_g"][0])], axis=1)
    m["gT"] = np.ascontiguousarray(gT, f32)
    m["grow"] = np.ascontiguousarray(np.stack([np.asarray(inputs["ffn_norm_g"], f32)[0],
                                               np.asarray(inputs["final_norm_g"], f32)], axis=0))
    m["lam"] = np.ascontiguousarray(np.stack([np.asarray(inputs[k], f32)[0] for k in
                                              ("diff_lambda_q1", "diff_lambda_k1", "diff_lambda_q2", "diff_lambda_k2")]).reshape(1, 256))
    ident = np.eye(128, dtype=f32)
    ones = np.ones((128, 128), f32)
    Rd = np.zeros((128, 128), f32)
    for blk in range(2):
        for i in range(8):
            Rd[blk * 64 + i + 8, blk * 64 + i] = -1.0
            Rd[blk * 64 + i, blk * 64 + i + 8] = 1.0
    U = np.triu(np.ones((128, 128), f32), k=1)
    m["cb"] = np.ascontiguousarray(np.concatenate([ident, ones, Rd, U], axis=1).astype(bf))
    Rm = np.zeros((64, 64), f32)
    for i in range(32):
        Rm[i + 32, i] = -1.0
        Rm[i, i + 32] = 1.0
    m["rm"] = Rm.astype(bf)
    cf = np.zeros((128, 2 + NE), f32)
    for pp in range(128):
        i = pp % 64
        if i < 16:
            cf[pp, 0] = ROPE_THETA ** (-(i % 8) * 2.0 / 16.0) / (2 * math.pi)
        cf[pp, 1] = ROPE_THETA ** (-(i % 32) * 2.0 / 64.0) / (2 * math.pi)
    cf[:, 2:] = (np.arange(NE, dtype=f32) * CAP)[None, :]
    m["cf"] = cf
    return m


_NC_CACHE = {}
_DEV_CORES = 0


def kernel(**inputs):
    return _run(inputs)


def _run(inputs, stop_after=None, dump=()):
    inputs = {k: np.asarray(v) for k, v in inputs.items()}
    key = (stop_after, tuple(dump))
    if key not in _NC_CACHE:
        _NC_CACHE[key] = build_program(stop_after=stop_after, dump=dump)
    nc = _NC_CACHE[key]
    shared = host_shared(inputs)
    in_maps = []
    ncores = _DEV_CORES or NCORES
    for b in range(ncores):
        m = dict(shared)
        m["x"] = np.ascontiguousarray(inputs["x"][b], np.float32)
        m["mem"] = np.ascontiguousarray(inputs["mem"][b], np.float32)
        m["pos"] = np.ascontiguousarray(inputs["positions"][b], np.int32)[None, :]
        in_maps.append(m)
    res = run_bass_kernel_spmd(nc, in_maps, core_ids=list(range(ncores)))
    out = np.stack([np.asarray(r["out"]) for r in res.results], axis=0).astype(np.float32)
    if dump:
        return out, [{k: np.asarray(r[k]) for k in dump} for r in res.results]
    return out
```

```python
import math
from contextlib import ExitStack

import numpy as np
import ml_dtypes

import concourse.bass as bass
import concourse.mybir as mybir
from concourse.bass_utils import run_bass_kernel_spmd

F32 = mybir.dt.float32
BF16 = mybir.dt.bfloat16
I32 = mybir.dt.int32
ALU = mybir.AluOpType
AF = mybir.ActivationFunctionType
AX = mybir.AxisListType

NCORES = 8
S = 2048
D = 2048
NT = S // 128
DC = D // 128
D_IN = 8000
EPS = 1e-6


class Buf:
    __slots__ = ("name", "last_w", "readers")

    def __init__(self, name):
        self.name = name
        self.last_w = None
        self.readers = {}


class Prog:
    ENGINES = ("pe", "act", "dve", "pool", "sp")

    def __init__(self, nc, stack, n_lanes=12):
        self.nc = nc
        self.handles = {"pe": nc.tensor, "act": nc.scalar, "dve": nc.vector,
                        "pool": nc.gpsimd, "sp": nc.sync}
        self.thunks = {e: [] for e in self.ENGINES}
        self.sems = {}
        self.count = {}
        for e in self.ENGINES:
            self.sems[e] = stack.enter_context(nc.semaphore("c_" + e))
            self.count[e] = 0
        self.lanes = {}
        for q in ("sp", "pool"):
            ls = []
            for i in range(n_lanes):
                key = "l_%s%d" % (q, i)
                self.sems[key] = stack.enter_context(nc.semaphore(key))
                self.count[key] = 0
                ls.append(key)
            self.lanes[q] = ls
        self.lane_rr = {"sp": 0, "pool": 0}
        self.known = {e: {} for e in self.ENGINES}
        self.n_waits = 0
        self.events = {e: [] for e in self.ENGINES}

    def _collect(self, eng, reads, writes):
        need = {}

        def add(tok, same_ok):
            if tok is None:
                return
            k, v = tok
            if k == eng and same_ok:
                return
            if need.get(k, 0) < v:
                need[k] = v

        for b in reads:
            add(b.last_w, same_ok=(eng == "pe"))
        for b in writes:
            add(b.last_w, same_ok=True)
            for k, v in b.readers.items():
                add((k, v), same_ok=True)
        out = []
        kn = self.known[eng]
        for k, v in need.items():
            if kn.get(k, 0) >= v:
                continue
            kn[k] = v
            out.append((k, v))
        return out

    def _emit_waits(self, eng, waits):
        h = self.handles[eng]
        sems = self.sems
        for k, v in waits:
            self.n_waits += 1
            self.events[eng].append(("w", k, v))
            self.thunks[eng].append(lambda h=h, s=sems[k], v=v: h.wait_ge(s, v))

    def _update(self, tok, reads, writes):
        k, v = tok
        for b in reads:
            if b.readers.get(k, 0) < v:
                b.readers[k] = v
        for b in writes:
            b.last_w = tok
            b.readers = {}

    def op(self, eng, fn, reads=(), writes=(), sig=True):
        waits = self._collect(eng, reads, writes)
        self._emit_waits(eng, waits)
        h = self.handles[eng]
        sem = self.sems[eng]
        if sig:
            self.count[eng] += 1
            tok = (eng, self.count[eng])
            self.events[eng].append(("s", eng, 1))
            self.thunks[eng].append(lambda: fn(h).then_inc(sem, 1))
        else:
            tok = (eng, self.count[eng] + 1)
            self.thunks[eng].append(lambda: fn(h))
        self._update(tok, reads, writes)
        return tok

    def dma(self, q, fn, reads=(), writes=()):
        lanes = self.lanes[q]
        lane = lanes[self.lane_rr[q] % len(lanes)]
        self.lane_rr[q] += 1
        waits = self._collect(q, reads, writes)
        prev = self.count[lane]
        if prev > 0 and self.known[q].get(lane, 0) < prev:
            self.known[q][lane] = prev
            waits.append((lane, prev))
        self._emit_waits(q, waits)
        h = self.handles[q]
        sem = self.sems[lane]
        self.count[lane] += 16
        tok = (lane, self.count[lane])
        self.events[q].append(("s", lane, 16))
        self.thunks[q].append(lambda: fn(h).then_inc(sem, 16))
        self._update(tok, reads, writes)
        return tok

    def wait_all(self, eng, bufs):
        waits = self._collect(eng, bufs, ())
        self._emit_waits(eng, waits)

    def check_deadlock(self):
        val = {}
        pos = {e: 0 for e in self.ENGINES}
        progress = True
        while progress:
            progress = False
            for e in self.ENGINES:
                ev = self.events[e]
                i = pos[e]
                while i < len(ev):
                    kind, k, v = ev[i]
                    if kind == "w":
                        if val.get(k, 0) < v:
                            break
                    else:
                        val[k] = val.get(k, 0) + v
                    i += 1
                if i != pos[e]:
                    progress = True
                    pos[e] = i
        stuck = {e: (pos[e], len(self.events[e]), self.events[e][pos[e]]) for e in self.ENGINES
                 if pos[e] < len(self.events[e])}
        if stuck:
            raise RuntimeError("build-time deadlock check failed: %r" % (stuck,))

    def emit(self):
        self.check_deadlock()
        nc = self.nc
        with nc.Block() as block:
            @block.tensor
            def _(e):
                for t in self.thunks["pe"]:
                    t()

            @block.scalar
            def _(e):
                for t in self.thunks["act"]:
                    t()

            @block.vector
            def _(e):
                for t in self.thunks["dve"]:
                    t()

            @block.gpsimd
            def _(e):
                for t in self.thunks["pool"]:
                    t()

            @block.sync
            def _(e):
                for t in self.thunks["sp"]:
                    t()


class Ring:
    def __init__(self, tiles):
        self.tiles = tiles
        self.i = 0

    def next(self):
        t = self.tiles[self.i % len(self.tiles)]
        self.i += 1
        return t


class Tile:
    __slots__ = ("t", "b")

    def __init__(self, t, name):
        self.t = t
        self.b = Buf(name)

    def __getitem__(self, k):
        return self.t[k]


class Ctx:
    def __init__(self, nc, stack):
        self.nc = nc
        self.stack = stack
        self.p = Prog(nc, stack)
        self._n = 0

    def sb(self, name, shape, dt):
        self._n += 1
        return Tile(self.stack.enter_context(self.nc.sbuf_tensor("s%d_%s" % (self._n, name), list(shape), dt)), name)

    def ps(self, name, shape, dt=F32):
        self._n += 1
        return Tile(self.stack.enter_context(self.nc.psum_tensor("p%d_%s" % (self._n, name), list(shape), dt)), name)

    def dram(self, name, shape, dt, kind="Internal"):
        return Tile(self.nc.dram_tensor(name, list(shape), dt, kind=kind), name)

    def sb_ring(self, name, n, shape, dt):
        return Ring([self.sb("%s%d" % (name, i), shape, dt) for i in range(n)])

    def ps_ring(self, name, n, shape, dt=F32):
        return Ring([self.ps("%s%d" % (name, i), shape, dt) for i in range(n)])


H = 8
ROPE_THETA = 500000.0
CAP = 384
NE = 32
NSLOT = NE * CAP
DFF = 512
LAM_INIT = 0.8 - 0.6 * math.exp(-0.3 * 0)
PI = math.pi
NO_SCATTER = False


def bs(tiles):
    return [t.b for t in tiles]


class K(Ctx):
    def mm(self, out, lhsT, rhs, start, stop, R, W, sig=None):
        if sig is None:
            sig = stop
        self.p.op("pe", lambda h: h.matmul(out, lhsT, rhs, start=start, stop=stop), bs(R), bs(W), sig=sig)

    def tr(self, out, in_, ident, R, W, sig=True):
        self.p.op("pe", lambda h: h.transpose(out, in_, ident), bs(R), bs(W), sig=sig)

    def act(self, out, in_, func, R, W, bias=None, scale=None, accum=None):
        kw = {}
        if bias is not None:
            kw["bias"] = bias
        if scale is not None:
            kw["scale"] = scale
        if accum is not None:
            kw["accum_out"] = accum
        self.p.op("act", lambda h: h.activation(out, in_, func, **kw), bs(R), bs(W))

    def ts(self, eng, out, in0, s1, s2, op0, op1, R, W):
        if op1 is None:
            self.p.op(eng, lambda h: h.tensor_scalar(out, in0, s1, None, op0), bs(R), bs(W))
        else:
            self.p.op(eng, lambda h: h.tensor_scalar(out, in0, s1, s2, op0, op1), bs(R), bs(W))

    def tt(self, eng, out, in0, in1, op, R, W):
        self.p.op(eng, lambda h: h.tensor_tensor(out, in0, in1, op), bs(R), bs(W))

    def stt(self, out, in0, scalar, in1, op0, op1, R, W, accum=None):
        if accum is None:
            self.p.op("dve", lambda h: h.scalar_tensor_tensor(out, in0, scalar, in1, op0, op1), bs(R), bs(W))
        else:
            self.p.op("dve", lambda h: h.scalar_tensor_tensor(out, in0, scalar, in1, op0, op1, accum_out=accum),
                      bs(R), bs(W))

    def copy(self, eng, out, in_, R, W):
        if eng == "act":
            self.p.op("act", lambda h: h.copy(out, in_), bs(R), bs(W))
        else:
            self.p.op(eng, lambda h: h.tensor_copy(out, in_), bs(R), bs(W))

    def recip(self, out, in_, R, W):
        self.p.op("dve", lambda h: h.reciprocal(out, in_), bs(R), bs(W))

    def memset(self, eng, out, val, W):
        self.p.op(eng, lambda h: h.memset(out, val), [], bs(W))

    def dma(self, q, out, in_, R, W):
        self.p.dma(q, lambda h: h.dma_start(out=out, in_=in_), bs(R), bs(W))

    def barrier(self):
        p = self.p
        toks = [(e, p.count[e]) for e in p.ENGINES if p.count[e] > 0]
        for q in ("sp", "pool"):
            for lane in p.lanes[q]:
                if p.count[lane] > 0:
                    toks.append((lane, p.count[lane]))
        for e in p.ENGINES:
            w = []
            for k, v in toks:
                if k == e:
                    continue
                if p.known[e].get(k, 0) < v:
                    p.known[e][k] = v
                    w.append((k, v))
            p._emit_waits(e, w)


def rmsnorm_rstd(c, x_t, d, ss, junk, eps):
    c.act(junk[:, :d], x_t[:, :d], AF.Square, [x_t], [junk, ss], accum=ss[:, 0:1])
    c.ts("dve", ss[:, 1:2], ss[:, 0:1], 1.0 / d, eps, ALU.mult, ALU.add, [ss], [ss])
    c.p.op("act", lambda h: h.sqrt(ss[:, 3:4], ss[:, 1:2]), bs([ss]), bs([ss]))
    c.recip(ss[:, 2:3], ss[:, 3:4], [ss], [ss])


def norm_phase1(c, x_t, rg):
    junk = rg["junk"].next()
    ss = rg["ss"].next()
    c.act(junk[:, :D], x_t[:, :D], AF.Square, [x_t], [junk, ss], accum=ss[:, 0:1])
    return ss


def norm_phase2(c, x_t, ss, gT, goff, dstT, tcol, rg, ident):
    xs = rg["xs"].next()
    c.ts("dve", ss[:, 1:2], ss[:, 0:1], 1.0 / D, EPS, ALU.mult, ALU.add, [ss], [ss])
    c.p.op("act", lambda h: h.sqrt(ss[:, 3:4], ss[:, 1:2]), bs([ss]), bs([ss]))
    c.recip(ss[:, 2:3], ss[:, 3:4], [ss], [ss])
    half = D // 2
    c.ts("dve", xs[:, :half], x_t[:, :half], ss[:, 2:3], None, ALU.mult, None, [x_t, ss], [xs])
    c.act(xs[:, half:], x_t[:, half:], AF.Copy, [x_t, ss], [xs], scale=ss[:, 2:3])
    for g4 in range(DC // 4):
        pt = rg["pst"].next()
        for j in range(4):
            ch = g4 * 4 + j
            c.tr(pt[:, j * 128:(j + 1) * 128], xs[:, ch * 128:(ch + 1) * 128], ident[:, :], [xs, ident], [pt],
                 sig=(j == 3))
        for j in range(4):
            ch = g4 * 4 + j
            if j % 2 == 0:
                c.ts("dve", dstT[:, ch, tcol:tcol + 128], pt[:, j * 128:(j + 1) * 128],
                     gT[:, goff + ch:goff + ch + 1], None, ALU.mult, None, [pt, gT], [dstT])
            else:
                c.act(dstT[:, ch, tcol:tcol + 128], pt[:, j * 128:(j + 1) * 128], AF.Copy, [pt, gT], [dstT],
                      scale=gT[:, goff + ch:goff + ch + 1])


def norm_transpose_stream(c, n_tiles, load_fn, gT, goff, dstT, rg, ident, dst_fn=None, after_fn=None):
    cur = load_fn(0)
    ss = norm_phase1(c, cur, rg)
    for t in range(n_tiles):
        nxt = nss = None
        if t + 1 < n_tiles:
            nxt = load_fn(t + 1)
            nss = norm_phase1(c, nxt, rg)
        if dst_fn is None:
            norm_phase2(c, cur, ss, gT, goff, dstT, t * 128, rg, ident)
        else:
            dt_, tcol = dst_fn(t)
            norm_phase2(c, cur, ss, gT, goff, dt_, tcol, rg, ident)
        if after_fn is not None:
            after_fn(t)
        cur, ss = nxt, nss


def norm_transpose_tile(c, x_t, gT, goff, dstT, tcol, rg, ident):
    ss = norm_phase1(c, x_t, rg)
    norm_phase2(c, x_t, ss, gT, goff, dstT, tcol, rg, ident)


def rope_tables(c, pos_d, invf, col, nparts, Ct, St, tmp_i, tmp_f):
    n = nparts
    c.dma("sp", tmp_i[:n, :], pos_d[0:1, :].partition_broadcast(n), [pos_d], [tmp_i])
    c.copy("dve", tmp_f[:n, :], tmp_i[:n, :], [tmp_i], [tmp_f])
    for dst, phase in ((St, 0.0), (Ct, 0.25)):
        c.ts("dve", dst[:n, :], tmp_f[:n, :], invf[:n, col:col + 1], phase, ALU.mult, ALU.add, [tmp_f, invf], [dst])
        c.copy("dve", tmp_i[:n, :], dst[:n, :], [dst], [tmp_i])
        c.copy("dve", Ct[:n, :] if dst is St else tmp_f[:n, :], tmp_i[:n, :], [tmp_i], [Ct if dst is St else tmp_f])
        kf = Ct if dst is St else tmp_f
        c.tt("dve", dst[:n, :], dst[:n, :], kf[:n, :], ALU.subtract, [dst, kf], [dst])
        for _ in range(2):
            c.stt(dst[:n, :], dst[:n, :], 0.5, dst[:n, :], ALU.is_gt, ALU.subtract, [dst], [dst])
        c.act(dst[:n, :], dst[:n, :], AF.Sin, [dst], [dst], scale=2 * PI)


def rope_evac(c, ps, npart, blk, Rm, Ct, St, rg, dst_ap, dst_tile, defer=None):
    qs = rg["qs"].next()
    c.copy("act", qs[:npart, :], ps[:npart, :], [ps], [qs])
    if defer is not None:
        defer.append(lambda: _rope_rest(c, qs, npart, blk, Rm, Ct, St, rg, dst_ap, dst_tile))
        return
    _rope_rest(c, qs, npart, blk, Rm, Ct, St, rg, dst_ap, dst_tile)


def _rope_rest(c, qs, npart, blk, Rm, Ct, St, rg, dst_ap, dst_tile):
    pr = rg["psr"].next()
    c.mm(pr[:npart, :], Rm[:npart, :npart], qs[:npart, :], True, True, [Rm, qs], [pr])
    t1 = rg["t1"].next()
    t2 = rg["t2"].next()
    cs = slice(blk * 512, (blk + 1) * 512)
    c.tt("dve", t1[:npart, :], qs[:npart, :], Ct[:npart, cs], ALU.mult, [qs, Ct], [t1])
    c.tt("dve", t2[:npart, :], pr[:npart, :], St[:npart, cs], ALU.mult, [pr, St], [t2])
    c.tt("pool", dst_ap, t1[:npart, :], t2[:npart, :], ALU.add, [t1, t2], [dst_tile])


def build_program(stop_after=None, dump=()):
    nc = bass.Bass("TRN2", target_bir_lowering=False)
    outer = ExitStack()
    with outer:
        c = K(nc, outer)
        p = c.p

        def dram(name, shape, dt, kind=None):
            if kind is None:
                kind = "ExternalOutput" if name in dump else "Internal"
            return c.dram(name, shape, dt, kind=kind)

        x_d = dram("x", [S, D], F32, "ExternalInput")
        mem_d = dram("mem", [256, D], F32, "ExternalInput")
        pos_d = dram("pos", [1, S], I32, "ExternalInput")
        w_in_d = dram("w_in", [D, D_IN], F32, "ExternalInput")
        w_od_d = dram("w_o_diff", [1024, D], F32, "ExternalInput")
        w_uq_d = dram("w_uq", [512, 1536], F32, "ExternalInput")
        w_ukv_d = dram("w_ukv", [256, 2048], F32, "ExternalInput")
        w_om_d = dram("w_o_mla", [1024, D], F32, "ExternalInput")
        w_out_d = dram("w_out", [D, D], F32, "ExternalInput")
        w_cq_d = dram("w_cq", [D, 512], F32, "ExternalInput")
        w_ckv_d = dram("w_ckv", [D, 1024], F32, "ExternalInput")
        w_co_d = dram("w_co", [512, D], F32, "ExternalInput")
        w_r_d = dram("w_router", [D, 36], F32, "ExternalInput")
        b_r_d = dram("b_router", [1, 36], F32, "ExternalInput")
        w_eg_d = dram("w_expert_gate", [NE, D, DFF], F32, "ExternalInput")
        w_eu_d = dram("w_expert_up", [NE, D, DFF], F32, "ExternalInput")
        w_ed_d = dram("w_expert_down", [NE, DFF, D], F32, "ExternalInput")
        gT_d = dram("gT", [128, 4 * DC + 8], F32, "ExternalInput")
        grow_d = dram("grow", [2, D], F32, "ExternalInput")
        lam_d = dram("lam", [1, 256], F32, "ExternalInput")
        cb_d = dram("cb", [128, 4 * 128], BF16, "ExternalInput")
        rm_d = dram("rm", [64, 64], BF16, "ExternalInput")
        cf_d = dram("cf", [128, 2 + NE], F32, "ExternalInput")
        out_d = dram("out", [S, D], F32, "ExternalOutput")

        qT_d = dram("qT_s", [H, 128, S], BF16)
        kT_d = dram("kT_s", [H, 128, S], BF16)
        v_d = dram("v_s", [S, 1024], BF16)
        cq_d = dram("cq_s", [4, 128, S], F32)
        ckv_d = dram("ckv_s", [2, 128, S], F32)
        kpe_d = dram("kpe_s", [64, S], BF16)
        ga_d = dram("ga_s", [DC, 128, S], BF16)
        gb_d = dram("gb_s", [DC, 128, S], BF16)
        oa_d = dram("oa_s", [H, 128, S], BF16)
        ob_d = dram("ob_s", [H, 128, S], BF16)
        h1_d = dram("h1_s", [S, D], F32)
        h2_d = dram("h2_s", [S, D], F32)
        xg_d = dram("xg_s", [NSLOT + 128, D], BF16)
        y_d = dram("y_s", [NSLOT + 128, D], BF16)

        cb = c.sb("cb", [128, 4 * 128], BF16)
        rm = c.sb("rm", [64, 64], BF16)
        cf = c.sb("cf", [128, 2 + NE], F32)
        gT = c.sb("gTs", [128, 4 * DC + 8], F32)
        c.dma("sp", cb[:, :], cb_d[:, :], [cb_d], [cb])
        c.dma("sp", rm[:, :], rm_d[:, :], [rm_d], [rm])
        c.dma("sp", cf[:, :], cf_d[:, :], [cf_d], [cf])
        c.dma("sp", gT[:, :], gT_d[:, :], [gT_d], [gT])
        ident = Tile(cb.t[:, 0:128], "ident"); ident.b = cb.b
        ones = Tile(cb.t[:, 128:256], "ones"); ones.b = cb.b
        Rd = Tile(cb.t[:, 256:384], "Rd"); Rd.b = cb.b
        Utri = Tile(cb.t[:, 384:512], "Utri"); Utri.b = cb.b
        slot_i = c.sb("slot_i", [128, NT, 2], I32)
        wts = c.sb("wts", [128, NT, 2], F32)
        zrow = c.sb("zrow", [128, D], BF16)
        c.memset("pool", zrow[:, :], 0.0, [zrow])

        final_outs = []
        bc_reg = nc.gpsimd.alloc_register("bc_reg")
        p.thunks["pool"].insert(0, lambda: nc.gpsimd.reg_mov(bc_reg, NSLOT + 127))

        def finish():
            p.wait_all("sp", [b for b in final_outs])
            p.emit()

        with ExitStack() as sa:
            c.stack = sa
            xnT = c.sb("xnT", [128, DC, S], BF16)
            with ExitStack() as sa1:
                c.stack = sa1
                rg = {"junk": c.sb_ring("junk", 1, [128, D], BF16), "ss": c.sb_ring("ss", 4, [128, 4], F32),
                      "xs": c.sb_ring("xs", 2, [128, D], BF16), "pst": c.ps_ring("pst", 2, [128, 512], BF16)}
                xin = c.sb_ring("xin", 3, [128, D], F32)

                def load_x(t):
                    xt = xin.next()
                    c.dma("sp", xt[:, :], x_d[t * 128:(t + 1) * 128, :], [x_d], [xt])
                    return xt

                norm_transpose_stream(c, NT, load_x, gT, 0, xnT, rg, ident)
                c.barrier()
            c.stack = sa
            if "xnT_dbg" in dump:
                dbg = dram("xnT_dbg", [128, DC, S], BF16)
                c.dma("sp", dbg[:, :, :], xnT[:, :, :], [xnT], [dbg])
                final_outs.append(dbg.b)
            Cd = c.sb("Cd", [128, S], F32)
            Sd = c.sb("Sd", [128, S], F32)
            Cm = c.sb("Cm", [64, S], F32)
            Sm = c.sb("Sm", [64, S], F32)
            with ExitStack() as sa2:
                c.stack = sa2
                tmp_i = c.sb("tmp_i", [128, S], I32)
                tmp_f = c.sb("tmp_f", [128, S], F32)
                rope_tables(c, pos_d, cf, 0, 128, Cd, Sd, tmp_i, tmp_f)
                rope_tables(c, pos_d, cf, 1, 64, Cm, Sm, tmp_i, tmp_f)
                c.barrier()
            c.stack = sa
            slabs = c.sb_ring("wslab", 3, [128, DC, 512], BF16)
            rg = {"qs": c.sb_ring("qs", 3, [128, 512], BF16), "psr": c.ps_ring("psr", 2, [128, 512], F32),
                  "t1": c.sb_ring("t1", 2, [128, 512], F32), "t2": c.sb_ring("t2", 2, [128, 512], F32)}
            psA = c.ps_ring("psA", 4, [128, 512], F32)
            stg = c.sb_ring("stgA", 4, [128, 512], BF16)
            stgf = c.sb_ring("stgAf", 3, [128, 512], F32)

            def load_slab(c0, ncols):
                sl = slabs.next()
                c.dma("pool", sl[:, :, :ncols], w_in_d[:, c0:c0 + ncols].rearrange("(kc p) n -> p kc n", p=128),
                      [w_in_d], [sl])
                return sl

            slab_specs = ([(2048, 512), (2560, 512), (0, 512), (512, 512), (1024, 512), (1536, 512), (3072, 512), (3584, 320)]
                          + [(3904 + w_ * 2048 + s_ * 512, 512) for w_ in range(2) for s_ in range(4)])
            slab_q = []
            slab_i = {"i": 0}

            def issue_slab():
                if slab_i["i"] < len(slab_specs):
                    c0_, n_ = slab_specs[slab_i["i"]]
                    slab_i["i"] += 1
                    slab_q.append(((c0_, n_), load_slab(c0_, n_)))

            def get_slab(c0, ncols):
                if not slab_q:
                    issue_slab()
                spec, sl_ = slab_q.pop(0)
                assert spec == (c0, ncols), (spec, c0, ncols)
                issue_slab()
                return sl_

            def proj_fm(sl, off, width, blk):
                ps = psA.next()
                for kc in range(DC):
                    c.mm(ps[:width, :], sl[:, kc, off:off + width], xnT[:, kc, blk * 512:(blk + 1) * 512],
                         kc == 0, kc == DC - 1, [sl, xnT], [ps])
                return ps

            for sidx in range(2):
                sl = get_slab(2048 + sidx * 512, 512)
                for t in range(NT):
                    ps = psA.next()
                    for kc in range(DC):
                        c.mm(ps[:, :], xnT[:, kc, t * 128:(t + 1) * 128], sl[:, kc, :], kc == 0, kc == DC - 1,
                             [sl, xnT], [ps])
                    st = stg.next()
                    c.copy("act" if t % 2 == 0 else "dve", st[:, :], ps[:, :], [ps], [st])
                    c.dma("sp", v_d[t * 128:(t + 1) * 128, sidx * 512:(sidx + 1) * 512], st[:, :], [st], [v_d])
            pend = []

            def flush_pending():
                while pend:
                    pend.pop(0)()

            for which, dst_d in ((0, qT_d), (1, kT_d)):
                for sidx in range(2):
                    sl = get_slab(which * 1024 + sidx * 512, 512)
                    for j in range(4):
                        hh = sidx * 4 + j
                        for blk in range(4):
                            ps = proj_fm(sl, j * 128, 128, blk)
                            flush_pending()
                            st = stg.next()
                            todo = []
                            rope_evac(c, ps, 128, blk, Rd, Cd, Sd, rg, st[:, :], st, defer=todo)

                            def tail(todo=todo, st=st, dst_d=dst_d, hh=hh, blk=blk):
                                todo[0]()
                                c.dma("sp", dst_d[hh, :, blk * 512:(blk + 1) * 512], st[:, :], [st], [dst_d])
                            pend.append(tail)
            flush_pending()
            sl = get_slab(3072, 512)
            for j in range(4):
                for blk in range(4):
                    ps = proj_fm(sl, j * 128, 128, blk)
                    st = stgf.next()
                    c.copy("act" if blk % 2 == 0 else "dve", st[:, :], ps[:, :], [ps], [st])
                    c.dma("sp", cq_d[j, :, blk * 512:(blk + 1) * 512], st[:, :], [st], [cq_d])
            sl = get_slab(3584, 320)
            for j in range(2):
                for blk in range(4):
                    ps = proj_fm(sl, j * 128, 128, blk)
                    st = stgf.next()
                    c.copy("act" if blk % 2 == 0 else "dve", st[:, :], ps[:, :], [ps], [st])
                    c.dma("sp", ckv_d[j, :, blk * 512:(blk + 1) * 512], st[:, :], [st], [ckv_d])
            for blk in range(4):
                ps = proj_fm(sl, 256, 64, blk)
                st = stg.next()
                rope_evac(c, ps, 64, blk, rm, Cm, Sm, rg, st[:64, :], st)
                c.dma("sp", kpe_d[:, blk * 512:(blk + 1) * 512], st[:64, :], [st], [kpe_d])
            for which, dst_d in ((0, ga_d), (1, gb_d)):
                for sidx in range(4):
                    sl = get_slab(3904 + which * 2048 + sidx * 512, 512)
                    for j in range(4):
                        ch = sidx * 4 + j
                        for blk in range(4):
                            ps = proj_fm(sl, j * 128, 128, blk)
                            st = stg.next()
                            c.act(st[:, :], ps[:, :], AF.Sigmoid, [ps], [st])
                            c.dma("sp", dst_d[ch, :, blk * 512:(blk + 1) * 512], st[:, :], [st], [dst_d])
            c.barrier()
        c.stack = outer
        for nm, tl in (("qT_s", qT_d), ("kT_s", kT_d), ("v_s", v_d), ("cq_s", cq_d), ("ckv_s", ckv_d),
                       ("kpe_s", kpe_d), ("ga_s", ga_d), ("gb_s", gb_d)):
            if nm in dump:
                final_outs.append(tl.b)
        if stop_after == "A":
            final_outs.append(out_d.b)
            c.dma("sp", out_d[0:128, :], x_d[0:128, :], [x_d], [out_d])
            finish()
            return nc

        def stop_here():
            final_outs.append(out_d.b)
            c.dma("sp", out_d[0:128, :], x_d[0:128, :], [x_d], [out_d])
            finish()

        def attn_core(nk_tiles, qblk, score_fn, v_fn, ps_s, e_ring, acc_o, acc_d, scale):
            Es = {}

            def score(kt):
                ps = ps_s.next()
                score_fn(ps, kt)
                E = e_ring.next()
                c.act(E[:, :], ps[:, :], AF.Exp, [ps], [E], scale=scale)
                Es[kt] = E

            score(0)
            for kt in range(nk_tiles):
                if kt + 1 < nk_tiles:
                    score(kt + 1)
                E = Es.pop(kt)
                vap, vt = v_fn(kt)
                last = kt == nk_tiles - 1
                c.mm(acc_o[:, :], vap, E[:, :], kt == 0, last, [vt, E], [acc_o], sig=last)
                c.mm(acc_d[:, :], ones[:, :], E[:, :], kt == 0, last, [ones, E], [acc_d], sig=last)

        def attn_stream(calls, ps_s, e_ring, ps_o, ps_d, scale, look):
            flat = [(ci, kt) for ci, cl in enumerate(calls) for kt in range(0, cl["nk"], 2)]
            Es = {}
            state = {"nxt": 0}

            def score(i):
                ci, kt = flat[i]
                cl = calls[ci]
                if kt == 0 and cl.get("pre_fn") is not None:
                    cl["pre_fn"]()
                ps = ps_s.next()
                cl["score_fn"](ps, 0, kt)
                cl["score_fn"](ps, 512, kt + 1)
                E = e_ring.next()
                c.act(E[:, :], ps[:, :], AF.Exp, [ps], [E], scale=scale)
                Es[i] = E

            def fill():
                if state["nxt"] < len(flat):
                    score(state["nxt"])
                    state["nxt"] += 1

            deferred = []
            for _ in range(look):
                fill()
            for i, (ci, kt) in enumerate(flat):
                fill()
                for dfr in list(deferred):
                    dfr[0] -= 1
                    if dfr[0] <= 0:
                        deferred.remove(dfr)
                        dfr[1]()
                cl = calls[ci]
                if kt == 0:
                    cl["acc_o"] = ps_o.next()
                    cl["acc_d"] = cl["acc_d_tile"] if cl.get("acc_d_tile") is not None else ps_d.next()
                acc_o, acc_d = cl["acc_o"], cl["acc_d"]
                E = Es.pop(i)
                for j in range(2):
                    vap, vt = cl["v_fn"](kt + j)
                    first = (kt + j == 0)
                    last = (kt + j == cl["nk"] - 1)
                    c.mm(acc_o[:, :], vap, E[:, j * 512:(j + 1) * 512], first, last, [vt, E], [acc_o], sig=last)
                    c.mm(acc_d[:, :], ones[:, :], E[:, j * 512:(j + 1) * 512], first, last, [ones, E], [acc_d], sig=last)
                if kt + 2 >= cl["nk"]:
                    tail = cl["post_fn"](acc_o, acc_d)
                    if tail is not None:
                        deferred.append([3, tail])
            for dfr in deferred:
                dfr[1]()

        with ExitStack() as sb_:
            c.stack = sb_
            lamt = c.sb("lamt", [128, 256], F32)
            lams = c.sb("lams", [128, 8], F32)
            ljunk = c.sb("ljunk", [128, 64], F32)
            c.dma("sp", lamt[:, :], lam_d[0:1, :].partition_broadcast(128), [lam_d], [lamt])
            for i in range(2):
                c.tt("dve", ljunk[:, :], lamt[:, i * 128:i * 128 + 64], lamt[:, i * 128 + 64:i * 128 + 128], ALU.mult,
                     [lamt], [ljunk])
                c.p.op("dve", lambda h, i=i: h.reduce_sum(lams[:, i:i + 1], ljunk[:, :], axis=AX.X), bs([ljunk]), bs([lams]))
                c.act(lams[:, 2 + i:3 + i], lams[:, i:i + 1], AF.Exp, [lams], [lams])
            c.tt("dve", lams[:, 4:5], lams[:, 3:4], lams[:, 2:3], ALU.subtract, [lams], [lams])
            c.ts("dve", lams[:, 5:6], lams[:, 4:5], -LAM_INIT, None, ALU.add, None, [lams], [lams])
            neglam = lams[:, 5:6]

            zf = {"i": 0}

            def zero_fill(n):
                for _ in range(n):
                    i = zf["i"]
                    if i < NSLOT // 128:
                        c.dma("sp", xg_d[i * 128:(i + 1) * 128, :], zrow[:, :], [zrow], [xg_d])
                    elif i == NSLOT // 128:
                        c.dma("sp", y_d[NSLOT:NSLOT + 128, :], zrow[:, :], [zrow], [y_d])
                    zf["i"] = i + 1

            qh_r = c.sb_ring("qh", 2, [128, S], BF16)
            kh_r = c.sb_ring("kh", 2, [128, 2, S], BF16)
            for _kt in kh_r.tiles:
                c.memset("pool", _kt[:, :, :], 0.0, [_kt])
            vh_r = c.sb_ring("vh", 2, [128, NT, 128], BF16)
            e_ring = c.sb_ring("E", 4, [128, 1024], BF16)
            ps_s = c.ps_ring("ps_s", 2, [128, 1024], F32)
            ps_o = c.ps_ring("ps_o", 2, [128, 512], F32)
            ps_d = c.ps_ring("ps_d", 2, [128, 512], F32)
            rec_r = c.sb_ring("rec", 3, [128, 512], F32)
            oc_r = c.sb_ring("oc", 6, [128, 512], F32)
            sq_r = c.sb_ring("sq", 3, [128, 512], BF16)
            ost_r = c.sb_ring("ost", 3, [128, 512], BF16)
            heads = {}

            def load_head(hh):
                if hh >= H or hh in heads:
                    return
                qh, kh, vh = qh_r.next(), kh_r.next(), vh_r.next()
                c.dma("sp", qh[:, :], qT_d[hh, :, :], [qT_d], [qh])
                c.dma("sp", kh[0:64, 0, :], kT_d[hh, 0:64, :], [kT_d], [kh])
                c.dma("sp", kh[64:128, 1, :], kT_d[hh, 64:128, :], [kT_d], [kh])
                c.dma("sp", vh[:, :, :], v_d[:, hh * 128:(hh + 1) * 128].rearrange("(kt p) d -> p kt d", p=128),
                      [v_d], [vh])
                heads[hh] = (qh, kh, vh)

            calls = []
            for hh in range(H):
                for qb in range(4):
                    qs_ = slice(qb * 512, (qb + 1) * 512)
                    pair = {}
                    for comp in range(2):
                        r0 = comp * 64

                        def pre_fn(hh=hh, qb=qb, comp=comp):
                            if comp == 0 and qb == 0:
                                load_head(hh)
                            if comp == 0 and qb == 1:
                                load_head(hh + 1)
                            if comp == 0:
                                zero_fill(4)

                        def score_fn(ps, off, kt, comp=comp, hh=hh, qs_=qs_):
                            qh, kh, vh = heads[hh]
                            c.mm(ps[:, off:off + 512], kh[:, comp, kt * 128:(kt + 1) * 128], qh[:, qs_], True, True,
                                 [kh, qh], [ps], sig=(off == 512))

                        def v_fn(kt, hh=hh):
                            vh = heads[hh][2]
                            return vh[:, kt, :], vh

                        def post_fn(acc_o, acc_d, hh=hh, qs_=qs_, comp=comp, pair=pair):
                            rec = rec_r.next()
                            c.recip(rec[:, :], acc_d[:, :], [acc_d], [rec])
                            oc = oc_r.next()
                            c.tt("dve", oc[:, :], acc_o[:, :], rec[:, :], ALU.mult, [acc_o, rec], [oc])
                            pair[comp] = oc
                            if comp == 0:
                                return
                            o = oc_r.next()
                            c.stt(o[:, :], pair[1][:, :], neglam, pair[0][:, :], ALU.mult, ALU.add,
                                  [pair[0], pair[1], lams], [o])
                            sq = sq_r.next()
                            c.tt("pool", sq[:, :], o[:, :], o[:, :], ALU.mult, [o], [sq])
                            return lambda: subln_tail(o, sq, hh, qs_)

                        def subln_tail(o, sq, hh, qs_):
                            pn = ps_d.tiles[1]
                            c.mm(pn[:, :], ones[:, :], sq[:, :], True, True, [ones, sq], [pn])
                            rec = rec_r.next()
                            c.ts("dve", rec[:, :], pn[:, :], 1.0 / 128.0, 1e-5, ALU.mult, ALU.add, [pn], [rec])
                            c.act(rec[:, :], rec[:, :], AF.Ln, [rec], [rec])
                            c.act(rec[:, :], rec[:, :], AF.Exp, [rec], [rec], scale=-0.5)
                            c.tt("pool", o[:, :], o[:, :], rec[:, :], ALU.mult, [o, rec], [o])
                            ost = ost_r.next()
                            c.ts("pool", ost[:, :], o[:, :], gT[:, 54:55], 1.0 - LAM_INIT, ALU.mult, ALU.mult, [o, gT], [ost])
                            c.dma("sp", oa_d[hh, :, qs_], ost[:, :], [ost], [oa_d])

                        calls.append({"nk": NT, "score_fn": score_fn, "v_fn": v_fn, "post_fn": post_fn, "pre_fn": pre_fn,
                                      "acc_d_tile": ps_d.tiles[comp]})
            attn_stream(calls, ps_s, e_ring, ps_o, ps_d, 0.125, 1)
            c.barrier()
        c.stack = outer
        if "oa_s" in dump:
            final_outs.append(oa_d.b)
        if stop_after == "B":
            stop_here()
            return nc

        with ExitStack() as sc_:
            c.stack = sc_
            cqn = c.sb("cqn", [128, 4, S], BF16)
            ckvn = c.sb("ckvn", [128, 2, S], BF16)
            kpe = c.sb("kpe", [128, S], BF16)
            c.memset("pool", kpe[:, :], 0.0, [kpe])
            Cm = c.sb("Cm2", [64, S], F32)
            Sm = c.sb("Sm2", [64, S], F32)
            c.dma("sp", kpe[0:64, :], kpe_d[:, :], [kpe_d], [kpe])
            with ExitStack() as sc1:
                c.stack = sc1
                tmp_i = c.sb("tmp_i2", [64, S], I32)
                tmp_f = c.sb("tmp_f2", [64, S], F32)
                rope_tables(c, pos_d, cf, 1, 64, Cm, Sm, tmp_i, tmp_f)
                c.barrier()
            with ExitStack() as sc2:
                c.stack = sc2
                cf32 = c.sb("cf32", [128, 4, S], F32)
                sq_r = c.sb_ring("sqc", 2, [128, 512], BF16)
                ps_n = c.ps_ring("ps_nc", 2, [128, 512], F32)
                rec_r = c.sb_ring("recc", 2, [128, 512], F32)
                for src_d, nch, dst, goff in ((cq_d, 4, cqn, 48), (ckv_d, 2, ckvn, 52)):
                    for ch in range(nch):
                        c.dma("sp", cf32[:, ch, :], src_d[ch, :, :], [src_d], [cf32])
                    for blk in range(4):
                        cs = slice(blk * 512, (blk + 1) * 512)
                        pn = ps_n.next()
                        for ch in range(nch):
                            sq = sq_r.next()
                            c.act(sq[:, :], cf32[:, ch, cs], AF.Square, [cf32], [sq])
                            c.mm(pn[:, :], ones[:, :], sq[:, :], ch == 0, ch == nch - 1, [ones, sq], [pn], sig=True)
                        rec = rec_r.next()
                        c.ts("dve", rec[:, :], pn[:, :], 1.0 / (nch * 128), EPS, ALU.mult, ALU.add, [pn], [rec])
                        c.p.op("act", lambda h, rec=rec: h.sqrt(rec[:, :], rec[:, :]), bs([rec]), bs([rec]))
                        c.recip(rec[:, :], rec[:, :], [rec], [rec])
                        for ch in range(nch):
                            c.stt(dst[:, ch, cs], cf32[:, ch, cs], gT[:, goff + ch:goff + ch + 1], rec[:, :],
                                  ALU.mult, ALU.mult, [cf32, gT, rec], [dst])
                c.barrier()
            if stop_after == "C1":
                dbg = dram("cqn_dbg", [128, 4, S], BF16)
                c.dma("sp", dbg[:, :, :], cqn[:, :, :], [cqn], [dbg])
                final_outs.append(dbg.b)
                stop_here()
                return nc
            c.stack = sc_
            psA = c.ps_ring("psAC", 2, [128, 512], F32)
            rg = {"qs": c.sb_ring("qsC", 2, [128, 512], BF16), "psr": psA,
                  "t1": c.sb_ring("t1C", 2, [128, 512], F32), "t2": c.sb_ring("t2C", 2, [128, 512], F32)}
            slq_r = c.sb_ring("slq", 2, [128, 4, 192], BF16)
            slkv_r = c.sb_ring("slkv", 2, [128, 2, 256], BF16)
            qn_r = c.sb_ring("qn", 2, [128, S], BF16)
            qp_r = c.sb_ring("qp", 2, [128, S], BF16)
            for _qt in qp_r.tiles:
                c.memset("pool", _qt[:, :], 0.0, [_qt])
            kn_r = c.sb_ring("kn", 2, [128, S], BF16)
            vh_r = c.sb_ring("vhC", 2, [128, NT, 128], BF16)
            ps_s = c.ps_ring("ps_sC", 2, [128, 1024], F32)
            ps_o = c.ps_ring("ps_oC", 1, [128, 512], F32)
            ps_d = c.ps_ring("ps_dC", 1, [128, 512], F32)
            rec_r = c.sb_ring("recC", 2, [128, 512], F32)
            ost_r = c.sb_ring("ostC", 3, [128, 512], BF16)
            e_ring = c.sb_ring("EC", 4, [128, 1024], BF16)
            heads = {}

            def prep_head(hh):
                if hh >= H or hh in heads:
                    return
                slq, slkv = slq_r.next(), slkv_r.next()
                c.dma("pool", slq[:, :, :], w_uq_d[:, hh * 192:(hh + 1) * 192].rearrange("(kc p) n -> p kc n", p=128),
                      [w_uq_d], [slq])
                c.dma("pool", slkv[:, :, :], w_ukv_d[:, hh * 256:(hh + 1) * 256].rearrange("(kc p) n -> p kc n", p=128),
                      [w_ukv_d], [slkv])
                qn, qp, kn, vh = qn_r.next(), qp_r.next(), kn_r.next(), vh_r.next()
                heads[hh] = (qn, qp, kn, vh)
                for blk in range(4):
                    cs = slice(blk * 512, (blk + 1) * 512)
                    ps = psA.next()
                    for kc in range(4):
                        c.mm(ps[:, :], slq[:, kc, 0:128], cqn[:, kc, cs], kc == 0, kc == 3, [slq, cqn], [ps])
                    c.copy("dve", qn[:, cs], ps[:, :], [ps], [qn])
                    ps = psA.next()
                    for kc in range(4):
                        c.mm(ps[:64, :], slq[:, kc, 128:192], cqn[:, kc, cs], kc == 0, kc == 3, [slq, cqn], [ps])
                    rope_evac(c, ps, 64, blk, rm, Cm, Sm, rg, qp[:64, cs], qp)
                    ps = psA.next()
                    for kc in range(2):
                        c.mm(ps[:, :], slkv[:, kc, 0:128], ckvn[:, kc, cs], kc == 0, kc == 1, [slkv, ckvn], [ps])
                    c.copy("dve", kn[:, cs], ps[:, :], [ps], [kn])
                for t4 in range(NT // 4):
                    ps = psA.next()
                    for j in range(4):
                        t = t4 * 4 + j
                        for kc in range(2):
                            c.mm(ps[:, j * 128:(j + 1) * 128], ckvn[:, kc, t * 128:(t + 1) * 128], slkv[:, kc, 128:256],
                                 kc == 0, kc == 1, [slkv, ckvn], [ps], sig=(kc == 1 and j == 3))
                    c.copy("dve", vh[:, t4 * 4:(t4 + 1) * 4, :],
                           ps[:, :].rearrange("p (a b) -> p a b", a=4), [ps], [vh])

            calls = []
            for hh in range(H):
                for qb in range(4):
                    qs_ = slice(qb * 512, (qb + 1) * 512)

                    def pre_fn(hh=hh, qb=qb):
                        if qb == 0:
                            prep_head(hh)
                        if qb == 2:
                            prep_head(hh + 1)

                    def score_fn(ps, off, kt, hh=hh, qs_=qs_):
                        qn, qp, kn, vh = heads[hh]
                        ks = slice(kt * 128, (kt + 1) * 128)
                        c.mm(ps[:, off:off + 512], kn[:, ks], qn[:, qs_], True, False, [kn, qn], [ps], sig=False)
                        c.mm(ps[:, off:off + 512], kpe[:, ks], qp[:, qs_], False, True, [kpe, qp], [ps], sig=(off == 512))

                    def v_fn(kt, hh=hh):
                        vh = heads[hh][3]
                        return vh[:, kt, :], vh

                    def post_fn(acc_o, acc_d, hh=hh, qs_=qs_):
                        rec = rec_r.next()
                        c.recip(rec[:, :], acc_d[:, :], [acc_d], [rec])
                        ost = ost_r.next()
                        c.tt("dve", ost[:, :], acc_o[:, :], rec[:, :], ALU.mult, [acc_o, rec], [ost])
                        c.dma("sp", ob_d[hh, :, qs_], ost[:, :], [ost], [ob_d])

                    calls.append({"nk": NT, "score_fn": score_fn, "v_fn": v_fn, "post_fn": post_fn, "pre_fn": pre_fn})
            attn_stream(calls, ps_s, e_ring, ps_o, ps_d, 192.0 ** -0.5, 1)
            c.barrier()
        c.stack = outer
        if "ob_s" in dump:
            final_outs.append(ob_d.b)
        if stop_after == "C":
            stop_here()
            return nc

        with ExitStack() as sd_:
            c.stack = sd_
            mergedT = c.sb("mergedT", [128, DC, S], BF16)
            with ExitStack() as sd1:
                c.stack = sd1
                oaT = c.sb("oaT", [128, H, S], BF16)
                obT = c.sb("obT", [128, H, S], BF16)
                for hh in range(H):
                    c.dma("sp", oaT[:, hh, :], oa_d[hh, :, :], [oa_d], [oaT])
                    c.dma("sp", obT[:, hh, :], ob_d[hh, :, :], [ob_d], [obT])
                sla_r = c.sb_ring("sla", 2, [128, H, 512], BF16)
                slb_r = c.sb_ring("slb", 2, [128, H, 512], BF16)
                ga_r = c.sb_ring("gaT", 2, [128, S], BF16)
                gb_r = c.sb_ring("gbT", 2, [128, S], BF16)
                ps_a = c.ps_ring("ps_a", 3, [128, 512], F32)
                ps_b = c.ps_ring("ps_b", 3, [128, 512], F32)
                t1_r = c.sb_ring("t1D", 2, [128, 512], F32)
                t2_r = c.sb_ring("t2D", 2, [128, 512], F32)
                slabs_d = {}
                gates_d = {}

                def load_slabs_d(c4):
                    if c4 >= 4 or c4 in slabs_d:
                        return
                    sla, slb = sla_r.next(), slb_r.next()
                    c.dma("pool", sla[:, :, :], w_od_d[:, c4 * 512:(c4 + 1) * 512].rearrange("(kc p) n -> p kc n", p=128),
                          [w_od_d], [sla])
                    c.dma("pool", slb[:, :, :], w_om_d[:, c4 * 512:(c4 + 1) * 512].rearrange("(kc p) n -> p kc n", p=128),
                          [w_om_d], [slb])
                    slabs_d[c4] = (sla, slb)

                def load_gates_d(ch):
                    if ch >= DC or ch in gates_d:
                        return
                    ga, gb = ga_r.next(), gb_r.next()
                    c.dma("sp", ga[:, :], ga_d[ch, :, :], [ga_d], [ga])
                    c.dma("sp", gb[:, :], gb_d[ch, :, :], [gb_d], [gb])
                    gates_d[ch] = (ga, gb)

                load_slabs_d(0)
                load_gates_d(0)
                for c4 in range(4):
                    sla, slb = slabs_d.pop(c4)
                    for j in range(4):
                        ch = c4 * 4 + j
                        ga, gb = gates_d.pop(ch)
                        load_gates_d(ch + 1)
                        if j == 1:
                            load_slabs_d(c4 + 1)
                        for blk in range(4):
                            cs = slice(blk * 512, (blk + 1) * 512)
                            pa, pb = ps_a.next(), ps_b.next()
                            for kc in range(H):
                                c.mm(pa[:, :], sla[:, kc, j * 128:(j + 1) * 128], oaT[:, kc, cs], kc == 0, kc == H - 1,
                                     [sla, oaT], [pa])
                            for kc in range(H):
                                c.mm(pb[:, :], slb[:, kc, j * 128:(j + 1) * 128], obT[:, kc, cs], kc == 0, kc == H - 1,
                                     [slb, obT], [pb])
                            t1, t2 = t1_r.next(), t2_r.next()
                            c.tt("dve", t1[:, :], pa[:, :], ga[:, cs], ALU.mult, [pa, ga], [t1])
                            c.tt("dve", t2[:, :], pb[:, :], gb[:, cs], ALU.mult, [pb, gb], [t2])
                            c.tt("pool", mergedT[:, ch, cs], t1[:, :], t2[:, :], ALU.add, [t1, t2], [mergedT])
                c.barrier()
            c.stack = sd_
            wout = c.sb("wout", [128, DC, D], BF16)
            for nb in range(4):
                c.dma("pool", wout[:, :, nb * 512:(nb + 1) * 512],
                      w_out_d[:, nb * 512:(nb + 1) * 512].rearrange("(kc p) n -> p kc n", p=128), [w_out_d], [wout])
            xin = c.sb_ring("xinD", 2, [128, D], F32)
            hout = c.sb_ring("houtD", 2, [128, D], F32)
            ps_h = c.ps_ring("ps_h", 4, [128, 512], F32)
            for t in range(NT):
                ts_ = slice(t * 128, (t + 1) * 128)
                xt, ht = xin.next(), hout.next()
                c.dma("sp", xt[:, :], x_d[ts_, :], [x_d], [xt])
                for nb in range(4):
                    ns = slice(nb * 512, (nb + 1) * 512)
                    ps = ps_h.next()
                    for kc in range(DC):
                        c.mm(ps[:, :], mergedT[:, kc, ts_], wout[:, kc, ns], kc == 0, kc == DC - 1, [mergedT, wout], [ps])
                    c.tt("dve", ht[:, ns], ps[:, :], xt[:, ns], ALU.add, [ps, xt], [ht])
                c.dma("sp", h1_d[ts_, :], ht[:, :], [ht], [h1_d])
            c.barrier()
        c.stack = outer
        if "h1_s" in dump:
            final_outs.append(h1_d.b)
        if stop_after == "D":
            stop_here()
            return nc

        with ExitStack() as se_:
            c.stack = se_
            qcT = c.sb("qcT", [128, 4, S], BF16)
            kcT = c.sb("kcT", [128, 4, 256], BF16)
            vc = c.sb("vc", [128, 2, 512], BF16)
            ocT = c.sb("ocT", [128, 4, S], BF16)
            with ExitStack() as se1:
                c.stack = se1
                hn1T_b = [c.sb("hn1T%d" % i, [128, DC, 512], BF16) for i in range(4)]
                memnT = c.sb("memnT", [128, DC, 256], BF16)
                rg = {"junk": c.sb_ring("junkE", 1, [128, D], BF16), "ss": c.sb_ring("ssE", 4, [128, 4], F32),
                      "xs": c.sb_ring("xsE", 2, [128, D], BF16), "pst": c.ps_ring("pstE", 2, [128, 512], BF16)}
                xin = c.sb_ring("xinE", 3, [128, D], F32)
                slab_r = c.sb_ring("slabE", 2, [128, DC, 512], BF16)
                psA = c.ps_ring("psAE", 4, [128, 512], F32)
                for mt in range(2):
                    xt = xin.next()
                    c.dma("sp", xt[:, :], mem_d[mt * 128:(mt + 1) * 128, :], [mem_d], [xt])
                    norm_transpose_tile(c, xt, gT, 32, memnT, mt * 128, rg, ident)
                slq_ = slab_r.next()
                c.dma("pool", slq_[:, :, :], w_cq_d[:, :].rearrange("(kc p) n -> p kc n", p=128), [w_cq_d], [slq_])

                def load_h1(t):
                    xt = xin.next()
                    c.dma("sp", xt[:, :], h1_d[t * 128:(t + 1) * 128, :], [h1_d], [xt])
                    return xt

                def q_proj_block(t):
                    if t % 4 != 3:
                        return
                    blk = t // 4
                    cs = slice(blk * 512, (blk + 1) * 512)
                    for hh in range(4):
                        ps = psA.next()
                        for kc in range(DC):
                            c.mm(ps[:, :], slq_[:, kc, hh * 128:(hh + 1) * 128], hn1T_b[blk][:, kc, :], kc == 0, kc == DC - 1,
                                 [slq_, hn1T_b[blk]], [ps])
                        c.copy("act" if hh % 2 == 0 else "dve", qcT[:, hh, cs], ps[:, :], [ps], [qcT])

                norm_transpose_stream(c, NT, load_h1, gT, 16, None, rg, ident,
                                      dst_fn=lambda t: (hn1T_b[t // 4], (t % 4) * 128), after_fn=q_proj_block)
                sl = slab_r.next()
                c.dma("pool", sl[:, :, :], w_ckv_d[:, 0:512].rearrange("(kc p) n -> p kc n", p=128), [w_ckv_d], [sl])
                for hh in range(4):
                    ps = psA.next()
                    for kc in range(DC):
                        c.mm(ps[:, 0:256], sl[:, kc, hh * 128:(hh + 1) * 128], memnT[:, kc, :], kc == 0, kc == DC - 1,
                             [sl, memnT], [ps])
                    c.copy("act", kcT[:, hh, :], ps[:, 0:256], [ps], [kcT])
                sl = slab_r.next()
                c.dma("pool", sl[:, :, :], w_ckv_d[:, 512:1024].rearrange("(kc p) n -> p kc n", p=128), [w_ckv_d], [sl])
                for mt in range(2):
                    ps = psA.next()
                    for kc in range(DC):
                        c.mm(ps[:, :], memnT[:, kc, mt * 128:(mt + 1) * 128], sl[:, kc, :], kc == 0, kc == DC - 1,
                             [sl, memnT], [ps])
                    c.copy("dve", vc[:, mt, :], ps[:, :], [ps], [vc])
                c.barrier()
            with ExitStack() as se2:
                c.stack = se2
                e_ring = c.sb_ring("EE", 4, [128, 512], BF16)
                ps_s = c.ps_ring("ps_sE", 3, [128, 512], F32)
                ps_o = c.ps_ring("ps_oE", 2, [128, 512], F32)
                ps_d = c.ps_ring("ps_dE", 2, [128, 512], F32)
                rec_r = c.sb_ring("recE", 2, [128, 512], F32)
                for hh in range(4):
                    for qb in range(4):
                        qs_ = slice(qb * 512, (qb + 1) * 512)
                        acc_o, acc_d = ps_o.next(), ps_d.next()

                        def score_fn(ps, kt, hh=hh, qs_=qs_):
                            c.mm(ps[:, :], kcT[:, hh, kt * 128:(kt + 1) * 128], qcT[:, hh, qs_], True, True,
                                 [kcT, qcT], [ps])

                        def v_fn(kt, hh=hh):
                            return vc[:, kt, hh * 128:(hh + 1) * 128], vc

                        attn_core(2, qb, score_fn, v_fn, ps_s, e_ring, acc_o, acc_d, 128.0 ** -0.5)
                        rec = rec_r.next()
                        c.recip(rec[:, :], acc_d[:, :], [acc_d], [rec])
                        c.tt("dve", ocT[:, hh, qs_], acc_o[:, :], rec[:, :], ALU.mult, [acc_o, rec], [ocT])
                c.barrier()
            with ExitStack() as se3:
                c.stack = se3
                wco = c.sb("wco", [128, 4, D], BF16)
                c.dma("pool", wco[:, :, :], w_co_d[:, :].rearrange("(kc p) n -> p kc n", p=128), [w_co_d], [wco])
                gff = c.sb("gff", [128, D], F32)
                c.dma("sp", gff[:, :], grow_d[0:1, :].partition_broadcast(128), [grow_d], [gff])
                wr = c.sb("wr", [128, DC, 36], BF16)
                c.dma("pool", wr[:, :, :], w_r_d[:, :].rearrange("(kc p) n -> p kc n", p=128), [w_r_d], [wr])
                br = c.sb("br", [128, 36], F32)
                c.dma("sp", br[:, :], b_r_d[0:1, :].partition_broadcast(128), [b_r_d], [br])
                A_all = c.sb("A_all", [128, NT, 32], BF16)
                hin = c.sb_ring("hinE", 2, [128, D], F32)
                hout = c.sb_ring("houtE", 2, [128, D], F32)
                ttok_r = c.sb_ring("ttok", 3, [128, D], BF16)
                tTs_r = c.sb_ring("tTs", 2, [128, DC, 128], BF16)
                junk_r = c.sb_ring("junkE3", 1, [128, D], BF16)
                ss_r = c.sb_ring("ssE3", 4, [128, 4], F32)
                rt_r = c.sb_ring("rt", 3, [128, 320], F32)
                ps_h = c.ps_ring("ps_hE", 3, [128, 512], F32)
                pst_r = c.ps_ring("pstE3", 2, [128, 512], BF16)
                ps_l = c.ps_ring("ps_l", 2, [128, 64], F32)
                ps_r = c.ps_ring("ps_r", 1, [128, 64], F32)
                pend_tail = []
                for t in range(NT):
                    ts_ = slice(t * 128, (t + 1) * 128)
                    xt, ht = hin.next(), hout.next()
                    c.dma("sp", xt[:, :], h1_d[ts_, :], [h1_d], [xt])
                    for nb in range(4):
                        ns = slice(nb * 512, (nb + 1) * 512)
                        ps = ps_h.next()
                        for kc in range(4):
                            c.mm(ps[:, :], ocT[:, kc, ts_], wco[:, kc, ns], kc == 0, kc == 3, [ocT, wco], [ps])
                        c.tt("dve", ht[:, ns], ps[:, :], xt[:, ns], ALU.add, [ps, xt], [ht])
                    c.dma("sp", h2_d[ts_, :], ht[:, :], [ht], [h2_d])
                    ss, junk, ttok = ss_r.next(), junk_r.next(), ttok_r.next()
                    rmsnorm_rstd(c, ht, D, ss, junk, EPS)
                    c.stt(ttok[:, :], ht[:, :], ss[:, 2:3], gff[:, :], ALU.mult, ALU.mult, [ht, ss, gff], [ttok])
                    tTs = tTs_r.next()
                    for g4 in range(DC // 4):
                        pt = pst_r.next()
                        for j in range(4):
                            ch = g4 * 4 + j
                            c.tr(pt[:, j * 128:(j + 1) * 128], ttok[:, ch * 128:(ch + 1) * 128], ident[:, :],
                                 [ttok, ident], [pt], sig=(j == 3))
                        c.copy("act" if g4 % 2 == 0 else "dve", tTs[:, g4 * 4:(g4 + 1) * 4, :],
                               pt[:, :].rearrange("p (a b) -> p a b", a=4), [pt], [tTs])
                    pl = ps_l.next()
                    for kc in range(DC):
                        c.mm(pl[:, 0:36], tTs[:, kc, :], wr[:, kc, :], kc == 0, kc == DC - 1, [tTs, wr], [pl])
                    rt = rt_r.next()
                    R_ = [rt]
                    L = rt[:, 0:36]
                    c.tt("dve", L, pl[:, 0:36], br[:, :], ALU.add, [pl, br], R_)
                    def tail(t=t, rt=rt, R_=R_, ttok=ttok):
                        gmax, ngmax, gsum, gp = rt[:, 40:41], rt[:, 41:42], rt[:, 42:43], rt[:, 43:44]
                        c.p.op("dve", lambda h, rt=rt: h.reduce_max(rt[:, 40:41], rt[:, 0:4], axis=AX.X), bs(R_), bs(R_))
                        c.ts("pool", ngmax, gmax, -1.0, None, ALU.mult, None, R_, R_)
                        c.act(rt[:, 44:48], rt[:, 0:4], AF.Exp, R_, R_, bias=ngmax, scale=1.0, accum=gsum)
                        c.recip(gp, gsum, R_, R_)
                        c.ts("pool", rt[:, 48:52], rt[:, 0:4], gmax, None, ALU.is_ge, None, R_, R_)
                        c.ts("pool", rt[:, 52:56], rt[:, 48:52], -1.0, 1e30, ALU.add, ALU.mult, R_, R_)
                        for g in range(4):
                            c.ts("pool", rt[:, 64 + g * 8:72 + g * 8], rt[:, 4 + g * 8:12 + g * 8], rt[:, 52 + g:53 + g], None,
                                 ALU.add, None, R_, R_)
                        Lm = rt[:, 64:96]
                        c.p.op("dve", lambda h, rt=rt: h.max(rt[:, 96:104], rt[:, 64:96]), bs(R_), bs(R_))
                        c.ts("pool", rt[:, 104:136], Lm, rt[:, 96:97], None, ALU.is_equal, None, R_, R_)
                        c.ts("pool", rt[:, 136:168], Lm, rt[:, 97:98], None, ALU.is_equal, None, R_, R_)
                        c.tt("pool", rt[:, 56:57], rt[:, 97:98], rt[:, 96:97], ALU.subtract, R_, R_)
                        c.act(rt[:, 57:58], rt[:, 56:57], AF.Exp, R_, R_)
                        c.ts("pool", rt[:, 58:59], rt[:, 57:58], 1.0, None, ALU.add, None, R_, R_)
                        c.recip(rt[:, 58:59], rt[:, 58:59], R_, R_)
                        c.tt("pool", wts[:, t, 0:1], rt[:, 58:59], gp, ALU.mult, R_, [wts])
                        c.stt(wts[:, t, 1:2], rt[:, 57:58], rt[:, 58:59], gp, ALU.mult, ALU.mult, R_, [wts])
                        c.tt("pool", A_all[:, t, :], rt[:, 104:136], rt[:, 136:168], ALU.add, R_, [A_all])
                        pr = ps_r.next()
                        c.mm(pr[:, 0:32], Utri[:, :], A_all[:, t, :], True, t == 0, [Utri, A_all], [pr], sig=(t == 0))
                        for tp in range(t):
                            c.mm(pr[:, 0:32], ones[:, :], A_all[:, tp, :], False, tp == t - 1, [ones, A_all], [pr],
                                 sig=(tp == t - 1))
                        c.ts("dve", rt[:, 200:232], pr[:, 0:32], float(CAP), 1e6, ALU.is_ge, ALU.mult, [pr], R_)
                        c.tt("dve", rt[:, 168:200], pr[:, 0:32], cf[:, 2:2 + NE], ALU.add, [pr, cf], R_)
                        c.tt("pool", rt[:, 168:200], rt[:, 168:200], rt[:, 200:232], ALU.add, R_, R_)
                        for k, oh0 in ((0, 104), (1, 136)):
                            c.tt("pool", rt[:, 232 + 32 * k:264 + 32 * k], rt[:, oh0:oh0 + 32], rt[:, 168:200], ALU.mult, R_, R_)
                            c.p.op("dve", lambda h, rt=rt, k=k: h.reduce_sum(rt[:, 59 + k:60 + k], rt[:, 232 + 32 * k:264 + 32 * k],
                                                                           axis=AX.X), bs(R_), bs(R_))
                            c.ts("pool", rt[:, 59 + k:60 + k], rt[:, 59 + k:60 + k], float(NSLOT), None, ALU.min, None, R_, R_)
                            c.copy("pool", slot_i[:, t, k:k + 1], rt[:, 59 + k:60 + k], R_, [slot_i])
                        for k in range(0 if NO_SCATTER else 2):
                            c.p.dma("pool", lambda h, ttok=ttok, t=t, k=k: h.indirect_dma_start(
                                out=xg_d[:, :], out_offset=bass.IndirectOffsetOnAxis(ap=slot_i[:, t, k:k + 1], axis=0),
                                in_=ttok[:, :], in_offset=None, bounds_check=bc_reg, oob_is_err=False),
                                bs([ttok, slot_i]), bs([xg_d]))

                    if pend_tail:
                        pend_tail.pop(0)()
                    pend_tail.append(tail)
                while pend_tail:
                    pend_tail.pop(0)()
                c.barrier()
        c.stack = outer
        for nm, tl in (("h2_s", h2_d), ("xg_s", xg_d)):
            if nm in dump:
                final_outs.append(tl.b)
        if "route" in dump:
            rdump = dram("route", [128, NT, 4], F32)
            c.dma("sp", rdump[:, :, 2:4], wts[:, :, :], [wts], [rdump])
            sfl = c.sb("sfl", [128, NT, 2], F32)
            c.copy("dve", sfl[:, :, :], slot_i[:, :, :], [slot_i], [sfl])
            c.dma("sp", rdump[:, :, 0:2], sfl[:, :, :], [sfl], [rdump])
            final_outs.append(rdump.b)
        if stop_after == "E":
            stop_here()
            return nc

        with ExitStack() as sf_:
            c.stack = sf_
            wg_r = c.sb_ring("wg", 2, [128, DC, DFF], BF16)
            wu_r = c.sb_ring("wu", 2, [128, DC, DFF], BF16)
            wd_r = c.sb_ring("wd", 2, [128, 4, D], BF16)
            xgT_r = c.sb_ring("xgT", 2, [128, DC, CAP], BF16)
            hid_r = c.sb_ring("hid", 2, [128, 4, CAP], BF16)
            sg_r = c.sb_ring("sg", 2, [128, CAP], F32)
            y_r = c.sb_ring("yt", 3, [128, D], BF16)
            pst_r = c.ps_ring("pstF", 2, [128, 512], BF16)
            ps_g = c.ps_ring("ps_g", 2, [128, CAP], F32)
            ps_u = c.ps_ring("ps_u", 2, [128, CAP], F32)
            ps_y = c.ps_ring("ps_y", 2, [128, 512], F32)
            NSB = CAP // 128
            xg_r6 = c.sb_ring("xgt6", 2 * NSB, [128, D], BF16)
            W = {}
            XT = {}
            TG = {}

            def load_weights(e):
                if e >= NE:
                    return
                wg, wu, wd = wg_r.next(), wu_r.next(), wd_r.next()
                c.dma("pool", wg[:, :, :], w_eg_d[e, :, :].rearrange("(p kc) n -> p kc n", p=128), [w_eg_d], [wg])
                c.dma("pool", wu[:, :, :], w_eu_d[e, :, :].rearrange("(p kc) n -> p kc n", p=128), [w_eu_d], [wu])
                c.dma("pool", wd[:, :, :], w_ed_d[e, :, :].rearrange("(p kc) n -> p kc n", p=128), [w_ed_d], [wd])
                W[e] = (wg, wu, wd)

            def load_xg(e):
                if e >= NE:
                    return
                xgT = xgT_r.next()
                XT[e] = xgT
                groups = []
                for sb in range(NSB):
                    xg = xg_r6.next()
                    r0 = e * CAP + sb * 128
                    c.dma("sp", xg[:, :], xg_d[r0:r0 + 128, :], [xg_d], [xg])
                    for g4 in range(DC // 4):
                        groups.append((xg, sb, g4))
                TG[e] = groups

            def transpose_groups(e, n):
                if e >= NE:
                    return
                xgT = XT[e]
                for _ in range(n):
                    if not TG[e]:
                        return
                    xg, sb, g4 = TG[e].pop(0)
                    pt = pst_r.next()
                    for j in range(4):
                        ch = g4 * 4 + j
                        c.tr(pt[:, j * 128:(j + 1) * 128], xg[:, ch:D:DC], ident[:, :], [xg, ident], [pt], sig=(j == 3))
                    c.copy("act" if g4 % 2 == 0 else "dve", xgT[:, g4 * 4:(g4 + 1) * 4, sb * 128:(sb + 1) * 128],
                           pt[:, :].rearrange("p (a b) -> p a b", a=4), [pt], [xgT])

            load_xg(0)
            load_xg(1)
            load_weights(0)
            transpose_groups(0, 4 * NSB)
            for e in range(NE):
                load_xg(e + 2)
                load_weights(e + 1)
                wg, wu, wd = W.pop(e)
                xgT = XT.pop(e)
                hid = hid_r.next()
                for dc in range(4):
                    pg, pu = ps_g.next(), ps_u.next()
                    for kc in range(DC):
                        c.mm(pg[:, :], wg[:, kc, dc:DFF:4], xgT[:, kc, :], kc == 0, kc == DC - 1,
                             [wg, xgT], [pg])
                    for kc in range(DC):
                        c.mm(pu[:, :], wu[:, kc, dc:DFF:4], xgT[:, kc, :], kc == 0, kc == DC - 1,
                             [wu, xgT], [pu])
                    transpose_groups(e + 1, 2)
                    sg = sg_r.next()
                    c.act(sg[:, :], pg[:, :], AF.Silu, [pg], [sg])
                    c.tt("dve", hid[:, dc, :], sg[:, :], pu[:, :], ALU.mult, [sg, pu], [hid])
                for sb in range(NSB):
                    yt = y_r.next()
                    for nb in range(4):
                        ns = slice(nb * 512, (nb + 1) * 512)
                        py = ps_y.next()
                        for dc in range(4):
                            c.mm(py[:, :], hid[:, dc, sb * 128:(sb + 1) * 128], wd[:, dc, ns], dc == 0, dc == 3,
                                 [hid, wd], [py])
                        c.copy("act" if nb % 2 == 0 else "dve", yt[:, ns], py[:, :], [py], [yt])
                    transpose_groups(e + 1, 2)
                    r0 = e * CAP + sb * 128
                    c.dma("sp", y_d[r0:r0 + 128, :], yt[:, :], [yt], [y_d])
                transpose_groups(e + 1, 4 * NSB)
            c.barrier()
        c.stack = outer
        if "y_s" in dump:
            final_outs.append(y_d.b)
        if stop_after == "F":
            stop_here()
            return nc

        with ExitStack() as sg_:
            c.stack = sg_
            gfin = c.sb("gfin", [128, D], F32)
            c.dma("sp", gfin[:, :], grow_d[1:2, :].partition_broadcast(128), [grow_d], [gfin])
            hin = c.sb_ring("hinG", 3, [128, D], F32)
            y1_r = c.sb_ring("y1", 3, [128, D], BF16)
            y2_r = c.sb_ring("y2", 3, [128, D], BF16)
            h3_r = c.sb_ring("h3", 3, [128, D], F32)
            o_r = c.sb_ring("og", 2, [128, D], F32)
            junk_r = c.sb_ring("junkG", 1, [128, D], BF16)
            ss_r = c.sb_ring("ssG", 4, [128, 4], F32)
            def g_phase1(t):
                ts_ = slice(t * 128, (t + 1) * 128)
                ht, y1, y2, h3 = hin.next(), y1_r.next(), y2_r.next(), h3_r.next()
                c.dma("sp", ht[:, :], h2_d[ts_, :], [h2_d], [ht])
                for k, yk in ((0, y1), (1, y2)):
                    c.memset("pool", yk[:, :], 0.0, [yk])
                    c.p.dma("pool", lambda h, yk=yk, t=t, k=k: h.indirect_dma_start(
                        out=yk[:, :], out_offset=None, in_=y_d[:, :],
                        in_offset=bass.IndirectOffsetOnAxis(ap=slot_i[:, t, k:k + 1], axis=0),
                        bounds_check=bc_reg, oob_is_err=False), bs([y_d, slot_i]), bs([yk]))
                c.stt(h3[:, :], y1[:, :], wts[:, t, 0:1], ht[:, :], ALU.mult, ALU.add, [y1, wts, ht], [h3])
                c.stt(h3[:, :], y2[:, :], wts[:, t, 1:2], h3[:, :], ALU.mult, ALU.add, [y2, wts, h3], [h3])
                ss, junk = ss_r.next(), junk_r.next()
                c.act(junk[:, :D], h3[:, :D], AF.Square, [h3], [junk, ss], accum=ss[:, 0:1])
                return h3, ss

            def g_phase2(t, h3, ss):
                ts_ = slice(t * 128, (t + 1) * 128)
                og = o_r.next()
                c.ts("dve", ss[:, 1:2], ss[:, 0:1], 1.0 / D, EPS, ALU.mult, ALU.add, [ss], [ss])
                c.p.op("act", lambda h: h.sqrt(ss[:, 3:4], ss[:, 1:2]), bs([ss]), bs([ss]))
                c.recip(ss[:, 2:3], ss[:, 3:4], [ss], [ss])
                c.stt(og[:, :], h3[:, :], ss[:, 2:3], gfin[:, :], ALU.mult, ALU.mult, [h3, ss, gfin], [og])
                c.dma("sp", out_d[ts_, :], og[:, :], [og], [out_d])

            cur = g_phase1(0)
            for t in range(NT):
                nxt = g_phase1(t + 1) if t + 1 < NT else None
                g_phase2(t, *cur)
                cur = nxt
            final_outs.append(out_d.b)
            finish()
        c.stack = outer
    return nc


def _fm(g):
    g = np.asarray(g, np.float32).reshape(-1)
    return np.ascontiguousarray(g.reshape(-1, 128).T)


def host_shared(inputs):
    f32 = np.float32
    bf = ml_dtypes.bfloat16
    m = {}
    for k in ("w_in", "w_o_diff", "w_uq", "w_ukv", "w_o_mla", "w_out", "w_cq", "w_ckv", "w_co",
              "w_expert_gate", "w_expert_up", "w_expert_down"):
        m[k] = np.ascontiguousarray(np.asarray(inputs[k], f32)[0])
    m["w_router"] = np.ascontiguousarray(np.concatenate(
        [np.asarray(inputs["w_router_group"], f32)[0], np.asarray(inputs["w_router_expert"], f32)[0]], axis=1))
    m["b_router"] = np.ascontiguousarray(np.concatenate(
        [np.asarray(inputs["b_router_group"], f32)[0], np.asarray(inputs["b_router_expert"], f32)[0]])[None, :])
    gT = np.concatenate([_fm(inputs["attn_norm_g"][0]), _fm(inputs["cross_norm_g"][0]), _fm(inputs["mem_norm_g"][0]),
                         _fm(inputs["mla_q_norm_g"][0]), _fm(inputs["mla_kv_norm_g"][0]),
                         _fm(inputs["diff_subln_g"][0]), np.zeros((128, 1), f32),
                         _fm(inputs["ffn_norm_g"][0])], axis=1)
    m["gT"] = np.ascontiguousarray(gT, f32)
    m["grow"] = np.ascontiguousarray(np.stack([np.asarray(inputs["ffn_norm_g"], f32)[0],
                                               np.asarray(inputs["final_norm_g"], f32)], axis=0))
    m["lam"] = np.ascontiguousarray(np.stack([np.asarray(inputs[k], f32)[0] for k in
                                              ("diff_lambda_q1", "diff_lambda_k1", "diff_lambda_q2", "diff_lambda_k2")]).reshape(1, 256))
    ident = np.eye(128, dtype=f32)
    ones = np.ones((128, 128), f32)
    Rd = np.zeros((128, 128), f32)
    for blk in range(2):
        for i in range(8):
            Rd[blk * 64 + i + 8, blk * 64 + i] = -1.0
            Rd[blk * 64 + i, blk * 64 + i + 8] = 1.0
    U = np.triu(np.ones((128, 128), f32), k=1)
    m["cb"] = np.ascontiguousarray(np.concatenate([ident, ones, Rd, U], axis=1).astype(bf))
    Rm = np.zeros((64, 64), f32)
    for i in range(32):
        Rm[i + 32, i] = -1.0
        Rm[i, i + 32] = 1.0
    m["rm"] = Rm.astype(bf)
    cf = np.zeros((128, 2 + NE), f32)
    for pp in range(128):
        i = pp % 64
        if i < 16:
            cf[pp, 0] = ROPE_THETA ** (-(i % 8) * 2.0 / 16.0) / (2 * math.pi)
        cf[pp, 1] = ROPE_THETA ** (-(i % 32) * 2.0 / 64.0) / (2 * math.pi)
    cf[:, 2:] = (np.arange(NE, dtype=f32) * CAP)[None, :]
    m["cf"] = cf
    return m


_NC_CACHE = {}
_DEV_CORES = 0


def kernel(**inputs):
    return _run(inputs)


def _run(inputs, stop_after=None, dump=()):
    inputs = {k: np.asarray(v) for k, v in inputs.items()}
    key = (stop_after, tuple(dump))
    if key not in _NC_CACHE:
        _NC_CACHE[key] = build_program(stop_after=stop_after, dump=dump)
    nc = _NC_CACHE[key]
    shared = host_shared(inputs)
    in_maps = []
    ncores = _DEV_CORES or NCORES
    for b in range(ncores):
        m = dict(shared)
        m["x"] = np.ascontiguousarray(inputs["x"][b], np.float32)
        m["mem"] = np.ascontiguousarray(inputs["mem"][b], np.float32)
        m["pos"] = np.ascontiguousarray(inputs["positions"][b], np.int32)[None, :]
        in_maps.append(m)
    res = run_bass_kernel_spmd(nc, in_maps, core_ids=list(range(ncores)))
    out = np.stack([np.asarray(r["out"]) for r in res.results], axis=0).astype(np.float32)
    if dump:
        return out, [{k: np.asarray(r[k]) for k in dump} for r in res.results]
    return out
```

```python
import math
from contextlib import ExitStack

import numpy as np
import ml_dtypes

import concourse.bass as bass
import concourse.mybir as mybir
from concourse.bass_utils import run_bass_kernel_spmd

F32 = mybir.dt.float32
BF16 = mybir.dt.bfloat16
I32 = mybir.dt.int32
ALU = mybir.AluOpType
AF = mybir.ActivationFunctionType
AX = mybir.AxisListType

NCORES = 8
S = 2048
D = 2048
NT = S // 128
DC = D // 128
D_IN = 8000
EPS = 1e-6


class Buf:
    __slots__ = ("name", "last_w", "readers")

    def __init__(self, name):
        self.name = name
        self.last_w = None
        self.readers = {}


class Prog:
    ENGINES = ("pe", "act", "dve", "pool", "sp")

    def __init__(self, nc, stack, n_lanes=12):
        self.nc = nc
        self.handles = {"pe": nc.tensor, "act": nc.scalar, "dve": nc.vector,
                        "pool": nc.gpsimd, "sp": nc.sync}
        self.thunks = {e: [] for e in self.ENGINES}
        self.sems = {}
        self.count = {}
        for e in self.ENGINES:
            self.sems[e] = stack.enter_context(nc.semaphore("c_" + e))
            self.count[e] = 0
        self.lanes = {}
        for q in ("sp", "pool"):
            ls = []
            for i in range(n_lanes):
                key = "l_%s%d" % (q, i)
                self.sems[key] = stack.enter_context(nc.semaphore(key))
                self.count[key] = 0
                ls.append(key)
            self.lanes[q] = ls
        self.lane_rr = {"sp": 0, "pool": 0}
        self.known = {e: {} for e in self.ENGINES}
        self.n_waits = 0
        self.events = {e: [] for e in self.ENGINES}

    def _collect(self, eng, reads, writes):
        need = {}

        def add(tok, same_ok):
            if tok is None:
                return
            k, v = tok
            if k == eng and same_ok:
                return
            if need.get(k, 0) < v:
                need[k] = v

        for b in reads:
            add(b.last_w, same_ok=(eng == "pe"))
        for b in writes:
            add(b.last_w, same_ok=True)
            for k, v in b.readers.items():
                add((k, v), same_ok=True)
        out = []
        kn = self.known[eng]
        for k, v in need.items():
            if kn.get(k, 0) >= v:
                continue
            kn[k] = v
            out.append((k, v))
        return out

    def _emit_waits(self, eng, waits):
        h = self.handles[eng]
        sems = self.sems
        for k, v in waits:
            self.n_waits += 1
            self.events[eng].append(("w", k, v))
            self.thunks[eng].append(lambda h=h, s=sems[k], v=v: h.wait_ge(s, v))

    def _update(self, tok, reads, writes):
        k, v = tok
        for b in reads:
            if b.readers.get(k, 0) < v:
                b.readers[k] = v
        for b in writes:
            b.last_w = tok
            b.readers = {}

    def op(self, eng, fn, reads=(), writes=(), sig=True):
        waits = self._collect(eng, reads, writes)
        self._emit_waits(eng, waits)
        h = self.handles[eng]
        sem = self.sems[eng]
        if sig:
            self.count[eng] += 1
            tok = (eng, self.count[eng])
            self.events[eng].append(("s", eng, 1))
            self.thunks[eng].append(lambda: fn(h).then_inc(sem, 1))
        else:
            tok = (eng, self.count[eng] + 1)
            self.thunks[eng].append(lambda: fn(h))
        self._update(tok, reads, writes)
        return tok

    def dma(self, q, fn, reads=(), writes=()):
        lanes = self.lanes[q]
        lane = lanes[self.lane_rr[q] % len(lanes)]
        self.lane_rr[q] += 1
        waits = self._collect(q, reads, writes)
        prev = self.count[lane]
        if prev > 0 and self.known[q].get(lane, 0) < prev:
            self.known[q][lane] = prev
            waits.append((lane, prev))
        self._emit_waits(q, waits)
        h = self.handles[q]
        sem = self.sems[lane]
        self.count[lane] += 16
        tok = (lane, self.count[lane])
        self.events[q].append(("s", lane, 16))
        self.thunks[q].append(lambda: fn(h).then_inc(sem, 16))
        self._update(tok, reads, writes)
        return tok

    def wait_all(self, eng, bufs):
        waits = self._collect(eng, bufs, ())
        self._emit_waits(eng, waits)

    def check_deadlock(self):
        val = {}
        pos = {e: 0 for e in self.ENGINES}
        progress = True
        while progress:
            progress = False
            for e in self.ENGINES:
                ev = self.events[e]
                i = pos[e]
                while i < len(ev):
                    kind, k, v = ev[i]
                    if kind == "w":
                        if val.get(k, 0) < v:
                            break
                    else:
                        val[k] = val.get(k, 0) + v
                    i += 1
                if i != pos[e]:
                    progress = True
                    pos[e] = i
        stuck = {e: (pos[e], len(self.events[e]), self.events[e][pos[e]]) for e in self.ENGINES
                 if pos[e] < len(self.events[e])}
        if stuck:
            raise RuntimeError("build-time deadlock check failed: %r" % (stuck,))

    def emit(self):
        self.check_deadlock()
        nc = self.nc
        with nc.Block() as block:
            @block.tensor
            def _(e):
                for t in self.thunks["pe"]:
                    t()

            @block.scalar
            def _(e):
                for t in self.thunks["act"]:
                    t()

            @block.vector
            def _(e):
                for t in self.thunks["dve"]:
                    t()

            @block.gpsimd
            def _(e):
                for t in self.thunks["pool"]:
                    t()

            @block.sync
            def _(e):
                for t in self.thunks["sp"]:
                    t()


class Ring:
    def __init__(self, tiles):
        self.tiles = tiles
        self.i = 0

    def next(self):
        t = self.tiles[self.i % len(self.tiles)]
        self.i += 1
        return t


class Tile:
    __slots__ = ("t", "b")

    def __init__(self, t, name):
        self.t = t
        self.b = Buf(name)

    def __getitem__(self, k):
        return self.t[k]


class Ctx:
    def __init__(self, nc, stack):
        self.nc = nc
        self.stack = stack
        self.p = Prog(nc, stack)
        self._n = 0

    def sb(self, name, shape, dt):
        self._n += 1
        return Tile(self.stack.enter_context(self.nc.sbuf_tensor("s%d_%s" % (self._n, name), list(shape), dt)), name)

    def ps(self, name, shape, dt=F32):
        self._n += 1
        return Tile(self.stack.enter_context(self.nc.psum_tensor("p%d_%s" % (self._n, name), list(shape), dt)), name)

    def dram(self, name, shape, dt, kind="Internal"):
        return Tile(self.nc.dram_tensor(name, list(shape), dt, kind=kind), name)

    def sb_ring(self, name, n, shape, dt):
        return Ring([self.sb("%s%d" % (name, i), shape, dt) for i in range(n)])

    def ps_ring(self, name, n, shape, dt=F32):
        return Ring([self.ps("%s%d" % (name, i), shape, dt) for i in range(n)])


H = 8
ROPE_THETA = 500000.0
CAP = 384
NE = 32
NSLOT = NE * CAP
DFF = 512
LAM_INIT = 0.8 - 0.6 * math.exp(-0.3 * 0)
PI = math.pi
NO_SCATTER = False


def bs(tiles):
    return [t.b for t in tiles]


class K(Ctx):
    def mm(self, out, lhsT, rhs, start, stop, R, W, sig=None):
        if sig is None:
            sig = stop
        self.p.op("pe", lambda h: h.matmul(out, lhsT, rhs, start=start, stop=stop), bs(R), bs(W), sig=sig)

    def tr(self, out, in_, ident, R, W, sig=True):
        self.p.op("pe", lambda h: h.transpose(out, in_, ident), bs(R), bs(W), sig=sig)

    def act(self, out, in_, func, R, W, bias=None, scale=None, accum=None):
        kw = {}
        if bias is not None:
            kw["bias"] = bias
        if scale is not None:
            kw["scale"] = scale
        if accum is not None:
            kw["accum_out"] = accum
        self.p.op("act", lambda h: h.activation(out, in_, func, **kw), bs(R), bs(W))

    def ts(self, eng, out, in0, s1, s2, op0, op1, R, W):
        if op1 is None:
            self.p.op(eng, lambda h: h.tensor_scalar(out, in0, s1, None, op0), bs(R), bs(W))
        else:
            self.p.op(eng, lambda h: h.tensor_scalar(out, in0, s1, s2, op0, op1), bs(R), bs(W))

    def tt(self, eng, out, in0, in1, op, R, W):
        self.p.op(eng, lambda h: h.tensor_tensor(out, in0, in1, op), bs(R), bs(W))

    def stt(self, out, in0, scalar, in1, op0, op1, R, W, accum=None):
        if accum is None:
            self.p.op("dve", lambda h: h.scalar_tensor_tensor(out, in0, scalar, in1, op0, op1), bs(R), bs(W))
        else:
            self.p.op("dve", lambda h: h.scalar_tensor_tensor(out, in0, scalar, in1, op0, op1, accum_out=accum),
                      bs(R), bs(W))

    def copy(self, eng, out, in_, R, W):
        if eng == "act":
            self.p.op("act", lambda h: h.copy(out, in_), bs(R), bs(W))
        else:
            self.p.op(eng, lambda h: h.tensor_copy(out, in_), bs(R), bs(W))

    def recip(self, out, in_, R, W):
        self.p.op("dve", lambda h: h.reciprocal(out, in_), bs(R), bs(W))

    def memset(self, eng, out, val, W):
        self.p.op(eng, lambda h: h.memset(out, val), [], bs(W))

    def dma(self, q, out, in_, R, W):
        self.p.dma(q, lambda h: h.dma_start(out=out, in_=in_), bs(R), bs(W))

    def barrier(self):
        p = self.p
        toks = [(e, p.count[e]) for e in p.ENGINES if p.count[e] > 0]
        for q in ("sp", "pool"):
            for lane in p.lanes[q]:
                if p.count[lane] > 0:
                    toks.append((lane, p.count[lane]))
        for e in p.ENGINES:
            w = []
            for k, v in toks:
                if k == e:
                    continue
                if p.known[e].get(k, 0) < v:
                    p.known[e][k] = v
                    w.append((k, v))
            p._emit_waits(e, w)


def rmsnorm_rstd(c, x_t, d, ss, junk, eps):
    c.act(junk[:, :d], x_t[:, :d], AF.Square, [x_t], [junk, ss], accum=ss[:, 0:1])
    c.ts("dve", ss[:, 1:2], ss[:, 0:1], 1.0 / d, eps, ALU.mult, ALU.add, [ss], [ss])
    c.p.op("act", lambda h: h.sqrt(ss[:, 3:4], ss[:, 1:2]), bs([ss]), bs([ss]))
    c.recip(ss[:, 2:3], ss[:, 3:4], [ss], [ss])


def norm_phase1(c, x_t, rg):
    junk = rg["junk"].next()
    ss = rg["ss"].next()
    c.act(junk[:, :D], x_t[:, :D], AF.Square, [x_t], [junk, ss], accum=ss[:, 0:1])
    return ss


def norm_phase2(c, x_t, ss, gT, goff, dstT, tcol, rg, ident):
    xs = rg["xs"].next()
    c.ts("dve", ss[:, 1:2], ss[:, 0:1], 1.0 / D, EPS, ALU.mult, ALU.add, [ss], [ss])
    c.p.op("act", lambda h: h.sqrt(ss[:, 3:4], ss[:, 1:2]), bs([ss]), bs([ss]))
    c.recip(ss[:, 2:3], ss[:, 3:4], [ss], [ss])
    half = D // 2
    c.ts("dve", xs[:, :half], x_t[:, :half], ss[:, 2:3], None, ALU.mult, None, [x_t, ss], [xs])
    c.act(xs[:, half:], x_t[:, half:], AF.Copy, [x_t, ss], [xs], scale=ss[:, 2:3])
    for g4 in range(DC // 4):
        pt = rg["pst"].next()
        for j in range(4):
            ch = g4 * 4 + j
            c.tr(pt[:, j * 128:(j + 1) * 128], xs[:, ch * 128:(ch + 1) * 128], ident[:, :], [xs, ident], [pt],
                 sig=(j == 3))
        for j in range(4):
            ch = g4 * 4 + j
            if j % 2 == 0:
                c.ts("dve", dstT[:, ch, tcol:tcol + 128], pt[:, j * 128:(j + 1) * 128],
                     gT[:, goff + ch:goff + ch + 1], None, ALU.mult, None, [pt, gT], [dstT])
            else:
                c.act(dstT[:, ch, tcol:tcol + 128], pt[:, j * 128:(j + 1) * 128], AF.Copy, [pt, gT], [dstT],
                      scale=gT[:, goff + ch:goff + ch + 1])


def norm_transpose_stream(c, n_tiles, load_fn, gT, goff, dstT, rg, ident, dst_fn=None, after_fn=None):
    cur = load_fn(0)
    ss = norm_phase1(c, cur, rg)
    for t in range(n_tiles):
        nxt = nss = None
        if t + 1 < n_tiles:
            nxt = load_fn(t + 1)
            nss = norm_phase1(c, nxt, rg)
        if dst_fn is None:
            norm_phase2(c, cur, ss, gT, goff, dstT, t * 128, rg, ident)
        else:
            dt_, tcol = dst_fn(t)
            norm_phase2(c, cur, ss, gT, goff, dt_, tcol, rg, ident)
        if after_fn is not None:
            after_fn(t)
        cur, ss = nxt, nss


def norm_transpose_tile(c, x_t, gT, goff, dstT, tcol, rg, ident):
    ss = norm_phase1(c, x_t, rg)
    norm_phase2(c, x_t, ss, gT, goff, dstT, tcol, rg, ident)


def rope_tables(c, pos_d, invf, col, nparts, Ct, St, tmp_i, tmp_f):
    n = nparts
    c.dma("sp", tmp_i[:n, :], pos_d[0:1, :].partition_broadcast(n), [pos_d], [tmp_i])
    c.copy("dve", tmp_f[:n, :], tmp_i[:n, :], [tmp_i], [tmp_f])
    for dst, phase in ((St, 0.0), (Ct, 0.25)):
        c.ts("dve", dst[:n, :], tmp_f[:n, :], invf[:n, col:col + 1], phase, ALU.mult, ALU.add, [tmp_f, invf], [dst])
        c.copy("dve", tmp_i[:n, :], dst[:n, :], [dst], [tmp_i])
        c.copy("dve", Ct[:n, :] if dst is St else tmp_f[:n, :], tmp_i[:n, :], [tmp_i], [Ct if dst is St else tmp_f])
        kf = Ct if dst is St else tmp_f
        c.tt("dve", dst[:n, :], dst[:n, :], kf[:n, :], ALU.subtract, [dst, kf], [dst])
        for _ in range(2):
            c.stt(dst[:n, :], dst[:n, :], 0.5, dst[:n, :], ALU.is_gt, ALU.subtract, [dst], [dst])
        c.act(dst[:n, :], dst[:n, :], AF.Sin, [dst], [dst], scale=2 * PI)


def rope_evac(c, ps, npart, blk, Rm, Ct, St, rg, dst_ap, dst_tile, defer=None):
    qs = rg["qs"].next()
    c.copy("act", qs[:npart, :], ps[:npart, :], [ps], [qs])
    if defer is not None:
        defer.append(lambda: _rope_rest(c, qs, npart, blk, Rm, Ct, St, rg, dst_ap, dst_tile))
        return
    _rope_rest(c, qs, npart, blk, Rm, Ct, St, rg, dst_ap, dst_tile)


def _rope_rest(c, qs, npart, blk, Rm, Ct, St, rg, dst_ap, dst_tile):
    pr = rg["psr"].next()
    c.mm(pr[:npart, :], Rm[:npart, :npart], qs[:npart, :], True, True, [Rm, qs], [pr])
    t1 = rg["t1"].next()
    t2 = rg["t2"].next()
    cs = slice(blk * 512, (blk + 1) * 512)
    c.tt("dve", t1[:npart, :], qs[:npart, :], Ct[:npart, cs], ALU.mult, [qs, Ct], [t1])
    c.tt("dve", t2[:npart, :], pr[:npart, :], St[:npart, cs], ALU.mult, [pr, St], [t2])
    c.tt("pool", dst_ap, t1[:npart, :], t2[:npart, :], ALU.add, [t1, t2], [dst_tile])


def build_program(stop_after=None, dump=()):
    nc = bass.Bass("TRN2", target_bir_lowering=False)
    outer = ExitStack()
    with outer:
        c = K(nc, outer)
        p = c.p

        def dram(name, shape, dt, kind=None):
            if kind is None:
                kind = "ExternalOutput" if name in dump else "Internal"
            return c.dram(name, shape, dt, kind=kind)

        x_d = dram("x", [S, D], F32, "ExternalInput")
        mem_d = dram("mem", [256, D], F32, "ExternalInput")
        pos_d = dram("pos", [1, S], I32, "ExternalInput")
        w_in_d = dram("w_in", [D, D_IN], F32, "ExternalInput")
        w_od_d = dram("w_o_diff", [1024, D], F32, "ExternalInput")
        w_uq_d = dram("w_uq", [512, 1536], F32, "ExternalInput")
        w_ukv_d = dram("w_ukv", [256, 2048], F32, "ExternalInput")
        w_om_d = dram("w_o_mla", [1024, D], F32, "ExternalInput")
        w_out_d = dram("w_out", [D, D], F32, "ExternalInput")
        w_cq_d = dram("w_cq", [D, 512], F32, "ExternalInput")
        w_ckv_d = dram("w_ckv", [D, 1024], F32, "ExternalInput")
        w_co_d = dram("w_co", [512, D], F32, "ExternalInput")
        w_r_d = dram("w_router", [D, 36], F32, "ExternalInput")
        b_r_d = dram("b_router", [1, 36], F32, "ExternalInput")
        w_eg_d = dram("w_expert_gate", [NE, D, DFF], F32, "ExternalInput")
        w_eu_d = dram("w_expert_up", [NE, D, DFF], F32, "ExternalInput")
        w_ed_d = dram("w_expert_down", [NE, DFF, D], F32, "ExternalInput")
        gT_d = dram("gT", [128, 4 * DC + 8], F32, "ExternalInput")
        grow_d = dram("grow", [2, D], F32, "ExternalInput")
        lam_d = dram("lam", [1, 256], F32, "ExternalInput")
        cb_d = dram("cb", [128, 4 * 128], BF16, "ExternalInput")
        rm_d = dram("rm", [64, 64], BF16, "ExternalInput")
        cf_d = dram("cf", [128, 2 + NE], F32, "ExternalInput")
        out_d = dram("out", [S, D], F32, "ExternalOutput")

        qT_d = dram("qT_s", [H, 128, S], BF16)
        kT_d = dram("kT_s", [H, 128, S], BF16)
        v_d = dram("v_s", [S, 1024], BF16)
        cq_d = dram("cq_s", [4, 128, S], F32)
        ckv_d = dram("ckv_s", [2, 128, S], F32)
        kpe_d = dram("kpe_s", [64, S], BF16)
        ga_d = dram("ga_s", [DC, 128, S], BF16)
        gb_d = dram("gb_s", [DC, 128, S], BF16)
        oa_d = dram("oa_s", [H, 128, S], BF16)
        ob_d = dram("ob_s", [H, 128, S], BF16)
        h1_d = dram("h1_s", [S, D], F32)
        h2_d = dram("h2_s", [S, D], F32)
        xg_d = dram("xg_s", [NSLOT + 128, D], BF16)
        y_d = dram("y_s", [NSLOT + 128, D], BF16)

        cb = c.sb("cb", [128, 4 * 128], BF16)
        rm = c.sb("rm", [64, 64], BF16)
        cf = c.sb("cf", [128, 2 + NE], F32)
        gT = c.sb("gTs", [128, 4 * DC + 8], F32)
        c.dma("sp", cb[:, :], cb_d[:, :], [cb_d], [cb])
        c.dma("sp", rm[:, :], rm_d[:, :], [rm_d], [rm])
        c.dma("sp", cf[:, :], cf_d[:, :], [cf_d], [cf])
        c.dma("sp", gT[:, :], gT_d[:, :], [gT_d], [gT])
        ident = Tile(cb.t[:, 0:128], "ident"); ident.b = cb.b
        ones = Tile(cb.t[:, 128:256], "ones"); ones.b = cb.b
        Rd = Tile(cb.t[:, 256:384], "Rd"); Rd.b = cb.b
        Utri = Tile(cb.t[:, 384:512], "Utri"); Utri.b = cb.b
        slot_i = c.sb("slot_i", [128, NT, 2], I32)
        wts = c.sb("wts", [128, NT, 2], F32)
        zrow = c.sb("zrow", [128, D], BF16)
        c.memset("pool", zrow[:, :], 0.0, [zrow])

        final_outs = []
        bc_reg = nc.gpsimd.alloc_register("bc_reg")
        p.thunks["pool"].insert(0, lambda: nc.gpsimd.reg_mov(bc_reg, NSLOT + 127))

        def finish():
            p.wait_all("sp", [b for b in final_outs])
            p.emit()

        with ExitStack() as sa:
            c.stack = sa
            xnT = c.sb("xnT", [128, DC, S], BF16)
            with ExitStack() as sa1:
                c.stack = sa1
                rg = {"junk": c.sb_ring("junk", 1, [128, D], BF16), "ss": c.sb_ring("ss", 4, [128, 4], F32),
                      "xs": c.sb_ring("xs", 2, [128, D], BF16), "pst": c.ps_ring("pst", 2, [128, 512], BF16)}
                xin = c.sb_ring("xin", 3, [128, D], F32)

                def load_x(t):
                    xt = xin.next()
                    c.dma("sp", xt[:, :], x_d[t * 128:(t + 1) * 128, :], [x_d], [xt])
                    return xt

                norm_transpose_stream(c, NT, load_x, gT, 0, xnT, rg, ident)
                c.barrier()
            c.stack = sa
            if "xnT_dbg" in dump:
                dbg = dram("xnT_dbg", [128, DC, S], BF16)
                c.dma("sp", dbg[:, :, :], xnT[:, :, :], [xnT], [dbg])
                final_outs.append(dbg.b)
            Cd = c.sb("Cd", [128, S], F32)
            Sd = c.sb("Sd", [128, S], F32)
            Cm = c.sb("Cm", [64, S], F32)
            Sm = c.sb("Sm", [64, S], F32)
            with ExitStack() as sa2:
                c.stack = sa2
                tmp_i = c.sb("tmp_i", [128, S], I32)
                tmp_f = c.sb("tmp_f", [128, S], F32)
                rope_tables(c, pos_d, cf, 0, 128, Cd, Sd, tmp_i, tmp_f)
                rope_tables(c, pos_d, cf, 1, 64, Cm, Sm, tmp_i, tmp_f)
                c.barrier()
            c.stack = sa
            slabs = c.sb_ring("wslab", 3, [128, DC, 512], BF16)
            rg = {"qs": c.sb_ring("qs", 3, [128, 512], BF16), "psr": c.ps_ring("psr", 2, [128, 512], F32),
                  "t1": c.sb_ring("t1", 2, [128, 512], F32), "t2": c.sb_ring("t2", 2, [128, 512], F32)}
            psA = c.ps_ring("psA", 4, [128, 512], F32)
            stg = c.sb_ring("stgA", 4, [128, 512], BF16)
            stgf = c.sb_ring("stgAf", 3, [128, 512], F32)

            def load_slab(c0, ncols):
                sl = slabs.next()
                c.dma("pool", sl[:, :, :ncols], w_in_d[:, c0:c0 + ncols].rearrange("(kc p) n -> p kc n", p=128),
                      [w_in_d], [sl])
                return sl

            slab_specs = ([(2048, 512), (2560, 512), (0, 512), (512, 512), (1024, 512), (1536, 512), (3072, 512), (3584, 320)]
                          + [(3904 + w_ * 2048 + s_ * 512, 512) for w_ in range(2) for s_ in range(4)])
            slab_q = []
            slab_i = {"i": 0}

            def issue_slab():
                if slab_i["i"] < len(slab_specs):
                    c0_, n_ = slab_specs[slab_i["i"]]
                    slab_i["i"] += 1
                    slab_q.append(((c0_, n_), load_slab(c0_, n_)))

            def get_slab(c0, ncols):
                if not slab_q:
                    issue_slab()
                spec, sl_ = slab_q.pop(0)
                assert spec == (c0, ncols), (spec, c0, ncols)
                issue_slab()
                return sl_

            def proj_fm(sl, off, width, blk):
                ps = psA.next()
                for kc in range(DC):
                    c.mm(ps[:width, :], sl[:, kc, off:off + width], xnT[:, kc, blk * 512:(blk + 1) * 512],
                         kc == 0, kc == DC - 1, [sl, xnT], [ps])
                return ps

            for sidx in range(2):
                sl = get_slab(2048 + sidx * 512, 512)
                for t in range(NT):
                    ps = psA.next()
                    for kc in range(DC):
                        c.mm(ps[:, :], xnT[:, kc, t * 128:(t + 1) * 128], sl[:, kc, :], kc == 0, kc == DC - 1,
                             [sl, xnT], [ps])
                    st = stg.next()
                    c.copy("act" if t % 2 == 0 else "dve", st[:, :], ps[:, :], [ps], [st])
                    c.dma("sp", v_d[t * 128:(t + 1) * 128, sidx * 512:(sidx + 1) * 512], st[:, :], [st], [v_d])
            pend = []

            def flush_pending():
                while pend:
                    pend.pop(0)()

            for which, dst_d in ((0, qT_d), (1, kT_d)):
                for sidx in range(2):
                    sl = get_slab(which * 1024 + sidx * 512, 512)
                    for j in range(4):
                        hh = sidx * 4 + j
                        for blk in range(4):
                            ps = proj_fm(sl, j * 128, 128, blk)
                            flush_pending()
                            st = stg.next()
                            todo = []
                            rope_evac(c, ps, 128, blk, Rd, Cd, Sd, rg, st[:, :], st, defer=todo)

                            def tail(todo=todo, st=st, dst_d=dst_d, hh=hh, blk=blk):
                                todo[0]()
                                c.dma("sp", dst_d[hh, :, blk * 512:(blk + 1) * 512], st[:, :], [st], [dst_d])
                            pend.append(tail)
            flush_pending()
            sl = get_slab(3072, 512)
            for j in range(4):
                for blk in range(4):
                    ps = proj_fm(sl, j * 128, 128, blk)
                    st = stgf.next()
                    c.copy("act" if blk % 2 == 0 else "dve", st[:, :], ps[:, :], [ps], [st])
                    c.dma("sp", cq_d[j, :, blk * 512:(blk + 1) * 512], st[:, :], [st], [cq_d])
            sl = get_slab(3584, 320)
            for j in range(2):
                for blk in range(4):
                    ps = proj_fm(sl, j * 128, 128, blk)
                    st = stgf.next()
                    c.copy("act" if blk % 2 == 0 else "dve", st[:, :], ps[:, :], [ps], [st])
                    c.dma("sp", ckv_d[j, :, blk * 512:(blk + 1) * 512], st[:, :], [st], [ckv_d])
            for blk in range(4):
                ps = proj_fm(sl, 256, 64, blk)
                st = stg.next()
                rope_evac(c, ps, 64, blk, rm, Cm, Sm, rg, st[:64, :], st)
                c.dma("sp", kpe_d[:, blk * 512:(blk + 1) * 512], st[:64, :], [st], [kpe_d])
            for which, dst_d in ((0, ga_d), (1, gb_d)):
                for sidx in range(4):
                    sl = get_slab(3904 + which * 2048 + sidx * 512, 512)
                    for j in range(4):
                        ch = sidx * 4 + j
                        for blk in range(4):
                            ps = proj_fm(sl, j * 128, 128, blk)
                            st = stg.next()
                            c.act(st[:, :], ps[:, :], AF.Sigmoid, [ps], [st])
                            c.dma("sp", dst_d[ch, :, blk * 512:(blk + 1) * 512], st[:, :], [st], [dst_d])
            c.barrier()
        c.stack = outer
        for nm, tl in (("qT_s", qT_d), ("kT_s", kT_d), ("v_s", v_d), ("cq_s", cq_d), ("ckv_s", ckv_d),
                       ("kpe_s", kpe_d), ("ga_s", ga_d), ("gb_s", gb_d)):
            if nm in dump:
                final_outs.append(tl.b)
        if stop_after == "A":
            final_outs.append(out_d.b)
            c.dma("sp", out_d[0:128, :], x_d[0:128, :], [x_d], [out_d])
            finish()
            return nc

        def stop_here():
            final_outs.append(out_d.b)
            c.dma("sp", out_d[0:128, :], x_d[0:128, :], [x_d], [out_d])
            finish()

        def attn_core(nk_tiles, qblk, score_fn, v_fn, ps_s, e_ring, acc_o, acc_d, scale):
            Es = {}

            def score(kt):
                ps = ps_s.next()
                score_fn(ps, kt)
                E = e_ring.next()
                c.act(E[:, :], ps[:, :], AF.Exp, [ps], [E], scale=scale)
                Es[kt] = E

            score(0)
            for kt in range(nk_tiles):
                if kt + 1 < nk_tiles:
                    score(kt + 1)
                E = Es.pop(kt)
                vap, vt = v_fn(kt)
                last = kt == nk_tiles - 1
                c.mm(acc_o[:, :], vap, E[:, :], kt == 0, last, [vt, E], [acc_o], sig=last)
                c.mm(acc_d[:, :], ones[:, :], E[:, :], kt == 0, last, [ones, E], [acc_d], sig=last)

        def attn_stream(calls, ps_s, e_ring, ps_o, ps_d, scale, look):
            flat = [(ci, kt) for ci, cl in enumerate(calls) for kt in range(0, cl["nk"], 2)]
            Es = {}
            state = {"nxt": 0}

            def score(i):
                ci, kt = flat[i]
                cl = calls[ci]
                if kt == 0 and cl.get("pre_fn") is not None:
                    cl["pre_fn"]()
                ps = ps_s.next()
                cl["score_fn"](ps, 0, kt)
                cl["score_fn"](ps, 512, kt + 1)
                E = e_ring.next()
                c.act(E[:, :], ps[:, :], AF.Exp, [ps], [E], scale=scale)
                Es[i] = E

            def fill():
                if state["nxt"] < len(flat):
                    score(state["nxt"])
                    state["nxt"] += 1

            deferred = []
            for _ in range(look):
                fill()
            for i, (ci, kt) in enumerate(flat):
                fill()
                for dfr in list(deferred):
                    dfr[0] -= 1
                    if dfr[0] <= 0:
                        deferred.remove(dfr)
                        dfr[1]()
                cl = calls[ci]
                if kt == 0:
                    cl["acc_o"] = ps_o.next()
                    cl["acc_d"] = cl["acc_d_tile"] if cl.get("acc_d_tile") is not None else ps_d.next()
                acc_o, acc_d = cl["acc_o"], cl["acc_d"]
                E = Es.pop(i)
                for j in range(2):
                    vap, vt = cl["v_fn"](kt + j)
                    first = (kt + j == 0)
                    last = (kt + j == cl["nk"] - 1)
                    c.mm(acc_o[:, :], vap, E[:, j * 512:(j + 1) * 512], first, last, [vt, E], [acc_o], sig=last)
                    c.mm(acc_d[:, :], ones[:, :], E[:, j * 512:(j + 1) * 512], first, last, [ones, E], [acc_d], sig=last)
                if kt + 2 >= cl["nk"]:
                    tail = cl["post_fn"](acc_o, acc_d)
                    if tail is not None:
                        deferred.append([3, tail])
            for dfr in deferred:
                dfr[1]()

        with ExitStack() as sb_:
            c.stack = sb_
            lamt = c.sb("lamt", [128, 256], F32)
            lams = c.sb("lams", [128, 8], F32)
            ljunk = c.sb("ljunk", [128, 64], F32)
            c.dma("sp", lamt[:, :], lam_d[0:1, :].partition_broadcast(128), [lam_d], [lamt])
            for i in range(2):
                c.tt("dve", ljunk[:, :], lamt[:, i * 128:i * 128 + 64], lamt[:, i * 128 + 64:i * 128 + 128], ALU.mult,
                     [lamt], [ljunk])
                c.p.op("dve", lambda h, i=i: h.reduce_sum(lams[:, i:i + 1], ljunk[:, :], axis=AX.X), bs([ljunk]), bs([lams]))
                c.act(lams[:, 2 + i:3 + i], lams[:, i:i + 1], AF.Exp, [lams], [lams])
            c.tt("dve", lams[:, 4:5], lams[:, 3:4], lams[:, 2:3], ALU.subtract, [lams], [lams])
            c.ts("dve", lams[:, 5:6], lams[:, 4:5], -LAM_INIT, None, ALU.add, None, [lams], [lams])
            neglam = lams[:, 5:6]

            zf = {"i": 0}

            def zero_fill(n):
                for _ in range(n):
                    i = zf["i"]
                    if i < NSLOT // 128:
                        c.dma("sp", xg_d[i * 128:(i + 1) * 128, :], zrow[:, :], [zrow], [xg_d])
                    elif i == NSLOT // 128:
                        c.dma("sp", y_d[NSLOT:NSLOT + 128, :], zrow[:, :], [zrow], [y_d])
                    zf["i"] = i + 1

            qh_r = c.sb_ring("qh", 2, [128, S], BF16)
            kh_r = c.sb_ring("kh", 2, [128, 2, S], BF16)
            for _kt in kh_r.tiles:
                c.memset("pool", _kt[:, :, :], 0.0, [_kt])
            vh_r = c.sb_ring("vh", 2, [128, NT, 128], BF16)
            e_ring = c.sb_ring("E", 4, [128, 1024], BF16)
            ps_s = c.ps_ring("ps_s", 2, [128, 1024], F32)
            ps_o = c.ps_ring("ps_o", 2, [128, 512], F32)
            ps_d = c.ps_ring("ps_d", 2, [128, 512], F32)
            rec_r = c.sb_ring("rec", 3, [128, 512], F32)
            oc_r = c.sb_ring("oc", 6, [128, 512], F32)
            sq_r = c.sb_ring("sq", 3, [128, 512], BF16)
            ost_r = c.sb_ring("ost", 3, [128, 512], BF16)
            heads = {}

            def load_head(hh):
                if hh >= H or hh in heads:
                    return
                qh, kh, vh = qh_r.next(), kh_r.next(), vh_r.next()
                c.dma("sp", qh[:, :], qT_d[hh, :, :], [qT_d], [qh])
                c.dma("sp", kh[0:64, 0, :], kT_d[hh, 0:64, :], [kT_d], [kh])
                c.dma("sp", kh[64:128, 1, :], kT_d[hh, 64:128, :], [kT_d], [kh])
                c.dma("sp", vh[:, :, :], v_d[:, hh * 128:(hh + 1) * 128].rearrange("(kt p) d -> p kt d", p=128),
                      [v_d], [vh])
                heads[hh] = (qh, kh, vh)

            calls = []
            for hh in range(H):
                for qb in range(4):
                    qs_ = slice(qb * 512, (qb + 1) * 512)
                    pair = {}
                    for comp in range(2):
                        r0 = comp * 64

                        def pre_fn(hh=hh, qb=qb, comp=comp):
                            if comp == 0 and qb == 0:
                                load_head(hh)
                            if comp == 0 and qb == 1:
                                load_head(hh + 1)
                            if comp == 0:
                                zero_fill(4)

                        def score_fn(ps, off, kt, comp=comp, hh=hh, qs_=qs_):
                            qh, kh, vh = heads[hh]
                            c.mm(ps[:, off:off + 512], kh[:, comp, kt * 128:(kt + 1) * 128], qh[:, qs_], True, True,
                                 [kh, qh], [ps], sig=(off == 512))

                        def v_fn(kt, hh=hh):
                            vh = heads[hh][2]
                            return vh[:, kt, :], vh

                        def post_fn(acc_o, acc_d, hh=hh, qs_=qs_, comp=comp, pair=pair):
                            rec = rec_r.next()
                            c.recip(rec[:, :], acc_d[:, :], [acc_d], [rec])
                            oc = oc_r.next()
                            c.tt("dve", oc[:, :], acc_o[:, :], rec[:, :], ALU.mult, [acc_o, rec], [oc])
                            pair[comp] = oc
                            if comp == 0:
                                return
                            o = oc_r.next()
                            c.stt(o[:, :], pair[1][:, :], neglam, pair[0][:, :], ALU.mult, ALU.add,
                                  [pair[0], pair[1], lams], [o])
                            sq = sq_r.next()
                            c.tt("pool", sq[:, :], o[:, :], o[:, :], ALU.mult, [o], [sq])
                            return lambda: subln_tail(o, sq, hh, qs_)

                        def subln_tail(o, sq, hh, qs_):
                            pn = ps_d.tiles[1]
                            c.mm(pn[:, :], ones[:, :], sq[:, :], True, True, [ones, sq], [pn])
                            rec = rec_r.next()
                            c.ts("dve", rec[:, :], pn[:, :], 1.0 / 128.0, 1e-5, ALU.mult, ALU.add, [pn], [rec])
                            c.act(rec[:, :], rec[:, :], AF.Ln, [rec], [rec])
                            c.act(rec[:, :], rec[:, :], AF.Exp, [rec], [rec], scale=-0.5)
                            c.tt("pool", o[:, :], o[:, :], rec[:, :], ALU.mult, [o, rec], [o])
                            ost = ost_r.next()
                            c.ts("pool", ost[:, :], o[:, :], gT[:, 54:55], 1.0 - LAM_INIT, ALU.mult, ALU.mult, [o, gT], [ost])
                            c.dma("sp", oa_d[hh, :, qs_], ost[:, :], [ost], [oa_d])

                        calls.append({"nk": NT, "score_fn": score_fn, "v_fn": v_fn, "post_fn": post_fn, "pre_fn": pre_fn,
                                      "acc_d_tile": ps_d.tiles[comp]})
            attn_stream(calls, ps_s, e_ring, ps_o, ps_d, 0.125, 1)
            c.barrier()
        c.stack = outer
        if "oa_s" in dump:
            final_outs.append(oa_d.b)
        if stop_after == "B":
            stop_here()
            return nc

        with ExitStack() as sc_:
            c.stack = sc_
            cqn = c.sb("cqn", [128, 4, S], BF16)
            ckvn = c.sb("ckvn", [128, 2, S], BF16)
            kpe = c.sb("kpe", [128, S], BF16)
            c.memset("pool", kpe[:, :], 0.0, [kpe])
            Cm = c.sb("Cm2", [64, S], F32)
            Sm = c.sb("Sm2", [64, S], F32)
            c.dma("sp", kpe[0:64, :], kpe_d[:, :], [kpe_d], [kpe])
            with ExitStack() as sc1:
                c.stack = sc1
                tmp_i = c.sb("tmp_i2", [64, S], I32)
                tmp_f = c.sb("tmp_f2", [64, S], F32)
                rope_tables(c, pos_d, cf, 1, 64, Cm, Sm, tmp_i, tmp_f)
                c.barrier()
            with ExitStack() as sc2:
                c.stack = sc2
                cf32 = c.sb("cf32", [128, 4, S], F32)
                sq_r = c.sb_ring("sqc", 2, [128, 512], BF16)
                ps_n = c.ps_ring("ps_nc", 2, [128, 512], F32)
                rec_r = c.sb_ring("recc", 2, [128, 512], F32)
                for src_d, nch, dst, goff in ((cq_d, 4, cqn, 48), (ckv_d, 2, ckvn, 52)):
                    for ch in range(nch):
                        c.dma("sp", cf32[:, ch, :], src_d[ch, :, :], [src_d], [cf32])
                    for blk in range(4):
                        cs = slice(blk * 512, (blk + 1) * 512)
                        pn = ps_n.next()
                        for ch in range(nch):
                            sq = sq_r.next()
                            c.act(sq[:, :], cf32[:, ch, cs], AF.Square, [cf32], [sq])
                            c.mm(pn[:, :], ones[:, :], sq[:, :], ch == 0, ch == nch - 1, [ones, sq], [pn], sig=True)
                        rec = rec_r.next()
                        c.ts("dve", rec[:, :], pn[:, :], 1.0 / (nch * 128), EPS, ALU.mult, ALU.add, [pn], [rec])
                        c.p.op("act", lambda h, rec=rec: h.sqrt(rec[:, :], rec[:, :]), bs([rec]), bs([rec]))
                        c.recip(rec[:, :], rec[:, :], [rec], [rec])
                        for ch in range(nch):
                            c.stt(dst[:, ch, cs], cf32[:, ch, cs], gT[:, goff + ch:goff + ch + 1], rec[:, :],
                                  ALU.mult, ALU.mult, [cf32, gT, rec], [dst])
                c.barrier()
            if stop_after == "C1":
                dbg = dram("cqn_dbg", [128, 4, S], BF16)
                c.dma("sp", dbg[:, :, :], cqn[:, :, :], [cqn], [dbg])
                final_outs.append(dbg.b)
                stop_here()
                return nc
            c.stack = sc_
            psA = c.ps_ring("psAC", 2, [128, 512], F32)
            rg = {"qs": c.sb_ring("qsC", 2, [128, 512], BF16), "psr": psA,
                  "t1": c.sb_ring("t1C", 2, [128, 512], F32), "t2": c.sb_ring("t2C", 2, [128, 512], F32)}
            slq_r = c.sb_ring("slq", 2, [128, 4, 192], BF16)
            slkv_r = c.sb_ring("slkv", 2, [128, 2, 256], BF16)
            qn_r = c.sb_ring("qn", 2, [128, S], BF16)
            qp_r = c.sb_ring("qp", 2, [128, S], BF16)
            for _qt in qp_r.tiles:
                c.memset("pool", _qt[:, :], 0.0, [_qt])
            kn_r = c.sb_ring("kn", 2, [128, S], BF16)
            vh_r = c.sb_ring("vhC", 2, [128, NT, 128], BF16)
            ps_s = c.ps_ring("ps_sC", 2, [128, 1024], F32)
            ps_o = c.ps_ring("ps_oC", 1, [128, 512], F32)
            ps_d = c.ps_ring("ps_dC", 1, [128, 512], F32)
            rec_r = c.sb_ring("recC", 2, [128, 512], F32)
            ost_r = c.sb_ring("ostC", 3, [128, 512], BF16)
            e_ring = c.sb_ring("EC", 4, [128, 1024], BF16)
            heads = {}

            def prep_head(hh):
                if hh >= H or hh in heads:
                    return
                slq, slkv = slq_r.next(), slkv_r.next()
                c.dma("pool", slq[:, :, :], w_uq_d[:, hh * 192:(hh + 1) * 192].rearrange("(kc p) n -> p kc n", p=128),
                      [w_uq_d], [slq])
                c.dma("pool", slkv[:, :, :], w_ukv_d[:, hh * 256:(hh + 1) * 256].rearrange("(kc p) n -> p kc n", p=128),
                      [w_ukv_d], [slkv])
                qn, qp, kn, vh = qn_r.next(), qp_r.next(), kn_r.next(), vh_r.next()
                heads[hh] = (qn, qp, kn, vh)
                for blk in range(4):
                    cs = slice(blk * 512, (blk + 1) * 512)
                    ps = psA.next()
                    for kc in range(4):
                        c.mm(ps[:, :], slq[:, kc, 0:128], cqn[:, kc, cs], kc == 0, kc == 3, [slq, cqn], [ps])
                    c.copy("dve", qn[:, cs], ps[:, :], [ps], [qn])
                    ps = psA.next()
                    for kc in range(4):
                        c.mm(ps[:64, :], slq[:, kc, 128:192], cqn[:, kc, cs], kc == 0, kc == 3, [slq, cqn], [ps])
                    rope_evac(c, ps, 64, blk, rm, Cm, Sm, rg, qp[:64, cs], qp)
                    ps = psA.next()
                    for kc in range(2):
                        c.mm(ps[:, :], slkv[:, kc, 0:128], ckvn[:, kc, cs], kc == 0, kc == 1, [slkv, ckvn], [ps])
                    c.copy("dve", kn[:, cs], ps[:, :], [ps], [kn])
                for t4 in range(NT // 4):
                    ps = psA.next()
                    for j in range(4):
                        t = t4 * 4 + j
                        for kc in range(2):
                            c.mm(ps[:, j * 128:(j + 1) * 128], ckvn[:, kc, t * 128:(t + 1) * 128], slkv[:, kc, 128:256],
                                 kc == 0, kc == 1, [slkv, ckvn], [ps], sig=(kc == 1 and j == 3))
                    c.copy("dve", vh[:, t4 * 4:(t4 + 1) * 4, :],
                           ps[:, :].rearrange("p (a b) -> p a b", a=4), [ps], [vh])

            calls = []
            for hh in range(H):
                for qb in range(4):
                    qs_ = slice(qb * 512, (qb + 1) * 512)

                    def pre_fn(hh=hh, qb=qb):
                        if qb == 0:
                            prep_head(hh)
                        if qb == 2:
                            prep_head(hh + 1)

                    def score_fn(ps, off, kt, hh=hh, qs_=qs_):
                        qn, qp, kn, vh = heads[hh]
                        ks = slice(kt * 128, (kt + 1) * 128)
                        c.mm(ps[:, off:off + 512], kn[:, ks], qn[:, qs_], True, False, [kn, qn], [ps], sig=False)
                        c.mm(ps[:, off:off + 512], kpe[:, ks], qp[:, qs_], False, True, [kpe, qp], [ps], sig=(off == 512))

                    def v_fn(kt, hh=hh):
                        vh = heads[hh][3]
                        return vh[:, kt, :], vh

                    def post_fn(acc_o, acc_d, hh=hh, qs_=qs_):
                        rec = rec_r.next()
                        c.recip(rec[:, :], acc_d[:, :], [acc_d], [rec])
                        ost = ost_r.next()
                        c.tt("dve", ost[:, :], acc_o[:, :], rec[:, :], ALU.mult, [acc_o, rec], [ost])
                        c.dma("sp", ob_d[hh, :, qs_], ost[:, :], [ost], [ob_d])

                    calls.append({"nk": NT, "score_fn": score_fn, "v_fn": v_fn, "post_fn": post_fn, "pre_fn": pre_fn})
            attn_stream(calls, ps_s, e_ring, ps_o, ps_d, 192.0 ** -0.5, 1)
            c.barrier()
        c.stack = outer
        if "ob_s" in dump:
            final_outs.append(ob_d.b)
        if stop_after == "C":
            stop_here()
            return nc

        with ExitStack() as sd_:
            c.stack = sd_
            mergedT = c.sb("mergedT", [128, DC, S], BF16)
            with ExitStack() as sd1:
                c.stack = sd1
                oaT = c.sb("oaT", [128, H, S], BF16)
                obT = c.sb("obT", [128, H, S], BF16)
                for hh in range(H):
                    c.dma("sp", oaT[:, hh, :], oa_d[hh, :, :], [oa_d], [oaT])
                    c.dma("sp", obT[:, hh, :], ob_d[hh, :, :], [ob_d], [obT])
                sla_r = c.sb_ring("sla", 2, [128, H, 512], BF16)
                slb_r = c.sb_ring("slb", 2, [128, H, 512], BF16)
                ga_r = c.sb_ring("gaT", 2, [128, S], BF16)
                gb_r = c.sb_ring("gbT", 2, [128, S], BF16)
                ps_a = c.ps_ring("ps_a", 3, [128, 512], F32)
                ps_b = c.ps_ring("ps_b", 3, [128, 512], F32)
                t1_r = c.sb_ring("t1D", 2, [128, 512], F32)
                t2_r = c.sb_ring("t2D", 2, [128, 512], F32)
                slabs_d = {}
                gates_d = {}

                def load_slabs_d(c4):
                    if c4 >= 4 or c4 in slabs_d:
                        return
                    sla, slb = sla_r.next(), slb_r.next()
                    c.dma("pool", sla[:, :, :], w_od_d[:, c4 * 512:(c4 + 1) * 512].rearrange("(kc p) n -> p kc n", p=128),
                          [w_od_d], [sla])
                    c.dma("pool", slb[:, :, :], w_om_d[:, c4 * 512:(c4 + 1) * 512].rearrange("(kc p) n -> p kc n", p=128),
                          [w_om_d], [slb])
                    slabs_d[c4] = (sla, slb)

                def load_gates_d(ch):
                    if ch >= DC or ch in gates_d:
                        return
                    ga, gb = ga_r.next(), gb_r.next()
                    c.dma("sp", ga[:, :], ga_d[ch, :, :], [ga_d], [ga])
                    c.dma("sp", gb[:, :], gb_d[ch, :, :], [gb_d], [gb])
                    gates_d[ch] = (ga, gb)

                load_slabs_d(0)
                load_gates_d(0)
                for c4 in range(4):
                    sla, slb = slabs_d.pop(c4)
                    for j in range(4):
                        ch = c4 * 4 + j
                        ga, gb = gates_d.pop(ch)
                        load_gates_d(ch + 1)
                        if j == 1:
                            load_slabs_d(c4 + 1)
                        for blk in range(4):
                            cs = slice(blk * 512, (blk + 1) * 512)
                            pa, pb = ps_a.next(), ps_b.next()
                            for kc in range(H):
                                c.mm(pa[:, :], sla[:, kc, j * 128:(j + 1) * 128], oaT[:, kc, cs], kc == 0, kc == H - 1,
                                     [sla, oaT], [pa])
                            for kc in range(H):
                                c.mm(pb[:, :], slb[:, kc, j * 128:(j + 1) * 128], obT[:, kc, cs], kc == 0, kc == H - 1,
                                     [slb, obT], [pb])
                            t1, t2 = t1_r.next(), t2_r.next()
                            c.tt("dve", t1[:, :], pa[:, :], ga[:, cs], ALU.mult, [pa, ga], [t1])
                            c.tt("dve", t2[:, :], pb[:, :], gb[:, cs], ALU.mult, [pb, gb], [t2])
                            c.tt("pool", mergedT[:, ch, cs], t1[:, :], t2[:, :], ALU.add, [t1, t2], [mergedT])
                c.barrier()
            c.stack = sd_
            wout = c.sb("wout", [128, DC, D], BF16)
            for nb in range(4):
                c.dma("pool", wout[:, :, nb * 512:(nb + 1) * 512],
                      w_out_d[:, nb * 512:(nb + 1) * 512].rearrange("(kc p) n -> p kc n", p=128), [w_out_d], [wout])
            xin = c.sb_ring("xinD", 2, [128, D], F32)
            hout = c.sb_ring("houtD", 2, [128, D], F32)
            ps_h = c.ps_ring("ps_h", 4, [128, 512], F32)
            for t in range(NT):
                ts_ = slice(t * 128, (t + 1) * 128)
                xt, ht = xin.next(), hout.next()
                c.dma("sp", xt[:, :], x_d[ts_, :], [x_d], [xt])
                for nb in range(4):
                    ns = slice(nb * 512, (nb + 1) * 512)
                    ps = ps_h.next()
                    for kc in range(DC):
                        c.mm(ps[:, :], mergedT[:, kc, ts_], wout[:, kc, ns], kc == 0, kc == DC - 1, [mergedT, wout], [ps])
                    c.tt("dve", ht[:, ns], ps[:, :], xt[:, ns], ALU.add, [ps, xt], [ht])
                c.dma("sp", h1_d[ts_, :], ht[:, :], [ht], [h1_d])
            c.barrier()
        c.stack = outer
        if "h1_s" in dump:
            final_outs.append(h1_d.b)
        if stop_after == "D":
            stop_here()
            return nc

        with ExitStack() as se_:
            c.stack = se_
            qcT = c.sb("qcT", [128, 4, S], BF16)
            kcT = c.sb("kcT", [128, 4, 256], BF16)
            vc = c.sb("vc", [128, 2, 512], BF16)
            ocT = c.sb("ocT", [128, 4, S], BF16)
            with ExitStack() as se1:
                c.stack = se1
                hn1T_b = [c.sb("hn1T%d" % i, [128, DC, 512], BF16) for i in range(4)]
                memnT = c.sb("memnT", [128, DC, 256], BF16)
                rg = {"junk": c.sb_ring("junkE", 1, [128, D], BF16), "ss": c.sb_ring("ssE", 4, [128, 4], F32),
                      "xs": c.sb_ring("xsE", 2, [128, D], BF16), "pst": c.ps_ring("pstE", 2, [128, 512], BF16)}
                xin = c.sb_ring("xinE", 3, [128, D], F32)
                slab_r = c.sb_ring("slabE", 2, [128, DC, 512], BF16)
                psA = c.ps_ring("psAE", 4, [128, 512], F32)
                for mt in range(2):
                    xt = xin.next()
                    c.dma("sp", xt[:, :], mem_d[mt * 128:(mt + 1) * 128, :], [mem_d], [xt])
                    norm_transpose_tile(c, xt, gT, 32, memnT, mt * 128, rg, ident)
                slq_ = slab_r.next()
                c.dma("pool", slq_[:, :, :], w_cq_d[:, :].rearrange("(kc p) n -> p kc n", p=128), [w_cq_d], [slq_])

                def load_h1(t):
                    xt = xin.next()
                    c.dma("sp", xt[:, :], h1_d[t * 128:(t + 1) * 128, :], [h1_d], [xt])
                    return xt

                def q_proj_block(t):
                    if t % 4 != 3:
                        return
                    blk = t // 4
                    cs = slice(blk * 512, (blk + 1) * 512)
                    for hh in range(4):
                        ps = psA.next()
                        for kc in range(DC):
                            c.mm(ps[:, :], slq_[:, kc, hh * 128:(hh + 1) * 128], hn1T_b[blk][:, kc, :], kc == 0, kc == DC - 1,
                                 [slq_, hn1T_b[blk]], [ps])
                        c.copy("act" if hh % 2 == 0 else "dve", qcT[:, hh, cs], ps[:, :], [ps], [qcT])

                norm_transpose_stream(c, NT, load_h1, gT, 16, None, rg, ident,
                                      dst_fn=lambda t: (hn1T_b[t // 4], (t % 4) * 128), after_fn=q_proj_block)
                sl = slab_r.next()
                c.dma("pool", sl[:, :, :], w_ckv_d[:, 0:512].rearrange("(kc p) n -> p kc n", p=128), [w_ckv_d], [sl])
                for hh in range(4):
                    ps = psA.next()
                    for kc in range(DC):
                        c.mm(ps[:, 0:256], sl[:, kc, hh * 128:(hh + 1) * 128], memnT[:, kc, :], kc == 0, kc == DC - 1,
                             [sl, memnT], [ps])
                    c.copy("act", kcT[:, hh, :], ps[:, 0:256], [ps], [kcT])
                sl = slab_r.next()
                c.dma("pool", sl[:, :, :], w_ckv_d[:, 512:1024].rearrange("(kc p) n -> p kc n", p=128), [w_ckv_d], [sl])
                for mt in range(2):
                    ps = psA.next()
                    for kc in range(DC):
                        c.mm(ps[:, :], memnT[:, kc, mt * 128:(mt + 1) * 128], sl[:, kc, :], kc == 0, kc == DC - 1,
                             [sl, memnT], [ps])
                    c.copy("dve", vc[:, mt, :], ps[:, :], [ps], [vc])
                c.barrier()
            with ExitStack() as se2:
                c.stack = se2
                e_ring = c.sb_ring("EE", 4, [128, 512], BF16)
                ps_s = c.ps_ring("ps_sE", 3, [128, 512], F32)
                ps_o = c.ps_ring("ps_oE", 2, [128, 512], F32)
                ps_d = c.ps_ring("ps_dE", 2, [128, 512], F32)
                rec_r = c.sb_ring("recE", 2, [128, 512], F32)
                for hh in range(4):
                    for qb in range(4):
                        qs_ = slice(qb * 512, (qb + 1) * 512)
                        acc_o, acc_d = ps_o.next(), ps_d.next()

                        def score_fn(ps, kt, hh=hh, qs_=qs_):
                            c.mm(ps[:, :], kcT[:, hh, kt * 128:(kt + 1) * 128], qcT[:, hh, qs_], True, True,
                                 [kcT, qcT], [ps])

                        def v_fn(kt, hh=hh):
                            return vc[:, kt, hh * 128:(hh + 1) * 128], vc

                        attn_core(2, qb, score_fn, v_fn, ps_s, e_ring, acc_o, acc_d, 128.0 ** -0.5)
                        rec = rec_r.next()
                        c.recip(rec[:, :], acc_d[:, :], [acc_d], [rec])
                        c.tt("dve", ocT[:, hh, qs_], acc_o[:, :], rec[:, :], ALU.mult, [acc_o, rec], [ocT])
                c.barrier()
            with ExitStack() as se3:
                c.stack = se3
                wco = c.sb("wco", [128, 4, D], BF16)
                c.dma("pool", wco[:, :, :], w_co_d[:, :].rearrange("(kc p) n -> p kc n", p=128), [w_co_d], [wco])
                gff = c.sb("gff", [128, D], F32)
                c.dma("sp", gff[:, :], grow_d[0:1, :].partition_broadcast(128), [grow_d], [gff])
                wr = c.sb("wr", [128, DC, 36], BF16)
                c.dma("pool", wr[:, :, :], w_r_d[:, :].rearrange("(kc p) n -> p kc n", p=128), [w_r_d], [wr])
                br = c.sb("br", [128, 36], F32)
                c.dma("sp", br[:, :], b_r_d[0:1, :].partition_broadcast(128), [b_r_d], [br])
                A_all = c.sb("A_all", [128, NT, 32], BF16)
                hin = c.sb_ring("hinE", 2, [128, D], F32)
                hout = c.sb_ring("houtE", 2, [128, D], F32)
                ttok_r = c.sb_ring("ttok", 3, [128, D], BF16)
                tTs_r = c.sb_ring("tTs", 2, [128, DC, 128], BF16)
                junk_r = c.sb_ring("junkE3", 1, [128, D], BF16)
                ss_r = c.sb_ring("ssE3", 4, [128, 4], F32)
                rt_r = c.sb_ring("rt", 3, [128, 320], F32)
                ps_h = c.ps_ring("ps_hE", 3, [128, 512], F32)
                pst_r = c.ps_ring("pstE3", 2, [128, 512], BF16)
                ps_l = c.ps_ring("ps_l", 2, [128, 64], F32)
                ps_r = c.ps_ring("ps_r", 1, [128, 64], F32)
                pend_tail = []
                for t in range(NT):
                    ts_ = slice(t * 128, (t + 1) * 128)
                    xt, ht = hin.next(), hout.next()
                    c.dma("sp", xt[:, :], h1_d[ts_, :], [h1_d], [xt])
                    for nb in range(4):
                        ns = slice(nb * 512, (nb + 1) * 512)
                        ps = ps_h.next()
                        for kc in range(4):
                            c.mm(ps[:, :], ocT[:, kc, ts_], wco[:, kc, ns], kc == 0, kc == 3, [ocT, wco], [ps])
                        c.tt("dve", ht[:, ns], ps[:, :], xt[:, ns], ALU.add, [ps, xt], [ht])
                    c.dma("sp", h2_d[ts_, :], ht[:, :], [ht], [h2_d])
                    ss, junk, ttok = ss_r.next(), junk_r.next(), ttok_r.next()
                    rmsnorm_rstd(c, ht, D, ss, junk, EPS)
                    c.stt(ttok[:, :], ht[:, :], ss[:, 2:3], gff[:, :], ALU.mult, ALU.mult, [ht, ss, gff], [ttok])
                    tTs = tTs_r.next()
                    for g4 in range(DC // 4):
                        pt = pst_r.next()
                        for j in range(4):
                            ch = g4 * 4 + j
                            c.tr(pt[:, j * 128:(j + 1) * 128], ttok[:, ch * 128:(ch + 1) * 128], ident[:, :],
                                 [ttok, ident], [pt], sig=(j == 3))
                        c.copy("act" if g4 % 2 == 0 else "dve", tTs[:, g4 * 4:(g4 + 1) * 4, :],
                               pt[:, :].rearrange("p (a b) -> p a b", a=4), [pt], [tTs])
                    pl = ps_l.next()
                    for kc in range(DC):
                        c.mm(pl[:, 0:36], tTs[:, kc, :], wr[:, kc, :], kc == 0, kc == DC - 1, [tTs, wr], [pl])
                    rt = rt_r.next()
                    R_ = [rt]
                    L = rt[:, 0:36]
                    c.tt("dve", L, pl[:, 0:36], br[:, :], ALU.add, [pl, br], R_)
                    gmax, ngmax, gsum, gp = rt[:, 40:41], rt[:, 41:42], rt[:, 42:43], rt[:, 43:44]
                    c.p.op("dve", lambda h, rt=rt: h.reduce_max(rt[:, 40:41], rt[:, 0:4], axis=AX.X), bs(R_), bs(R_))
                    c.ts("pool", ngmax, gmax, -1.0, None, ALU.mult, None, R_, R_)
                    c.act(rt[:, 44:48], rt[:, 0:4], AF.Exp, R_, R_, bias=ngmax, scale=1.0, accum=gsum)
                    c.recip(gp, gsum, R_, R_)
                    c.ts("pool", rt[:, 48:52], rt[:, 0:4], gmax, None, ALU.is_ge, None, R_, R_)
                    c.ts("pool", rt[:, 52:56], rt[:, 48:52], -1.0, 1e30, ALU.add, ALU.mult, R_, R_)
                    for g in range(4):
                        c.ts("pool", rt[:, 64 + g * 8:72 + g * 8], rt[:, 4 + g * 8:12 + g * 8], rt[:, 52 + g:53 + g], None,
                             ALU.add, None, R_, R_)
                    Lm = rt[:, 64:96]
                    c.p.op("dve", lambda h, rt=rt: h.max(rt[:, 96:104], rt[:, 64:96]), bs(R_), bs(R_))
                    c.ts("pool", rt[:, 104:136], Lm, rt[:, 96:97], None, ALU.is_equal, None, R_, R_)
                    c.ts("pool", rt[:, 136:168], Lm, rt[:, 97:98], None, ALU.is_equal, None, R_, R_)
                    c.tt("pool", rt[:, 56:57], rt[:, 97:98], rt[:, 96:97], ALU.subtract, R_, R_)
                    c.act(rt[:, 57:58], rt[:, 56:57], AF.Exp, R_, R_)
                    c.ts("pool", rt[:, 58:59], rt[:, 57:58], 1.0, None, ALU.add, None, R_, R_)
                    c.recip(rt[:, 58:59], rt[:, 58:59], R_, R_)
                    c.tt("pool", wts[:, t, 0:1], rt[:, 58:59], gp, ALU.mult, R_, [wts])
                    c.stt(wts[:, t, 1:2], rt[:, 57:58], rt[:, 58:59], gp, ALU.mult, ALU.mult, R_, [wts])
                    c.tt("pool", A_all[:, t, :], rt[:, 104:136], rt[:, 136:168], ALU.add, R_, [A_all])
                    def tail(t=t, rt=rt, R_=R_, ttok=ttok):
                        pr = ps_r.next()
                        c.mm(pr[:, 0:32], Utri[:, :], A_all[:, t, :], True, t == 0, [Utri, A_all], [pr], sig=(t == 0))
                        for tp in range(t):
                            c.mm(pr[:, 0:32], ones[:, :], A_all[:, tp, :], False, tp == t - 1, [ones, A_all], [pr],
                                 sig=(tp == t - 1))
                        c.ts("dve", rt[:, 200:232], pr[:, 0:32], float(CAP), 1e6, ALU.is_ge, ALU.mult, [pr], R_)
                        c.tt("dve", rt[:, 168:200], pr[:, 0:32], cf[:, 2:2 + NE], ALU.add, [pr, cf], R_)
                        c.tt("pool", rt[:, 168:200], rt[:, 168:200], rt[:, 200:232], ALU.add, R_, R_)
                        for k, oh0 in ((0, 104), (1, 136)):
                            c.tt("pool", rt[:, 232 + 32 * k:264 + 32 * k], rt[:, oh0:oh0 + 32], rt[:, 168:200], ALU.mult, R_, R_)
                            c.p.op("dve", lambda h, rt=rt, k=k: h.reduce_sum(rt[:, 59 + k:60 + k], rt[:, 232 + 32 * k:264 + 32 * k],
                                                                           axis=AX.X), bs(R_), bs(R_))
                            c.ts("pool", rt[:, 59 + k:60 + k], rt[:, 59 + k:60 + k], float(NSLOT), None, ALU.min, None, R_, R_)
                            c.copy("pool", slot_i[:, t, k:k + 1], rt[:, 59 + k:60 + k], R_, [slot_i])
                        for k in range(0 if NO_SCATTER else 2):
                            c.p.dma("pool", lambda h, ttok=ttok, t=t, k=k: h.indirect_dma_start(
                                out=xg_d[:, :], out_offset=bass.IndirectOffsetOnAxis(ap=slot_i[:, t, k:k + 1], axis=0),
                                in_=ttok[:, :], in_offset=None, bounds_check=bc_reg, oob_is_err=False),
                                bs([ttok, slot_i]), bs([xg_d]))

                    if pend_tail:
                        pend_tail.pop(0)()
                    pend_tail.append(tail)
                while pend_tail:
                    pend_tail.pop(0)()
                c.barrier()
        c.stack = outer
        for nm, tl in (("h2_s", h2_d), ("xg_s", xg_d)):
            if nm in dump:
                final_outs.append(tl.b)
        if "route" in dump:
            rdump = dram("route", [128, NT, 4], F32)
            c.dma("sp", rdump[:, :, 2:4], wts[:, :, :], [wts], [rdump])
            sfl = c.sb("sfl", [128, NT, 2], F32)
            c.copy("dve", sfl[:, :, :], slot_i[:, :, :], [slot_i], [sfl])
            c.dma("sp", rdump[:, :, 0:2], sfl[:, :, :], [sfl], [rdump])
            final_outs.append(rdump.b)
        if stop_after == "E":
            stop_here()
            return nc

        with ExitStack() as sf_:
            c.stack = sf_
            wg_r = c.sb_ring("wg", 2, [128, DC, DFF], BF16)
            wu_r = c.sb_ring("wu", 2, [128, DC, DFF], BF16)
            wd_r = c.sb_ring("wd", 2, [128, 4, D], BF16)
            xgT_r = c.sb_ring("xgT", 2, [128, DC, CAP], BF16)
            hid_r = c.sb_ring("hid", 2, [128, 4, CAP], BF16)
            sg_r = c.sb_ring("sg", 2, [128, CAP], F32)
            y_r = c.sb_ring("yt", 3, [128, D], BF16)
            pst_r = c.ps_ring("pstF", 2, [128, 512], BF16)
            ps_g = c.ps_ring("ps_g", 2, [128, CAP], F32)
            ps_u = c.ps_ring("ps_u", 2, [128, CAP], F32)
            ps_y = c.ps_ring("ps_y", 2, [128, 512], F32)
            NSB = CAP // 128
            xg_r6 = c.sb_ring("xgt6", 2 * NSB, [128, D], BF16)
            W = {}
            XT = {}
            TG = {}

            def load_weights(e):
                if e >= NE:
                    return
                wg, wu, wd = wg_r.next(), wu_r.next(), wd_r.next()
                c.dma("pool", wg[:, :, :], w_eg_d[e, :, :].rearrange("(p kc) n -> p kc n", p=128), [w_eg_d], [wg])
                c.dma("pool", wu[:, :, :], w_eu_d[e, :, :].rearrange("(p kc) n -> p kc n", p=128), [w_eu_d], [wu])
                c.dma("pool", wd[:, :, :], w_ed_d[e, :, :].rearrange("(p kc) n -> p kc n", p=128), [w_ed_d], [wd])
                W[e] = (wg, wu, wd)

            def load_xg(e):
                if e >= NE:
                    return
                xgT = xgT_r.next()
                XT[e] = xgT
                groups = []
                for sb in range(NSB):
                    xg = xg_r6.next()
                    r0 = e * CAP + sb * 128
                    c.dma("sp", xg[:, :], xg_d[r0:r0 + 128, :], [xg_d], [xg])
                    for g4 in range(DC // 4):
                        groups.append((xg, sb, g4))
                TG[e] = groups

            def transpose_groups(e, n):
                if e >= NE:
                    return
                xgT = XT[e]
                for _ in range(n):
                    if not TG[e]:
                        return
                    xg, sb, g4 = TG[e].pop(0)
                    pt = pst_r.next()
                    for j in range(4):
                        ch = g4 * 4 + j
                        c.tr(pt[:, j * 128:(j + 1) * 128], xg[:, ch:D:DC], ident[:, :], [xg, ident], [pt], sig=(j == 3))
                    c.copy("act" if g4 % 2 == 0 else "dve", xgT[:, g4 * 4:(g4 + 1) * 4, sb * 128:(sb + 1) * 128],
                           pt[:, :].rearrange("p (a b) -> p a b", a=4), [pt], [xgT])

            load_xg(0)
            load_xg(1)
            load_weights(0)
            transpose_groups(0, 4 * NSB)
            for e in range(NE):
                load_xg(e + 2)
                load_weights(e + 1)
                wg, wu, wd = W.pop(e)
                xgT = XT.pop(e)
                hid = hid_r.next()
                for dc in range(4):
                    pg, pu = ps_g.next(), ps_u.next()
                    for kc in range(DC):
                        c.mm(pg[:, :], wg[:, kc, dc:DFF:4], xgT[:, kc, :], kc == 0, kc == DC - 1,
                             [wg, xgT], [pg])
                    for kc in range(DC):
                        c.mm(pu[:, :], wu[:, kc, dc:DFF:4], xgT[:, kc, :], kc == 0, kc == DC - 1,
                             [wu, xgT], [pu])
                    transpose_groups(e + 1, 2)
                    sg = sg_r.next()
                    c.act(sg[:, :], pg[:, :], AF.Silu, [pg], [sg])
                    c.tt("dve", hid[:, dc, :], sg[:, :], pu[:, :], ALU.mult, [sg, pu], [hid])
                for sb in range(NSB):
                    yt = y_r.next()
                    for nb in range(4):
                        ns = slice(nb * 512, (nb + 1) * 512)
                        py = ps_y.next()
                        for dc in range(4):
                            c.mm(py[:, :], hid[:, dc, sb * 128:(sb + 1) * 128], wd[:, dc, ns], dc == 0, dc == 3,
                                 [hid, wd], [py])
                        c.copy("act" if nb % 2 == 0 else "dve", yt[:, ns], py[:, :], [py], [yt])
                    transpose_groups(e + 1, 2)
                    r0 = e * CAP + sb * 128
                    c.dma("sp", y_d[r0:r0 + 128, :], yt[:, :], [yt], [y_d])
                transpose_groups(e + 1, 4 * NSB)
            c.barrier()
        c.stack = outer
        if "y_s" in dump:
            final_outs.append(y_d.b)
        if stop_after == "F":
            stop_here()
            return nc

        with ExitStack() as sg_:
            c.stack = sg_
            gfin = c.sb("gfin", [128, D], F32)
            c.dma("sp", gfin[:, :], grow_d[1:2, :].partition_broadcast(128), [grow_d], [gfin])
            hin = c.sb_ring("hinG", 3, [128, D], F32)
            y1_r = c.sb_ring("y1", 3, [128, D], BF16)
            y2_r = c.sb_ring("y2", 3, [128, D], BF16)
            h3_r = c.sb_ring("h3", 3, [128, D], F32)
            o_r = c.sb_ring("og", 2, [128, D], F32)
            junk_r = c.sb_ring("junkG", 1, [128, D], BF16)
            ss_r = c.sb_ring("ssG", 4, [128, 4], F32)
            def g_phase1(t):
                ts_ = slice(t * 128, (t + 1) * 128)
                ht, y1, y2, h3 = hin.next(), y1_r.next(), y2_r.next(), h3_r.next()
                c.dma("sp", ht[:, :], h2_d[ts_, :], [h2_d], [ht])
                for k, yk in ((0, y1), (1, y2)):
                    c.memset("pool", yk[:, :], 0.0, [yk])
                    c.p.dma("pool", lambda h, yk=yk, t=t, k=k: h.indirect_dma_start(
                        out=yk[:, :], out_offset=None, in_=y_d[:, :],
                        in_offset=bass.IndirectOffsetOnAxis(ap=slot_i[:, t, k:k + 1], axis=0),
                        bounds_check=bc_reg, oob_is_err=False), bs([y_d, slot_i]), bs([yk]))
                c.stt(h3[:, :], y1[:, :], wts[:, t, 0:1], ht[:, :], ALU.mult, ALU.add, [y1, wts, ht], [h3])
                c.stt(h3[:, :], y2[:, :], wts[:, t, 1:2], h3[:, :], ALU.mult, ALU.add, [y2, wts, h3], [h3])
                ss, junk = ss_r.next(), junk_r.next()
                c.act(junk[:, :D], h3[:, :D], AF.Square, [h3], [junk, ss], accum=ss[:, 0:1])
                return h3, ss

            def g_phase2(t, h3, ss):
                ts_ = slice(t * 128, (t + 1) * 128)
                og = o_r.next()
                c.ts("dve", ss[:, 1:2], ss[:, 0:1], 1.0 / D, EPS, ALU.mult, ALU.add, [ss], [ss])
                c.p.op("act", lambda h: h.sqrt(ss[:, 3:4], ss[:, 1:2]), bs([ss]), bs([ss]))
                c.recip(ss[:, 2:3], ss[:, 3:4], [ss], [ss])
                c.stt(og[:, :], h3[:, :], ss[:, 2:3], gfin[:, :], ALU.mult, ALU.mult, [h3, ss, gfin], [og])
                c.dma("sp", out_d[ts_, :], og[:, :], [og], [out_d])

            cur = g_phase1(0)
            for t in range(NT):
                nxt = g_phase1(t + 1) if t + 1 < NT else None
                g_phase2(t, *cur)
                cur = nxt
            final_outs.append(out_d.b)
            finish()
        c.stack = outer
    return nc


def _fm(g):
    g = np.asarray(g, np.float32).reshape(-1)
    return np.ascontiguousarray(g.reshape(-1, 128).T)


def host_shared(inputs):
    f32 = np.float32
    bf = ml_dtypes.bfloat16
    m = {}
    for k in ("w_in", "w_o_diff", "w_uq", "w_ukv", "w_o_mla", "w_out", "w_cq", "w_ckv", "w_co",
              "w_expert_gate", "w_expert_up", "w_expert_down"):
        m[k] = np.ascontiguousarray(np.asarray(inputs[k], f32)[0])
    m["w_router"] = np.ascontiguousarray(np.concatenate(
        [np.asarray(inputs["w_router_group"], f32)[0], np.asarray(inputs["w_router_expert"], f32)[0]], axis=1))
    m["b_router"] = np.ascontiguousarray(np.concatenate(
        [np.asarray(inputs["b_router_group"], f32)[0], np.asarray(inputs["b_router_expert"], f32)[0]])[None, :])
    gT = np.concatenate([_fm(inputs["attn_norm_g"][0]), _fm(inputs["cross_norm_g"][0]), _fm(inputs["mem_norm_g"][0]),
                         _fm(inputs["mla_q_norm_g"][0]), _fm(inputs["mla_kv_norm_g"][0]),
                         _fm(inputs["diff_subln_g"][0]), np.zeros((128, 1), f32),
                         _fm(inputs["ffn_norm_g"][0])], axis=1)
    m["gT"] = np.ascontiguousarray(gT, f32)
    m["grow"] = np.ascontiguousarray(np.stack([np.asarray(inputs["ffn_norm_g"], f32)[0],
                                               np.asarray(inputs["final_norm_g"], f32)], axis=0))
    m["lam"] = np.ascontiguousarray(np.stack([np.asarray(inputs[k], f32)[0] for k in
                                              ("diff_lambda_q1", "diff_lambda_k1", "diff_lambda_q2", "diff_lambda_k2")]).reshape(1, 256))
    ident = np.eye(128, dtype=f32)
    ones = np.ones((128, 128), f32)
    Rd = np.zeros((128, 128), f32)
    for blk in range(2):
        for i in range(8):
            Rd[blk * 64 + i + 8, blk * 64 + i] = -1.0
            Rd[blk * 64 + i, blk * 64 + i + 8] = 1.0
    U = np.triu(np.ones((128, 128), f32), k=1)
    m["cb"] = np.ascontiguousarray(np.concatenate([ident, ones, Rd, U], axis=1).astype(bf))
    Rm = np.zeros((64, 64), f32)
    for i in range(32):
        Rm[i + 32, i] = -1.0
        Rm[i, i + 32] = 1.0
    m["rm"] = Rm.astype(bf)
    cf = np.zeros((128, 2 + NE), f32)
    for pp in range(128):
        i = pp % 64
        if i < 16:
            cf[pp, 0] = ROPE_THETA ** (-(i % 8) * 2.0 / 16.0) / (2 * math.pi)
        cf[pp, 1] = ROPE_THETA ** (-(i % 32) * 2.0 / 64.0) / (2 * math.pi)
    cf[:, 2:] = (np.arange(NE, dtype=f32) * CAP)[None, :]
    m["cf"] = cf
    return m


_NC_CACHE = {}
_DEV_CORES = 0


def kernel(**inputs):
    return _run(inputs)


def _run(inputs, stop_after=None, dump=()):
    inputs = {k: np.asarray(v) for k, v in inputs.items()}
    key = (stop_after, tuple(dump))
    if key not in _NC_CACHE:
        _NC_CACHE[key] = build_program(stop_after=stop_after, dump=dump)
    nc = _NC_CACHE[key]
    shared = host_shared(inputs)
    in_maps = []
    ncores = _DEV_CORES or NCORES
    for b in range(ncores):
        m = dict(shared)
        m["x"] = np.ascontiguousarray(inputs["x"][b], np.float32)
        m["mem"] = np.ascontiguousarray(inputs["mem"][b], np.float32)
        m["pos"] = np.ascontiguousarray(inputs["positions"][b], np.int32)[None, :]
        in_maps.append(m)
    res = run_bass_kernel_spmd(nc, in_maps, core_ids=list(range(ncores)))
    out = np.stack([np.asarray(r["out"]) for r in res.results], axis=0).astype(np.float32)
    if dump:
        return out, [{k: np.asarray(r[k]) for k in dump} for r in res.results]
    return out
```

```python
import math
from contextlib import ExitStack

import numpy as np
import ml_dtypes

import concourse.bass as bass
import concourse.mybir as mybir
from concourse.bass_utils import run_bass_kernel_spmd

F32 = mybir.dt.float32
BF16 = mybir.dt.bfloat16
I32 = mybir.dt.int32
ALU = mybir.AluOpType
AF = mybir.ActivationFunctionType
AX = mybir.AxisListType

NCORES = 8
S = 2048
D = 2048
NT = S // 128
DC = D // 128
D_IN = 8000
EPS = 1e-6


class Buf:
    __slots__ = ("name", "last_w", "readers")

    def __init__(self, name):
        self.name = name
        self.last_w = None
        self.readers = {}


class Prog:
    ENGINES = ("pe", "act", "dve", "pool", "sp")

    def __init__(self, nc, stack, n_lanes=12):
        self.nc = nc
        self.handles = {"pe": nc.tensor, "act": nc.scalar, "dve": nc.vector,
                        "pool": nc.gpsimd, "sp": nc.sync}
        self.thunks = {e: [] for e in self.ENGINES}
        self.sems = {}
        self.count = {}
        for e in self.ENGINES:
            self.sems[e] = stack.enter_context(nc.semaphore("c_" + e))
            self.count[e] = 0
        self.lanes = {}
        for q in ("sp", "pool"):
            ls = []
            for i in range(n_lanes):
                key = "l_%s%d" % (q, i)
                self.sems[key] = stack.enter_context(nc.semaphore(key))
                self.count[key] = 0
                ls.append(key)
            self.lanes[q] = ls
        self.lane_rr = {"sp": 0, "pool": 0}
        self.known = {e: {} for e in self.ENGINES}
        self.n_waits = 0
        self.events = {e: [] for e in self.ENGINES}

    def _collect(self, eng, reads, writes):
        need = {}

        def add(tok, same_ok):
            if tok is None:
                return
            k, v = tok
            if k == eng and same_ok:
                return
            if need.get(k, 0) < v:
                need[k] = v

        for b in reads:
            add(b.last_w, same_ok=(eng == "pe"))
        for b in writes:
            add(b.last_w, same_ok=True)
            for k, v in b.readers.items():
                add((k, v), same_ok=True)
        out = []
        kn = self.known[eng]
        for k, v in need.items():
            if kn.get(k, 0) >= v:
                continue
            kn[k] = v
            out.append((k, v))
        return out

    def _emit_waits(self, eng, waits):
        h = self.handles[eng]
        sems = self.sems
        for k, v in waits:
            self.n_waits += 1
            self.events[eng].append(("w", k, v))
            self.thunks[eng].append(lambda h=h, s=sems[k], v=v: h.wait_ge(s, v))

    def _update(self, tok, reads, writes):
        k, v = tok
        for b in reads:
            if b.readers.get(k, 0) < v:
                b.readers[k] = v
        for b in writes:
            b.last_w = tok
            b.readers = {}

    def op(self, eng, fn, reads=(), writes=(), sig=True):
        waits = self._collect(eng, reads, writes)
        self._emit_waits(eng, waits)
        h = self.handles[eng]
        sem = self.sems[eng]
        if sig:
            self.count[eng] += 1
            tok = (eng, self.count[eng])
            self.events[eng].append(("s", eng, 1))
            self.thunks[eng].append(lambda: fn(h).then_inc(sem, 1))
        else:
            tok = (eng, self.count[eng] + 1)
            self.thunks[eng].append(lambda: fn(h))
        self._update(tok, reads, writes)
        return tok

    def dma(self, q, fn, reads=(), writes=()):
        lanes = self.lanes[q]
        lane = lanes[self.lane_rr[q] % len(lanes)]
        self.lane_rr[q] += 1
        waits = self._collect(q, reads, writes)
        prev = self.count[lane]
        if prev > 0 and self.known[q].get(lane, 0) < prev:
            self.known[q][lane] = prev
            waits.append((lane, prev))
        self._emit_waits(q, waits)
        h = self.handles[q]
        sem = self.sems[lane]
        self.count[lane] += 16
        tok = (lane, self.count[lane])
        self.events[q].append(("s", lane, 16))
        self.thunks[q].append(lambda: fn(h).then_inc(sem, 16))
        self._update(tok, reads, writes)
        return tok

    def wait_all(self, eng, bufs):
        waits = self._collect(eng, bufs, ())
        self._emit_waits(eng, waits)

    def check_deadlock(self):
        val = {}
        pos = {e: 0 for e in self.ENGINES}
        progress = True
        while progress:
            progress = False
            for e in self.ENGINES:
                ev = self.events[e]
                i = pos[e]
                while i < len(ev):
                    kind, k, v = ev[i]
                    if kind == "w":
                        if val.get(k, 0) < v:
                            break
                    else:
                        val[k] = val.get(k, 0) + v
                    i += 1
                if i != pos[e]:
                    progress = True
                    pos[e] = i
        stuck = {e: (pos[e], len(self.events[e]), self.events[e][pos[e]]) for e in self.ENGINES
                 if pos[e] < len(self.events[e])}
        if stuck:
            raise RuntimeError("build-time deadlock check failed: %r" % (stuck,))

    def emit(self):
        self.check_deadlock()
        nc = self.nc
        with nc.Block() as block:
            @block.tensor
            def _(e):
                for t in self.thunks["pe"]:
                    t()

            @block.scalar
            def _(e):
                for t in self.thunks["act"]:
                    t()

            @block.vector
            def _(e):
                for t in self.thunks["dve"]:
                    t()

            @block.gpsimd
            def _(e):
                for t in self.thunks["pool"]:
                    t()

            @block.sync
            def _(e):
                for t in self.thunks["sp"]:
                    t()


class Ring:
    def __init__(self, tiles):
        self.tiles = tiles
        self.i = 0

    def next(self):
        t = self.tiles[self.i % len(self.tiles)]
        self.i += 1
        return t


class Tile:
    __slots__ = ("t", "b")

    def __init__(self, t, name):
        self.t = t
        self.b = Buf(name)

    def __getitem__(self, k):
        return self.t[k]


class Ctx:
    def __init__(self, nc, stack):
        self.nc = nc
        self.stack = stack
        self.p = Prog(nc, stack)
        self._n = 0

    def sb(self, name, shape, dt):
        self._n += 1
        return Tile(self.stack.enter_context(self.nc.sbuf_tensor("s%d_%s" % (self._n, name), list(shape), dt)), name)

    def ps(self, name, shape, dt=F32):
        self._n += 1
        return Tile(self.stack.enter_context(self.nc.psum_tensor("p%d_%s" % (self._n, name), list(shape), dt)), name)

    def dram(self, name, shape, dt, kind="Internal"):
        return Tile(self.nc.dram_tensor(name, list(shape), dt, kind=kind), name)

    def sb_ring(self, name, n, shape, dt):
        return Ring([self.sb("%s%d" % (name, i), shape, dt) for i in range(n)])

    def ps_ring(self, name, n, shape, dt=F32):
        return Ring([self.ps("%s%d" % (name, i), shape, dt) for i in range(n)])


H = 8
ROPE_THETA = 500000.0
CAP = 384
NE = 32
NSLOT = NE * CAP
DFF = 512
LAM_INIT = 0.8 - 0.6 * math.exp(-0.3 * 0)
PI = math.pi
NO_SCATTER = False


def bs(tiles):
    return [t.b for t in tiles]


class K(Ctx):
    def mm(self, out, lhsT, rhs, start, stop, R, W, sig=None):
        if sig is None:
            sig = stop
        self.p.op("pe", lambda h: h.matmul(out, lhsT, rhs, start=start, stop=stop), bs(R), bs(W), sig=sig)

    def tr(self, out, in_, ident, R, W, sig=True):
        self.p.op("pe", lambda h: h.transpose(out, in_, ident), bs(R), bs(W), sig=sig)

    def act(self, out, in_, func, R, W, bias=None, scale=None, accum=None):
        kw = {}
        if bias is not None:
            kw["bias"] = bias
        if scale is not None:
            kw["scale"] = scale
        if accum is not None:
            kw["accum_out"] = accum
        self.p.op("act", lambda h: h.activation(out, in_, func, **kw), bs(R), bs(W))

    def ts(self, eng, out, in0, s1, s2, op0, op1, R, W):
        if op1 is None:
            self.p.op(eng, lambda h: h.tensor_scalar(out, in0, s1, None, op0), bs(R), bs(W))
        else:
            self.p.op(eng, lambda h: h.tensor_scalar(out, in0, s1, s2, op0, op1), bs(R), bs(W))

    def tt(self, eng, out, in0, in1, op, R, W):
        self.p.op(eng, lambda h: h.tensor_tensor(out, in0, in1, op), bs(R), bs(W))

    def stt(self, out, in0, scalar, in1, op0, op1, R, W, accum=None):
        if accum is None:
            self.p.op("dve", lambda h: h.scalar_tensor_tensor(out, in0, scalar, in1, op0, op1), bs(R), bs(W))
        else:
            self.p.op("dve", lambda h: h.scalar_tensor_tensor(out, in0, scalar, in1, op0, op1, accum_out=accum),
                      bs(R), bs(W))

    def copy(self, eng, out, in_, R, W):
        if eng == "act":
            self.p.op("act", lambda h: h.copy(out, in_), bs(R), bs(W))
        else:
            self.p.op(eng, lambda h: h.tensor_copy(out, in_), bs(R), bs(W))

    def recip(self, out, in_, R, W):
        self.p.op("dve", lambda h: h.reciprocal(out, in_), bs(R), bs(W))

    def memset(self, eng, out, val, W):
        self.p.op(eng, lambda h: h.memset(out, val), [], bs(W))

    def dma(self, q, out, in_, R, W):
        self.p.dma(q, lambda h: h.dma_start(out=out, in_=in_), bs(R), bs(W))

    def barrier(self):
        p = self.p
        toks = [(e, p.count[e]) for e in p.ENGINES if p.count[e] > 0]
        for q in ("sp", "pool"):
            for lane in p.lanes[q]:
                if p.count[lane] > 0:
                    toks.append((lane, p.count[lane]))
        for e in p.ENGINES:
            w = []
            for k, v in toks:
                if k == e:
                    continue
                if p.known[e].get(k, 0) < v:
                    p.known[e][k] = v
                    w.append((k, v))
            p._emit_waits(e, w)


def rmsnorm_rstd(c, x_t, d, ss, junk, eps):
    c.act(junk[:, :d], x_t[:, :d], AF.Square, [x_t], [junk, ss], accum=ss[:, 0:1])
    c.ts("dve", ss[:, 1:2], ss[:, 0:1], 1.0 / d, eps, ALU.mult, ALU.add, [ss], [ss])
    c.p.op("act", lambda h: h.sqrt(ss[:, 3:4], ss[:, 1:2]), bs([ss]), bs([ss]))
    c.recip(ss[:, 2:3], ss[:, 3:4], [ss], [ss])


def norm_phase1(c, x_t, rg):
    junk = rg["junk"].next()
    ss = rg["ss"].next()
    c.act(junk[:, :D], x_t[:, :D], AF.Square, [x_t], [junk, ss], accum=ss[:, 0:1])
    return ss


def norm_phase2(c, x_t, ss, gT, goff, dstT, tcol, rg, ident):
    xs = rg["xs"].next()
    c.ts("dve", ss[:, 1:2], ss[:, 0:1], 1.0 / D, EPS, ALU.mult, ALU.add, [ss], [ss])
    c.p.op("act", lambda h: h.sqrt(ss[:, 3:4], ss[:, 1:2]), bs([ss]), bs([ss]))
    c.recip(ss[:, 2:3], ss[:, 3:4], [ss], [ss])
    half = D // 2
    c.ts("dve", xs[:, :half], x_t[:, :half], ss[:, 2:3], None, ALU.mult, None, [x_t, ss], [xs])
    c.act(xs[:, half:], x_t[:, half:], AF.Copy, [x_t, ss], [xs], scale=ss[:, 2:3])
    for g4 in range(DC // 4):
        pt = rg["pst"].next()
        for j in range(4):
            ch = g4 * 4 + j
            c.tr(pt[:, j * 128:(j + 1) * 128], xs[:, ch * 128:(ch + 1) * 128], ident[:, :], [xs, ident], [pt],
                 sig=(j == 3))
        for j in range(4):
            ch = g4 * 4 + j
            if j % 2 == 0:
                c.ts("dve", dstT[:, ch, tcol:tcol + 128], pt[:, j * 128:(j + 1) * 128],
                     gT[:, goff + ch:goff + ch + 1], None, ALU.mult, None, [pt, gT], [dstT])
            else:
                c.act(dstT[:, ch, tcol:tcol + 128], pt[:, j * 128:(j + 1) * 128], AF.Copy, [pt, gT], [dstT],
                      scale=gT[:, goff + ch:goff + ch + 1])


def norm_transpose_stream(c, n_tiles, load_fn, gT, goff, dstT, rg, ident, dst_fn=None, after_fn=None):
    cur = load_fn(0)
    ss = norm_phase1(c, cur, rg)
    for t in range(n_tiles):
        nxt = nss = None
        if t + 1 < n_tiles:
            nxt = load_fn(t + 1)
            nss = norm_phase1(c, nxt, rg)
        if dst_fn is None:
            norm_phase2(c, cur, ss, gT, goff, dstT, t * 128, rg, ident)
        else:
            dt_, tcol = dst_fn(t)
            norm_phase2(c, cur, ss, gT, goff, dt_, tcol, rg, ident)
        if after_fn is not None:
            after_fn(t)
        cur, ss = nxt, nss


def norm_transpose_tile(c, x_t, gT, goff, dstT, tcol, rg, ident):
    ss = norm_phase1(c, x_t, rg)
    norm_phase2(c, x_t, ss, gT, goff, dstT, tcol, rg, ident)


def rope_tables(c, pos_d, invf, col, nparts, Ct, St, tmp_i, tmp_f):
    n = nparts
    c.dma("sp", tmp_i[:n, :], pos_d[0:1, :].partition_broadcast(n), [pos_d], [tmp_i])
    c.copy("dve", tmp_f[:n, :], tmp_i[:n, :], [tmp_i], [tmp_f])
    for dst, phase in ((St, 0.0), (Ct, 0.25)):
        c.ts("dve", dst[:n, :], tmp_f[:n, :], invf[:n, col:col + 1], phase, ALU.mult, ALU.add, [tmp_f, invf], [dst])
        c.copy("dve", tmp_i[:n, :], dst[:n, :], [dst], [tmp_i])
        c.copy("dve", Ct[:n, :] if dst is St else tmp_f[:n, :], tmp_i[:n, :], [tmp_i], [Ct if dst is St else tmp_f])
        kf = Ct if dst is St else tmp_f
        c.tt("dve", dst[:n, :], dst[:n, :], kf[:n, :], ALU.subtract, [dst, kf], [dst])
        for _ in range(2):
            c.stt(dst[:n, :], dst[:n, :], 0.5, dst[:n, :], ALU.is_gt, ALU.subtract, [dst], [dst])
        c.act(dst[:n, :], dst[:n, :], AF.Sin, [dst], [dst], scale=2 * PI)


def rope_evac(c, ps, npart, blk, Rm, Ct, St, rg, dst_ap, dst_tile, defer=None, kfull=False):
    qs = rg["qs"].next()
    ncp = 128 if kfull else npart
    c.copy("act", qs[:ncp, :], ps[:ncp, :], [ps], [qs])
    if kfull:
        _rope_rest(c, qs, npart, blk, Rm, Ct, St, rg, dst_ap, dst_tile, kfull=True)
        return
    if defer is not None:
        defer.append(lambda: _rope_rest(c, qs, npart, blk, Rm, Ct, St, rg, dst_ap, dst_tile))
        return
    _rope_rest(c, qs, npart, blk, Rm, Ct, St, rg, dst_ap, dst_tile)


def _rope_rest(c, qs, npart, blk, Rm, Ct, St, rg, dst_ap, dst_tile, kfull=False):
    pr = rg["psr"].next()
    nk = 128 if kfull else npart
    c.mm(pr[:nk, :], Rm[:nk, :nk], qs[:nk, :], True, True, [Rm, qs], [pr])
    t1 = rg["t1"].next()
    t2 = rg["t2"].next()
    cs = slice(blk * 512, (blk + 1) * 512)
    c.tt("dve", t1[:npart, :], qs[:npart, :], Ct[:npart, cs], ALU.mult, [qs, Ct], [t1])
    c.tt("dve", t2[:npart, :], pr[:npart, :], St[:npart, cs], ALU.mult, [pr, St], [t2])
    c.tt("pool", dst_ap, t1[:npart, :], t2[:npart, :], ALU.add, [t1, t2], [dst_tile])


def build_program(stop_after=None, dump=()):
    nc = bass.Bass("TRN2", target_bir_lowering=False)
    outer = ExitStack()
    with outer:
        c = K(nc, outer)
        p = c.p

        def dram(name, shape, dt, kind=None):
            if kind is None:
                kind = "ExternalOutput" if name in dump else "Internal"
            return c.dram(name, shape, dt, kind=kind)

        x_d = dram("x", [S, D], F32, "ExternalInput")
        mem_d = dram("mem", [256, D], F32, "ExternalInput")
        pos_d = dram("pos", [1, S], I32, "ExternalInput")
        w_in_d = dram("w_in", [D, D_IN], F32, "ExternalInput")
        w_od_d = dram("w_o_diff", [1024, D], F32, "ExternalInput")
        w_uq_d = dram("w_uq", [512, 1536], F32, "ExternalInput")
        w_ukv_d = dram("w_ukv", [256, 2048], F32, "ExternalInput")
        w_om_d = dram("w_o_mla", [1024, D], F32, "ExternalInput")
        w_out_d = dram("w_out", [D, D], F32, "ExternalInput")
        w_cq_d = dram("w_cq", [D, 512], F32, "ExternalInput")
        w_ckv_d = dram("w_ckv", [D, 1024], F32, "ExternalInput")
        w_co_d = dram("w_co", [512, D], F32, "ExternalInput")
        w_r_d = dram("w_router", [D, 36], F32, "ExternalInput")
        b_r_d = dram("b_router", [1, 36], F32, "ExternalInput")
        w_eg_d = dram("w_expert_gate", [NE, D, DFF], F32, "ExternalInput")
        w_eu_d = dram("w_expert_up", [NE, D, DFF], F32, "ExternalInput")
        w_ed_d = dram("w_expert_down", [NE, DFF, D], F32, "ExternalInput")
        gT_d = dram("gT", [128, 4 * DC + 8], F32, "ExternalInput")
        grow_d = dram("grow", [2, D], F32, "ExternalInput")
        lam_d = dram("lam", [1, 256], F32, "ExternalInput")
        cb_d = dram("cb", [128, 4 * 128], BF16, "ExternalInput")
        rm_d = dram("rm", [128, 128], BF16, "ExternalInput")
        cf_d = dram("cf", [128, 2 + NE], F32, "ExternalInput")
        out_d = dram("out", [S, D], F32, "ExternalOutput")

        qT_d = dram("qT_s", [H, 128, S], BF16)
        kT_d = dram("kT_s", [H, 128, S], BF16)
        v_d = dram("v_s", [S, 1024], BF16)
        cq_d = dram("cq_s", [4, 128, S], F32)
        ckv_d = dram("ckv_s", [2, 128, S], F32)
        kpe_d = dram("kpe_s", [64, S], BF16)
        ga_d = dram("ga_s", [DC, 128, S], BF16)
        gb_d = dram("gb_s", [DC, 128, S], BF16)
        oa_d = dram("oa_s", [H, 128, S], BF16)
        ob_d = dram("ob_s", [H, 128, S], BF16)
        h1_d = dram("h1_s", [S, D], F32)
        h2_d = dram("h2_s", [S, D], F32)
        xg_d = dram("xg_s", [NSLOT + 128, D], BF16)
        y_d = dram("y_s", [NSLOT + 128, D], BF16)

        cb = c.sb("cb", [128, 4 * 128], BF16)
        rm = c.sb("rm", [128, 128], BF16)
        cf = c.sb("cf", [128, 2 + NE], F32)
        gT = c.sb("gTs", [128, 4 * DC + 8], F32)
        c.dma("sp", cb[:, :], cb_d[:, :], [cb_d], [cb])
        c.dma("sp", rm[:, :], rm_d[:, :], [rm_d], [rm])
        c.dma("sp", cf[:, :], cf_d[:, :], [cf_d], [cf])
        c.dma("sp", gT[:, :], gT_d[:, :], [gT_d], [gT])
        ident = Tile(cb.t[:, 0:128], "ident"); ident.b = cb.b
        ones = Tile(cb.t[:, 128:256], "ones"); ones.b = cb.b
        Rd = Tile(cb.t[:, 256:384], "Rd"); Rd.b = cb.b
        Utri = Tile(cb.t[:, 384:512], "Utri"); Utri.b = cb.b
        slot_i = c.sb("slot_i", [128, NT, 2], I32)
        wts = c.sb("wts", [128, NT, 2], F32)
        zrow = c.sb("zrow", [128, D], BF16)
        c.memset("pool", zrow[:, :], 0.0, [zrow])

        final_outs = []
        bc_reg = nc.gpsimd.alloc_register("bc_reg")
        p.thunks["pool"].insert(0, lambda: nc.gpsimd.reg_mov(bc_reg, NSLOT + 127))

        def finish():
            p.wait_all("sp", [b for b in final_outs])
            p.emit()

        with ExitStack() as sa:
            c.stack = sa
            xnT = c.sb("xnT", [128, DC, S], BF16)
            with ExitStack() as sa1:
                c.stack = sa1
                rg = {"junk": c.sb_ring("junk", 1, [128, D], BF16), "ss": c.sb_ring("ss", 4, [128, 4], F32),
                      "xs": c.sb_ring("xs", 2, [128, D], BF16), "pst": c.ps_ring("pst", 2, [128, 512], BF16)}
                xin = c.sb_ring("xin", 3, [128, D], F32)

                def load_x(t):
                    xt = xin.next()
                    c.dma("sp", xt[:, :], x_d[t * 128:(t + 1) * 128, :], [x_d], [xt])
                    return xt

                norm_transpose_stream(c, NT, load_x, gT, 0, xnT, rg, ident)
                c.barrier()
            c.stack = sa
            if "xnT_dbg" in dump:
                dbg = dram("xnT_dbg", [128, DC, S], BF16)
                c.dma("sp", dbg[:, :, :], xnT[:, :, :], [xnT], [dbg])
                final_outs.append(dbg.b)
            Cd = c.sb("Cd", [128, S], F32)
            Sd = c.sb("Sd", [128, S], F32)
            Cm = c.sb("Cm", [64, S], F32)
            Sm = c.sb("Sm", [64, S], F32)
            with ExitStack() as sa2:
                c.stack = sa2
                tmp_i = c.sb("tmp_i", [128, S], I32)
                tmp_f = c.sb("tmp_f", [128, S], F32)
                rope_tables(c, pos_d, cf, 0, 128, Cd, Sd, tmp_i, tmp_f)
                rope_tables(c, pos_d, cf, 1, 64, Cm, Sm, tmp_i, tmp_f)
                c.barrier()
            c.stack = sa
            slabs = c.sb_ring("wslab", 3, [128, DC, 512], BF16)
            rg = {"qs": c.sb_ring("qs", 3, [128, 512], BF16), "psr": c.ps_ring("psr", 2, [128, 512], F32),
                  "t1": c.sb_ring("t1", 2, [128, 512], F32), "t2": c.sb_ring("t2", 2, [128, 512], F32)}
            psA = c.ps_ring("psA", 4, [128, 512], F32)
            stg = c.sb_ring("stgA", 4, [128, 512], BF16)
            stgf = c.sb_ring("stgAf", 3, [128, 512], F32)

            def load_slab(c0, ncols):
                sl = slabs.next()
                c.dma("pool", sl[:, :, :ncols], w_in_d[:, c0:c0 + ncols].rearrange("(kc p) n -> p kc n", p=128),
                      [w_in_d], [sl])
                return sl

            def proj_fm(sl, off, width, blk):
                ps = psA.next()
                for kc in range(DC):
                    c.mm(ps[:width, :], sl[:, kc, off:off + width], xnT[:, kc, blk * 512:(blk + 1) * 512],
                         kc == 0, kc == DC - 1, [sl, xnT], [ps])
                return ps

            for sidx in range(2):
                sl = load_slab(2048 + sidx * 512, 512)
                for t in range(NT):
                    ps = psA.next()
                    for kc in range(DC):
                        c.mm(ps[:, :], xnT[:, kc, t * 128:(t + 1) * 128], sl[:, kc, :], kc == 0, kc == DC - 1,
                             [sl, xnT], [ps])
                    st = stg.next()
                    c.copy("act" if t % 2 == 0 else "dve", st[:, :], ps[:, :], [ps], [st])
                    c.dma("sp", v_d[t * 128:(t + 1) * 128, sidx * 512:(sidx + 1) * 512], st[:, :], [st], [v_d])
            pend = []

            def flush_pending():
                while pend:
                    pend.pop(0)()

            for which, dst_d in ((0, qT_d), (1, kT_d)):
                for sidx in range(2):
                    sl = load_slab(which * 1024 + sidx * 512, 512)
                    for j in range(4):
                        hh = sidx * 4 + j
                        for blk in range(4):
                            ps = proj_fm(sl, j * 128, 128, blk)
                            flush_pending()
                            st = stg.next()
                            todo = []
                            rope_evac(c, ps, 128, blk, Rd, Cd, Sd, rg, st[:, :], st, defer=todo)

                            def tail(todo=todo, st=st, dst_d=dst_d, hh=hh, blk=blk):
                                todo[0]()
                                c.dma("sp", dst_d[hh, :, blk * 512:(blk + 1) * 512], st[:, :], [st], [dst_d])
                            pend.append(tail)
            flush_pending()
            sl = load_slab(3072, 512)
            for j in range(4):
                for blk in range(4):
                    ps = proj_fm(sl, j * 128, 128, blk)
                    st = stgf.next()
                    c.copy("act" if blk % 2 == 0 else "dve", st[:, :], ps[:, :], [ps], [st])
                    c.dma("sp", cq_d[j, :, blk * 512:(blk + 1) * 512], st[:, :], [st], [cq_d])
            sl = load_slab(3584, 320)
            for j in range(2):
                for blk in range(4):
                    ps = proj_fm(sl, j * 128, 128, blk)
                    st = stgf.next()
                    c.copy("act" if blk % 2 == 0 else "dve", st[:, :], ps[:, :], [ps], [st])
                    c.dma("sp", ckv_d[j, :, blk * 512:(blk + 1) * 512], st[:, :], [st], [ckv_d])
            for blk in range(4):
                ps = proj_fm(sl, 256, 64, blk)
                st = stg.next()
                rope_evac(c, ps, 64, blk, rm, Cm, Sm, rg, st[:64, :], st)
                c.dma("sp", kpe_d[:, blk * 512:(blk + 1) * 512], st[:64, :], [st], [kpe_d])
            for which, dst_d in ((0, ga_d), (1, gb_d)):
                for sidx in range(4):
                    sl = load_slab(3904 + which * 2048 + sidx * 512, 512)
                    for j in range(4):
                        ch = sidx * 4 + j
                        for blk in range(4):
                            ps = proj_fm(sl, j * 128, 128, blk)
                            st = stg.next()
                            c.act(st[:, :], ps[:, :], AF.Sigmoid, [ps], [st])
                            c.dma("sp", dst_d[ch, :, blk * 512:(blk + 1) * 512], st[:, :], [st], [dst_d])
            c.barrier()
        c.stack = outer
        for nm, tl in (("qT_s", qT_d), ("kT_s", kT_d), ("v_s", v_d), ("cq_s", cq_d), ("ckv_s", ckv_d),
                       ("kpe_s", kpe_d), ("ga_s", ga_d), ("gb_s", gb_d)):
            if nm in dump:
                final_outs.append(tl.b)
        if stop_after == "A":
            final_outs.append(out_d.b)
            c.dma("sp", out_d[0:128, :], x_d[0:128, :], [x_d], [out_d])
            finish()
            return nc

        def stop_here():
            final_outs.append(out_d.b)
            c.dma("sp", out_d[0:128, :], x_d[0:128, :], [x_d], [out_d])
            finish()

        def attn_core(nk_tiles, qblk, score_fn, v_fn, ps_s, e_ring, acc_o, acc_d, scale):
            Es = {}

            def score(kt):
                ps = ps_s.next()
                score_fn(ps, kt)
                E = e_ring.next()
                c.act(E[:, :], ps[:, :], AF.Exp, [ps], [E], scale=scale)
                Es[kt] = E

            score(0)
            for kt in range(nk_tiles):
                if kt + 1 < nk_tiles:
                    score(kt + 1)
                E = Es.pop(kt)
                vap, vt = v_fn(kt)
                last = kt == nk_tiles - 1
                c.mm(acc_o[:, :], vap, E[:, :], kt == 0, last, [vt, E], [acc_o], sig=last)
                c.mm(acc_d[:, :], ones[:, :], E[:, :], kt == 0, last, [ones, E], [acc_d], sig=last)

        def attn_stream(calls, ps_s, e_ring, ps_o, ps_d, scale, look):
            flat = [(ci, kt) for ci, cl in enumerate(calls) for kt in range(0, cl["nk"], 2)]
            Es = {}
            state = {"nxt": 0}

            def score(i):
                ci, kt = flat[i]
                cl = calls[ci]
                if kt == 0 and cl.get("pre_fn") is not None:
                    cl["pre_fn"]()
                ps = ps_s.next()
                cl["score_fn"](ps, 0, kt)
                cl["score_fn"](ps, 512, kt + 1)
                E = e_ring.next()
                c.act(E[:, :], ps[:, :], AF.Exp, [ps], [E], scale=scale)
                Es[i] = E

            def fill():
                if state["nxt"] < len(flat):
                    score(state["nxt"])
                    state["nxt"] += 1

            deferred = []
            for _ in range(look):
                fill()
            for i, (ci, kt) in enumerate(flat):
                fill()
                for dfr in list(deferred):
                    dfr[0] -= 1
                    if dfr[0] <= 0:
                        deferred.remove(dfr)
                        dfr[1]()
                cl = calls[ci]
                if kt == 0:
                    cl["acc_o"] = ps_o.next()
                    cl["acc_d"] = cl["acc_d_tile"] if cl.get("acc_d_tile") is not None else ps_d.next()
                acc_o, acc_d = cl["acc_o"], cl["acc_d"]
                E = Es.pop(i)
                for j in range(2):
                    vap, vt = cl["v_fn"](kt + j)
                    first = (kt + j == 0)
                    last = (kt + j == cl["nk"] - 1)
                    c.mm(acc_o[:, :], vap, E[:, j * 512:(j + 1) * 512], first, last, [vt, E], [acc_o], sig=last)
                    c.mm(acc_d[:, :], ones[:, :], E[:, j * 512:(j + 1) * 512], first, last, [ones, E], [acc_d], sig=last)
                if kt + 2 >= cl["nk"]:
                    tail = cl["post_fn"](acc_o, acc_d)
                    if tail is not None:
                        deferred.append([3, tail])
            for dfr in deferred:
                dfr[1]()

        with ExitStack() as sb_:
            c.stack = sb_
            lamt = c.sb("lamt", [128, 256], F32)
            lams = c.sb("lams", [128, 8], F32)
            ljunk = c.sb("ljunk", [128, 64], F32)
            c.dma("sp", lamt[:, :], lam_d[0:1, :].partition_broadcast(128), [lam_d], [lamt])
            for i in range(2):
                c.tt("dve", ljunk[:, :], lamt[:, i * 128:i * 128 + 64], lamt[:, i * 128 + 64:i * 128 + 128], ALU.mult,
                     [lamt], [ljunk])
                c.p.op("dve", lambda h, i=i: h.reduce_sum(lams[:, i:i + 1], ljunk[:, :], axis=AX.X), bs([ljunk]), bs([lams]))
                c.act(lams[:, 2 + i:3 + i], lams[:, i:i + 1], AF.Exp, [lams], [lams])
            c.tt("dve", lams[:, 4:5], lams[:, 3:4], lams[:, 2:3], ALU.subtract, [lams], [lams])
            c.ts("dve", lams[:, 5:6], lams[:, 4:5], -LAM_INIT, None, ALU.add, None, [lams], [lams])
            neglam = lams[:, 5:6]

            zf = {"i": 0}

            def zero_fill(n):
                for _ in range(n):
                    i = zf["i"]
                    if i < NSLOT // 128:
                        c.dma("sp", xg_d[i * 128:(i + 1) * 128, :], zrow[:, :], [zrow], [xg_d])
                    elif i == NSLOT // 128:
                        c.dma("sp", y_d[NSLOT:NSLOT + 128, :], zrow[:, :], [zrow], [y_d])
                    zf["i"] = i + 1

            qh_r = c.sb_ring("qh", 2, [128, S], BF16)
            kh_r = c.sb_ring("kh", 2, [128, 2, S], BF16)
            for _kt in kh_r.tiles:
                c.memset("pool", _kt[:, :, :], 0.0, [_kt])
            vh_r = c.sb_ring("vh", 2, [128, NT, 128], BF16)
            e_ring = c.sb_ring("E", 4, [128, 1024], BF16)
            ps_s = c.ps_ring("ps_s", 2, [128, 1024], F32)
            ps_o = c.ps_ring("ps_o", 2, [128, 512], F32)
            ps_d = c.ps_ring("ps_d", 2, [128, 512], F32)
            rec_r = c.sb_ring("rec", 3, [128, 512], F32)
            oc_r = c.sb_ring("oc", 6, [128, 512], F32)
            sq_r = c.sb_ring("sq", 3, [128, 512], BF16)
            ost_r = c.sb_ring("ost", 3, [128, 512], BF16)
            heads = {}

            def load_head(hh):
                if hh >= H or hh in heads:
                    return
                qh, kh, vh = qh_r.next(), kh_r.next(), vh_r.next()
                c.dma("sp", qh[:, :], qT_d[hh, :, :], [qT_d], [qh])
                c.dma("sp", kh[0:64, 0, :], kT_d[hh, 0:64, :], [kT_d], [kh])
                c.dma("sp", kh[64:128, 1, :], kT_d[hh, 64:128, :], [kT_d], [kh])
                c.dma("sp", vh[:, :, :], v_d[:, hh * 128:(hh + 1) * 128].rearrange("(kt p) d -> p kt d", p=128),
                      [v_d], [vh])
                heads[hh] = (qh, kh, vh)

            calls = []
            for hh in range(H):
                for qb in range(4):
                    qs_ = slice(qb * 512, (qb + 1) * 512)
                    pair = {}
                    for comp in range(2):
                        r0 = comp * 64

                        def pre_fn(hh=hh, qb=qb, comp=comp):
                            if comp == 0 and qb == 0:
                                load_head(hh)
                            if comp == 0 and qb == 1:
                                load_head(hh + 1)
                            if comp == 0:
                                zero_fill(4)

                        def score_fn(ps, off, kt, comp=comp, hh=hh, qs_=qs_):
                            qh, kh, vh = heads[hh]
                            c.mm(ps[:, off:off + 512], kh[:, comp, kt * 128:(kt + 1) * 128], qh[:, qs_], True, True,
                                 [kh, qh], [ps], sig=(off == 512))

                        def v_fn(kt, hh=hh):
                            vh = heads[hh][2]
                            return vh[:, kt, :], vh

                        def post_fn(acc_o, acc_d, hh=hh, qs_=qs_, comp=comp, pair=pair):
                            rec = rec_r.next()
                            c.recip(rec[:, :], acc_d[:, :], [acc_d], [rec])
                            oc = oc_r.next()
                            c.tt("dve", oc[:, :], acc_o[:, :], rec[:, :], ALU.mult, [acc_o, rec], [oc])
                            pair[comp] = oc
                            if comp == 0:
                                return
                            o = oc_r.next()
                            c.stt(o[:, :], pair[1][:, :], neglam, pair[0][:, :], ALU.mult, ALU.add,
                                  [pair[0], pair[1], lams], [o])
                            sq = sq_r.next()
                            c.tt("pool", sq[:, :], o[:, :], o[:, :], ALU.mult, [o], [sq])
                            return lambda: subln_tail(o, sq, hh, qs_)

                        def subln_tail(o, sq, hh, qs_):
                            pn = ps_d.tiles[1]
                            c.mm(pn[:, :], ones[:, :], sq[:, :], True, True, [ones, sq], [pn])
                            rec = rec_r.next()
                            c.ts("dve", rec[:, :], pn[:, :], 1.0 / 128.0, 1e-5, ALU.mult, ALU.add, [pn], [rec])
                            c.act(rec[:, :], rec[:, :], AF.Ln, [rec], [rec])
                            c.act(rec[:, :], rec[:, :], AF.Exp, [rec], [rec], scale=-0.5)
                            c.tt("pool", o[:, :], o[:, :], rec[:, :], ALU.mult, [o, rec], [o])
                            ost = ost_r.next()
                            c.ts("pool", ost[:, :], o[:, :], gT[:, 54:55], 1.0 - LAM_INIT, ALU.mult, ALU.mult, [o, gT], [ost])
                            c.dma("sp", oa_d[hh, :, qs_], ost[:, :], [ost], [oa_d])

                        calls.append({"nk": NT, "score_fn": score_fn, "v_fn": v_fn, "post_fn": post_fn, "pre_fn": pre_fn,
                                      "acc_d_tile": ps_d.tiles[comp]})
            attn_stream(calls, ps_s, e_ring, ps_o, ps_d, 0.125, 1)
            c.barrier()
        c.stack = outer
        if "oa_s" in dump:
            final_outs.append(oa_d.b)
        if stop_after == "B":
            stop_here()
            return nc

        with ExitStack() as sc_:
            c.stack = sc_
            cqn = c.sb("cqn", [128, 4, S], BF16)
            ckvn = c.sb("ckvn", [128, 2, S], BF16)
            kpe = c.sb("kpe", [128, S], BF16)
            c.memset("pool", kpe[:, :], 0.0, [kpe])
            Cm = c.sb("Cm2", [64, S], F32)
            Sm = c.sb("Sm2", [64, S], F32)
            c.dma("sp", kpe[0:64, :], kpe_d[:, :], [kpe_d], [kpe])
            with ExitStack() as sc1:
                c.stack = sc1
                tmp_i = c.sb("tmp_i2", [64, S], I32)
                tmp_f = c.sb("tmp_f2", [64, S], F32)
                rope_tables(c, pos_d, cf, 1, 64, Cm, Sm, tmp_i, tmp_f)
                c.barrier()
            with ExitStack() as sc2:
                c.stack = sc2
                cf32 = c.sb("cf32", [128, 4, S], F32)
                sq_r = c.sb_ring("sqc", 2, [128, 512], BF16)
                ps_n = c.ps_ring("ps_nc", 2, [128, 512], F32)
                rec_r = c.sb_ring("recc", 2, [128, 512], F32)
                for src_d, nch, dst, goff in ((cq_d, 4, cqn, 48), (ckv_d, 2, ckvn, 52)):
                    for ch in range(nch):
                        c.dma("sp", cf32[:, ch, :], src_d[ch, :, :], [src_d], [cf32])
                    for blk in range(4):
                        cs = slice(blk * 512, (blk + 1) * 512)
                        pn = ps_n.next()
                        for ch in range(nch):
                            sq = sq_r.next()
                            c.act(sq[:, :], cf32[:, ch, cs], AF.Square, [cf32], [sq])
                            c.mm(pn[:, :], ones[:, :], sq[:, :], ch == 0, ch == nch - 1, [ones, sq], [pn], sig=True)
                        rec = rec_r.next()
                        c.ts("dve", rec[:, :], pn[:, :], 1.0 / (nch * 128), EPS, ALU.mult, ALU.add, [pn], [rec])
                        c.p.op("act", lambda h, rec=rec: h.sqrt(rec[:, :], rec[:, :]), bs([rec]), bs([rec]))
                        c.recip(rec[:, :], rec[:, :], [rec], [rec])
                        for ch in range(nch):
                            c.stt(dst[:, ch, cs], cf32[:, ch, cs], gT[:, goff + ch:goff + ch + 1], rec[:, :],
                                  ALU.mult, ALU.mult, [cf32, gT, rec], [dst])
                c.barrier()
            if stop_after == "C1":
                dbg = dram("cqn_dbg", [128, 4, S], BF16)
                c.dma("sp", dbg[:, :, :], cqn[:, :, :], [cqn], [dbg])
                final_outs.append(dbg.b)
                stop_here()
                return nc
            c.stack = sc_
            psA = c.ps_ring("psAC", 2, [128, 512], F32)
            rg = {"qs": c.sb_ring("qsC", 2, [128, 512], BF16), "psr": psA,
                  "t1": c.sb_ring("t1C", 2, [128, 512], F32), "t2": c.sb_ring("t2C", 2, [128, 512], F32)}
            slq_r = c.sb_ring("slq", 2, [128, 4, 256], BF16)
            for _st in slq_r.tiles:
                c.memset("pool", _st[:, :, :], 0.0, [_st])
            slkv_r = c.sb_ring("slkv", 2, [128, 2, 256], BF16)
            qn_r = c.sb_ring("qn", 2, [128, S], BF16)
            qp_r = c.sb_ring("qp", 2, [128, S], BF16)
            for _qt in qp_r.tiles:
                c.memset("pool", _qt[:, :], 0.0, [_qt])
            kn_r = c.sb_ring("kn", 2, [128, S], BF16)
            vh_r = c.sb_ring("vhC", 2, [128, NT, 128], BF16)
            ps_s = c.ps_ring("ps_sC", 2, [128, 1024], F32)
            ps_o = c.ps_ring("ps_oC", 1, [128, 512], F32)
            ps_d = c.ps_ring("ps_dC", 1, [128, 512], F32)
            rec_r = c.sb_ring("recC", 2, [128, 512], F32)
            ost_r = c.sb_ring("ostC", 3, [128, 512], BF16)
            e_ring = c.sb_ring("EC", 4, [128, 1024], BF16)
            heads = {}

            def prep_head(hh):
                if hh >= H or hh in heads:
                    return
                slq, slkv = slq_r.next(), slkv_r.next()
                c.dma("pool", slq[:, :, 0:192], w_uq_d[:, hh * 192:(hh + 1) * 192].rearrange("(kc p) n -> p kc n", p=128),
                      [w_uq_d], [slq])
                c.dma("pool", slkv[:, :, :], w_ukv_d[:, hh * 256:(hh + 1) * 256].rearrange("(kc p) n -> p kc n", p=128),
                      [w_ukv_d], [slkv])
                qn, qp, kn, vh = qn_r.next(), qp_r.next(), kn_r.next(), vh_r.next()
                heads[hh] = (qn, qp, kn, vh)
                for blk in range(4):
                    cs = slice(blk * 512, (blk + 1) * 512)
                    ps = psA.next()
                    for kc in range(4):
                        c.mm(ps[:, :], slq[:, kc, 0:128], cqn[:, kc, cs], kc == 0, kc == 3, [slq, cqn], [ps])
                    c.copy("dve", qn[:, cs], ps[:, :], [ps], [qn])
                    ps = psA.next()
                    for kc in range(4):
                        c.mm(ps[:, :], slq[:, kc, 128:256], cqn[:, kc, cs], kc == 0, kc == 3, [slq, cqn], [ps])
                    rope_evac(c, ps, 64, blk, rm, Cm, Sm, rg, qp[:64, cs], qp, kfull=True)
                    ps = psA.next()
                    for kc in range(2):
                        c.mm(ps[:, :], slkv[:, kc, 0:128], ckvn[:, kc, cs], kc == 0, kc == 1, [slkv, ckvn], [ps])
                    c.copy("dve", kn[:, cs], ps[:, :], [ps], [kn])
                for t4 in range(NT // 4):
                    ps = psA.next()
                    for j in range(4):
                        t = t4 * 4 + j
                        for kc in range(2):
                            c.mm(ps[:, j * 128:(j + 1) * 128], ckvn[:, kc, t * 128:(t + 1) * 128], slkv[:, kc, 128:256],
                                 kc == 0, kc == 1, [slkv, ckvn], [ps], sig=(kc == 1 and j == 3))
                    c.copy("dve", vh[:, t4 * 4:(t4 + 1) * 4, :],
                           ps[:, :].rearrange("p (a b) -> p a b", a=4), [ps], [vh])

            calls = []
            for hh in range(H):
                for qb in range(4):
                    qs_ = slice(qb * 512, (qb + 1) * 512)

                    def pre_fn(hh=hh, qb=qb):
                        if qb == 0:
                            prep_head(hh)
                        if qb == 2:
                            prep_head(hh + 1)

                    def score_fn(ps, off, kt, hh=hh, qs_=qs_):
                        qn, qp, kn, vh = heads[hh]
                        ks = slice(kt * 128, (kt + 1) * 128)
                        c.mm(ps[:, off:off + 512], kn[:, ks], qn[:, qs_], True, False, [kn, qn], [ps], sig=False)
                        c.mm(ps[:, off:off + 512], kpe[:, ks], qp[:, qs_], False, True, [kpe, qp], [ps], sig=(off == 512))

                    def v_fn(kt, hh=hh):
                        vh = heads[hh][3]
                        return vh[:, kt, :], vh

                    def post_fn(acc_o, acc_d, hh=hh, qs_=qs_):
                        rec = rec_r.next()
                        c.recip(rec[:, :], acc_d[:, :], [acc_d], [rec])
                        ost = ost_r.next()
                        c.tt("dve", ost[:, :], acc_o[:, :], rec[:, :], ALU.mult, [acc_o, rec], [ost])
                        c.dma("sp", ob_d[hh, :, qs_], ost[:, :], [ost], [ob_d])

                    calls.append({"nk": NT, "score_fn": score_fn, "v_fn": v_fn, "post_fn": post_fn, "pre_fn": pre_fn})
            attn_stream(calls, ps_s, e_ring, ps_o, ps_d, 192.0 ** -0.5, 1)
            c.barrier()
        c.stack = outer
        if "ob_s" in dump:
            final_outs.append(ob_d.b)
        if stop_after == "C":
            stop_here()
            return nc

        with ExitStack() as sd_:
            c.stack = sd_
            mergedT = c.sb("mergedT", [128, DC, S], BF16)
            with ExitStack() as sd1:
                c.stack = sd1
                oaT = c.sb("oaT", [128, H, S], BF16)
                obT = c.sb("obT", [128, H, S], BF16)
                for hh in range(H):
                    c.dma("sp", oaT[:, hh, :], oa_d[hh, :, :], [oa_d], [oaT])
                    c.dma("sp", obT[:, hh, :], ob_d[hh, :, :], [ob_d], [obT])
                sla_r = c.sb_ring("sla", 2, [128, H, 512], BF16)
                slb_r = c.sb_ring("slb", 2, [128, H, 512], BF16)
                ga_r = c.sb_ring("gaT", 2, [128, S], BF16)
                gb_r = c.sb_ring("gbT", 2, [128, S], BF16)
                ps_a = c.ps_ring("ps_a", 3, [128, 512], F32)
                ps_b = c.ps_ring("ps_b", 3, [128, 512], F32)
                t1_r = c.sb_ring("t1D", 2, [128, 512], F32)
                t2_r = c.sb_ring("t2D", 2, [128, 512], F32)
                for c4 in range(4):
                    sla, slb = sla_r.next(), slb_r.next()
                    c.dma("pool", sla[:, :, :], w_od_d[:, c4 * 512:(c4 + 1) * 512].rearrange("(kc p) n -> p kc n", p=128),
                          [w_od_d], [sla])
                    c.dma("pool", slb[:, :, :], w_om_d[:, c4 * 512:(c4 + 1) * 512].rearrange("(kc p) n -> p kc n", p=128),
                          [w_om_d], [slb])
                    for j in range(4):
                        ch = c4 * 4 + j
                        ga, gb = ga_r.next(), gb_r.next()
                        c.dma("sp", ga[:, :], ga_d[ch, :, :], [ga_d], [ga])
                        c.dma("sp", gb[:, :], gb_d[ch, :, :], [gb_d], [gb])
                        for blk in range(4):
                            cs = slice(blk * 512, (blk + 1) * 512)
                            pa, pb = ps_a.next(), ps_b.next()
                            for kc in range(H):
                                c.mm(pa[:, :], sla[:, kc, j * 128:(j + 1) * 128], oaT[:, kc, cs], kc == 0, kc == H - 1,
                                     [sla, oaT], [pa])
                            for kc in range(H):
                                c.mm(pb[:, :], slb[:, kc, j * 128:(j + 1) * 128], obT[:, kc, cs], kc == 0, kc == H - 1,
                                     [slb, obT], [pb])
                            t1, t2 = t1_r.next(), t2_r.next()
                            c.tt("dve", t1[:, :], pa[:, :], ga[:, cs], ALU.mult, [pa, ga], [t1])
                            c.tt("dve", t2[:, :], pb[:, :], gb[:, cs], ALU.mult, [pb, gb], [t2])
                            c.tt("pool", mergedT[:, ch, cs], t1[:, :], t2[:, :], ALU.add, [t1, t2], [mergedT])
                c.barrier()
            c.stack = sd_
            wout = c.sb("wout", [128, DC, D], BF16)
            for nb in range(4):
                c.dma("pool", wout[:, :, nb * 512:(nb + 1) * 512],
                      w_out_d[:, nb * 512:(nb + 1) * 512].rearrange("(kc p) n -> p kc n", p=128), [w_out_d], [wout])
            xin = c.sb_ring("xinD", 2, [128, D], F32)
            hout = c.sb_ring("houtD", 2, [128, D], F32)
            ps_h = c.ps_ring("ps_h", 4, [128, 512], F32)
            for t in range(NT):
                ts_ = slice(t * 128, (t + 1) * 128)
                xt, ht = xin.next(), hout.next()
                c.dma("sp", xt[:, :], x_d[ts_, :], [x_d], [xt])
                for nb in range(4):
                    ns = slice(nb * 512, (nb + 1) * 512)
                    ps = ps_h.next()
                    for kc in range(DC):
                        c.mm(ps[:, :], mergedT[:, kc, ts_], wout[:, kc, ns], kc == 0, kc == DC - 1, [mergedT, wout], [ps])
                    c.tt("dve", ht[:, ns], ps[:, :], xt[:, ns], ALU.add, [ps, xt], [ht])
                c.dma("sp", h1_d[ts_, :], ht[:, :], [ht], [h1_d])
            c.barrier()
        c.stack = outer
        if "h1_s" in dump:
            final_outs.append(h1_d.b)
        if stop_after == "D":
            stop_here()
            return nc

        with ExitStack() as se_:
            c.stack = se_
            qcT = c.sb("qcT", [128, 4, S], BF16)
            kcT = c.sb("kcT", [128, 4, 256], BF16)
            vc = c.sb("vc", [128, 2, 512], BF16)
            ocT = c.sb("ocT", [128, 4, S], BF16)
            with ExitStack() as se1:
                c.stack = se1
                hn1T_b = [c.sb("hn1T%d" % i, [128, DC, 512], BF16) for i in range(4)]
                memnT = c.sb("memnT", [128, DC, 256], BF16)
                rg = {"junk": c.sb_ring("junkE", 1, [128, D], BF16), "ss": c.sb_ring("ssE", 4, [128, 4], F32),
                      "xs": c.sb_ring("xsE", 2, [128, D], BF16), "pst": c.ps_ring("pstE", 2, [128, 512], BF16)}
                xin = c.sb_ring("xinE", 3, [128, D], F32)
                slab_r = c.sb_ring("slabE", 2, [128, DC, 512], BF16)
                psA = c.ps_ring("psAE", 4, [128, 512], F32)
                for mt in range(2):
                    xt = xin.next()
                    c.dma("sp", xt[:, :], mem_d[mt * 128:(mt + 1) * 128, :], [mem_d], [xt])
                    norm_transpose_tile(c, xt, gT, 32, memnT, mt * 128, rg, ident)
                slq_ = slab_r.next()
                c.dma("pool", slq_[:, :, :], w_cq_d[:, :].rearrange("(kc p) n -> p kc n", p=128), [w_cq_d], [slq_])

                def load_h1(t):
                    xt = xin.next()
                    c.dma("sp", xt[:, :], h1_d[t * 128:(t + 1) * 128, :], [h1_d], [xt])
                    return xt

                def q_proj_block(t):
                    if t % 4 != 3:
                        return
                    blk = t // 4
                    cs = slice(blk * 512, (blk + 1) * 512)
                    for hh in range(4):
                        ps = psA.next()
                        for kc in range(DC):
                            c.mm(ps[:, :], slq_[:, kc, hh * 128:(hh + 1) * 128], hn1T_b[blk][:, kc, :], kc == 0, kc == DC - 1,
                                 [slq_, hn1T_b[blk]], [ps])
                        c.copy("act" if hh % 2 == 0 else "dve", qcT[:, hh, cs], ps[:, :], [ps], [qcT])

                norm_transpose_stream(c, NT, load_h1, gT, 16, None, rg, ident,
                                      dst_fn=lambda t: (hn1T_b[t // 4], (t % 4) * 128), after_fn=q_proj_block)
                sl = slab_r.next()
                c.dma("pool", sl[:, :, :], w_ckv_d[:, 0:512].rearrange("(kc p) n -> p kc n", p=128), [w_ckv_d], [sl])
                for hh in range(4):
                    ps = psA.next()
                    for kc in range(DC):
                        c.mm(ps[:, 0:256], sl[:, kc, hh * 128:(hh + 1) * 128], memnT[:, kc, :], kc == 0, kc == DC - 1,
                             [sl, memnT], [ps])
                    c.copy("act", kcT[:, hh, :], ps[:, 0:256], [ps], [kcT])
                sl = slab_r.next()
                c.dma("pool", sl[:, :, :], w_ckv_d[:, 512:1024].rearrange("(kc p) n -> p kc n", p=128), [w_ckv_d], [sl])
                for mt in range(2):
                    ps = psA.next()
                    for kc in range(DC):
                        c.mm(ps[:, :], memnT[:, kc, mt * 128:(mt + 1) * 128], sl[:, kc, :], kc == 0, kc == DC - 1,
                             [sl, memnT], [ps])
                    c.copy("dve", vc[:, mt, :], ps[:, :], [ps], [vc])
                c.barrier()
            with ExitStack() as se2:
                c.stack = se2
                e_ring = c.sb_ring("EE", 4, [128, 512], BF16)
                ps_s = c.ps_ring("ps_sE", 3, [128, 512], F32)
                ps_o = c.ps_ring("ps_oE", 2, [128, 512], F32)
                ps_d = c.ps_ring("ps_dE", 2, [128, 512], F32)
                rec_r = c.sb_ring("recE", 2, [128, 512], F32)
                for hh in range(4):
                    for qb in range(4):
                        qs_ = slice(qb * 512, (qb + 1) * 512)
                        acc_o, acc_d = ps_o.next(), ps_d.next()

                        def score_fn(ps, kt, hh=hh, qs_=qs_):
                            c.mm(ps[:, :], kcT[:, hh, kt * 128:(kt + 1) * 128], qcT[:, hh, qs_], True, True,
                                 [kcT, qcT], [ps])

                        def v_fn(kt, hh=hh):
                            return vc[:, kt, hh * 128:(hh + 1) * 128], vc

                        attn_core(2, qb, score_fn, v_fn, ps_s, e_ring, acc_o, acc_d, 128.0 ** -0.5)
                        rec = rec_r.next()
                        c.recip(rec[:, :], acc_d[:, :], [acc_d], [rec])
                        c.tt("dve", ocT[:, hh, qs_], acc_o[:, :], rec[:, :], ALU.mult, [acc_o, rec], [ocT])
                c.barrier()
            with ExitStack() as se3:
                c.stack = se3
                wco = c.sb("wco", [128, 4, D], BF16)
                c.dma("pool", wco[:, :, :], w_co_d[:, :].rearrange("(kc p) n -> p kc n", p=128), [w_co_d], [wco])
                gff = c.sb("gff", [128, D], F32)
                c.dma("sp", gff[:, :], grow_d[0:1, :].partition_broadcast(128), [grow_d], [gff])
                wr = c.sb("wr", [128, DC, 36], BF16)
                c.dma("pool", wr[:, :, :], w_r_d[:, :].rearrange("(kc p) n -> p kc n", p=128), [w_r_d], [wr])
                br = c.sb("br", [128, 36], F32)
                c.dma("sp", br[:, :], b_r_d[0:1, :].partition_broadcast(128), [b_r_d], [br])
                A_all = c.sb("A_all", [128, NT, 32], BF16)
                hin = c.sb_ring("hinE", 2, [128, D], F32)
                hout = c.sb_ring("houtE", 2, [128, D], F32)
                ttok_r = c.sb_ring("ttok", 3, [128, D], BF16)
                tTs_r = c.sb_ring("tTs", 2, [128, DC, 128], BF16)
                junk_r = c.sb_ring("junkE3", 1, [128, D], BF16)
                ss_r = c.sb_ring("ssE3", 4, [128, 4], F32)
                rt_r = c.sb_ring("rt", 3, [128, 320], F32)
                ps_h = c.ps_ring("ps_hE", 3, [128, 512], F32)
                pst_r = c.ps_ring("pstE3", 2, [128, 512], BF16)
                ps_l = c.ps_ring("ps_l", 2, [128, 64], F32)
                ps_r = c.ps_ring("ps_r", 1, [128, 64], F32)
                pend_tail = []
                for t in range(NT):
                    ts_ = slice(t * 128, (t + 1) * 128)
                    xt, ht = hin.next(), hout.next()
                    c.dma("sp", xt[:, :], h1_d[ts_, :], [h1_d], [xt])
                    for nb in range(4):
                        ns = slice(nb * 512, (nb + 1) * 512)
                        ps = ps_h.next()
                        for kc in range(4):
                            c.mm(ps[:, :], ocT[:, kc, ts_], wco[:, kc, ns], kc == 0, kc == 3, [ocT, wco], [ps])
                        c.tt("dve", ht[:, ns], ps[:, :], xt[:, ns], ALU.add, [ps, xt], [ht])
                    c.dma("sp", h2_d[ts_, :], ht[:, :], [ht], [h2_d])
                    ss, junk, ttok = ss_r.next(), junk_r.next(), ttok_r.next()
                    rmsnorm_rstd(c, ht, D, ss, junk, EPS)
                    c.stt(ttok[:, :], ht[:, :], ss[:, 2:3], gff[:, :], ALU.mult, ALU.mult, [ht, ss, gff], [ttok])
                    tTs = tTs_r.next()
                    for g4 in range(DC // 4):
                        pt = pst_r.next()
                        for j in range(4):
                            ch = g4 * 4 + j
                            c.tr(pt[:, j * 128:(j + 1) * 128], ttok[:, ch * 128:(ch + 1) * 128], ident[:, :],
                                 [ttok, ident], [pt], sig=(j == 3))
                        c.copy("act" if g4 % 2 == 0 else "dve", tTs[:, g4 * 4:(g4 + 1) * 4, :],
                               pt[:, :].rearrange("p (a b) -> p a b", a=4), [pt], [tTs])
                    pl = ps_l.next()
                    for kc in range(DC):
                        c.mm(pl[:, 0:36], tTs[:, kc, :], wr[:, kc, :], kc == 0, kc == DC - 1, [tTs, wr], [pl])
                    rt = rt_r.next()
                    R_ = [rt]
                    L = rt[:, 0:36]
                    c.tt("dve", L, pl[:, 0:36], br[:, :], ALU.add, [pl, br], R_)
                    gmax, ngmax, gsum, gp = rt[:, 40:41], rt[:, 41:42], rt[:, 42:43], rt[:, 43:44]
                    c.p.op("dve", lambda h, rt=rt: h.reduce_max(rt[:, 40:41], rt[:, 0:4], axis=AX.X), bs(R_), bs(R_))
                    c.ts("pool", ngmax, gmax, -1.0, None, ALU.mult, None, R_, R_)
                    c.act(rt[:, 44:48], rt[:, 0:4], AF.Exp, R_, R_, bias=ngmax, scale=1.0, accum=gsum)
                    c.recip(gp, gsum, R_, R_)
                    c.ts("pool", rt[:, 48:52], rt[:, 0:4], gmax, None, ALU.is_ge, None, R_, R_)
                    c.ts("pool", rt[:, 52:56], rt[:, 48:52], -1.0, 1e30, ALU.add, ALU.mult, R_, R_)
                    for g in range(4):
                        c.ts("pool", rt[:, 64 + g * 8:72 + g * 8], rt[:, 4 + g * 8:12 + g * 8], rt[:, 52 + g:53 + g], None,
                             ALU.add, None, R_, R_)
                    Lm = rt[:, 64:96]
                    c.p.op("dve", lambda h, rt=rt: h.max(rt[:, 96:104], rt[:, 64:96]), bs(R_), bs(R_))
                    c.ts("pool", rt[:, 104:136], Lm, rt[:, 96:97], None, ALU.is_equal, None, R_, R_)
                    c.ts("pool", rt[:, 136:168], Lm, rt[:, 97:98], None, ALU.is_equal, None, R_, R_)
                    c.tt("pool", rt[:, 56:57], rt[:, 97:98], rt[:, 96:97], ALU.subtract, R_, R_)
                    c.act(rt[:, 57:58], rt[:, 56:57], AF.Exp, R_, R_)
                    c.ts("pool", rt[:, 58:59], rt[:, 57:58], 1.0, None, ALU.add, None, R_, R_)
                    c.recip(rt[:, 58:59], rt[:, 58:59], R_, R_)
                    c.tt("pool", wts[:, t, 0:1], rt[:, 58:59], gp, ALU.mult, R_, [wts])
                    c.stt(wts[:, t, 1:2], rt[:, 57:58], rt[:, 58:59], gp, ALU.mult, ALU.mult, R_, [wts])
                    c.tt("pool", A_all[:, t, :], rt[:, 104:136], rt[:, 136:168], ALU.add, R_, [A_all])
                    def tail(t=t, rt=rt, R_=R_, ttok=ttok):
                        pr = ps_r.next()
                        c.mm(pr[:, 0:32], Utri[:, :], A_all[:, t, :], True, t == 0, [Utri, A_all], [pr], sig=(t == 0))
                        for tp in range(t):
                            c.mm(pr[:, 0:32], ones[:, :], A_all[:, tp, :], False, tp == t - 1, [ones, A_all], [pr],
                                 sig=(tp == t - 1))
                        c.ts("dve", rt[:, 200:232], pr[:, 0:32], float(CAP), 1e6, ALU.is_ge, ALU.mult, [pr], R_)
                        c.tt("dve", rt[:, 168:200], pr[:, 0:32], cf[:, 2:2 + NE], ALU.add, [pr, cf], R_)
                        c.tt("pool", rt[:, 168:200], rt[:, 168:200], rt[:, 200:232], ALU.add, R_, R_)
                        for k, oh0 in ((0, 104), (1, 136)):
                            c.tt("pool", rt[:, 232 + 32 * k:264 + 32 * k], rt[:, oh0:oh0 + 32], rt[:, 168:200], ALU.mult, R_, R_)
                            c.p.op("dve", lambda h, rt=rt, k=k: h.reduce_sum(rt[:, 59 + k:60 + k], rt[:, 232 + 32 * k:264 + 32 * k],
                                                                           axis=AX.X), bs(R_), bs(R_))
                            c.ts("pool", rt[:, 59 + k:60 + k], rt[:, 59 + k:60 + k], float(NSLOT), None, ALU.min, None, R_, R_)
                            c.copy("pool", slot_i[:, t, k:k + 1], rt[:, 59 + k:60 + k], R_, [slot_i])
                        for k in range(0 if NO_SCATTER else 2):
                            c.p.dma("pool", lambda h, ttok=ttok, t=t, k=k: h.indirect_dma_start(
                                out=xg_d[:, :], out_offset=bass.IndirectOffsetOnAxis(ap=slot_i[:, t, k:k + 1], axis=0),
                                in_=ttok[:, :], in_offset=None, bounds_check=bc_reg, oob_is_err=False),
                                bs([ttok, slot_i]), bs([xg_d]))

                    if pend_tail:
                        pend_tail.pop(0)()
                    pend_tail.append(tail)
                while pend_tail:
                    pend_tail.pop(0)()
                c.barrier()
        c.stack = outer
        for nm, tl in (("h2_s", h2_d), ("xg_s", xg_d)):
            if nm in dump:
                final_outs.append(tl.b)
        if "route" in dump:
            rdump = dram("route", [128, NT, 4], F32)
            c.dma("sp", rdump[:, :, 2:4], wts[:, :, :], [wts], [rdump])
            sfl = c.sb("sfl", [128, NT, 2], F32)
            c.copy("dve", sfl[:, :, :], slot_i[:, :, :], [slot_i], [sfl])
            c.dma("sp", rdump[:, :, 0:2], sfl[:, :, :], [sfl], [rdump])
            final_outs.append(rdump.b)
        if stop_after == "E":
            stop_here()
            return nc

        with ExitStack() as sf_:
            c.stack = sf_
            wg_r = c.sb_ring("wg", 2, [128, DC, DFF], BF16)
            wu_r = c.sb_ring("wu", 2, [128, DC, DFF], BF16)
            wd_r = c.sb_ring("wd", 2, [128, 4, D], BF16)
            xgT_r = c.sb_ring("xgT", 2, [128, DC, CAP], BF16)
            hid_r = c.sb_ring("hid", 2, [128, 4, CAP], BF16)
            sg_r = c.sb_ring("sg", 2, [128, CAP], F32)
            y_r = c.sb_ring("yt", 3, [128, D], BF16)
            pst_r = c.ps_ring("pstF", 2, [128, 512], BF16)
            ps_g = c.ps_ring("ps_g", 2, [128, CAP], F32)
            ps_u = c.ps_ring("ps_u", 2, [128, CAP], F32)
            ps_y = c.ps_ring("ps_y", 2, [128, 512], F32)
            NSB = CAP // 128
            xg_r6 = c.sb_ring("xgt6", 2 * NSB, [128, D], BF16)
            W = {}
            XT = {}
            TG = {}

            def load_weights(e):
                if e >= NE:
                    return
                wg, wu, wd = wg_r.next(), wu_r.next(), wd_r.next()
                c.dma("pool", wg[:, :, :], w_eg_d[e, :, :].rearrange("(p kc) n -> p kc n", p=128), [w_eg_d], [wg])
                c.dma("pool", wu[:, :, :], w_eu_d[e, :, :].rearrange("(p kc) n -> p kc n", p=128), [w_eu_d], [wu])
                c.dma("pool", wd[:, :, :], w_ed_d[e, :, :].rearrange("(p kc) n -> p kc n", p=128), [w_ed_d], [wd])
                W[e] = (wg, wu, wd)

            def load_xg(e):
                if e >= NE:
                    return
                xgT = xgT_r.next()
                XT[e] = xgT
                groups = []
                for sb in range(NSB):
                    xg = xg_r6.next()
                    r0 = e * CAP + sb * 128
                    c.dma("sp", xg[:, :], xg_d[r0:r0 + 128, :], [xg_d], [xg])
                    for g4 in range(DC // 4):
                        groups.append((xg, sb, g4))
                TG[e] = groups

            def transpose_groups(e, n):
                if e >= NE:
                    return
                xgT = XT[e]
                for _ in range(n):
                    if not TG[e]:
                        return
                    xg, sb, g4 = TG[e].pop(0)
                    pt = pst_r.next()
                    for j in range(4):
                        ch = g4 * 4 + j
                        c.tr(pt[:, j * 128:(j + 1) * 128], xg[:, ch:D:DC], ident[:, :], [xg, ident], [pt], sig=(j == 3))
                    c.copy("act" if g4 % 2 == 0 else "dve", xgT[:, g4 * 4:(g4 + 1) * 4, sb * 128:(sb + 1) * 128],
                           pt[:, :].rearrange("p (a b) -> p a b", a=4), [pt], [xgT])

            load_xg(0)
            load_xg(1)
            load_weights(0)
            transpose_groups(0, 4 * NSB)
            for e in range(NE):
                load_xg(e + 2)
                load_weights(e + 1)
                wg, wu, wd = W.pop(e)
                xgT = XT.pop(e)
                hid = hid_r.next()
                for dc in range(4):
                    pg, pu = ps_g.next(), ps_u.next()
                    for kc in range(DC):
                        c.mm(pg[:, :], wg[:, kc, dc:DFF:4], xgT[:, kc, :], kc == 0, kc == DC - 1,
                             [wg, xgT], [pg])
                    for kc in range(DC):
                        c.mm(pu[:, :], wu[:, kc, dc:DFF:4], xgT[:, kc, :], kc == 0, kc == DC - 1,
                             [wu, xgT], [pu])
                    transpose_groups(e + 1, 2)
                    sg = sg_r.next()
                    c.act(sg[:, :], pg[:, :], AF.Silu, [pg], [sg])
                    c.tt("dve", hid[:, dc, :], sg[:, :], pu[:, :], ALU.mult, [sg, pu], [hid])
                for sb in range(NSB):
                    yt = y_r.next()
                    for nb in range(4):
                        ns = slice(nb * 512, (nb + 1) * 512)
                        py = ps_y.next()
                        for dc in range(4):
                            c.mm(py[:, :], hid[:, dc, sb * 128:(sb + 1) * 128], wd[:, dc, ns], dc == 0, dc == 3,
                                 [hid, wd], [py])
                        c.copy("act" if nb % 2 == 0 else "dve", yt[:, ns], py[:, :], [py], [yt])
                    transpose_groups(e + 1, 2)
                    r0 = e * CAP + sb * 128
                    c.dma("sp", y_d[r0:r0 + 128, :], yt[:, :], [yt], [y_d])
                transpose_groups(e + 1, 4 * NSB)
            c.barrier()
        c.stack = outer
        if "y_s" in dump:
            final_outs.append(y_d.b)
        if stop_after == "F":
            stop_here()
            return nc

        with ExitStack() as sg_:
            c.stack = sg_
            gfin = c.sb("gfin", [128, D], F32)
            c.dma("sp", gfin[:, :], grow_d[1:2, :].partition_broadcast(128), [grow_d], [gfin])
            hin = c.sb_ring("hinG", 3, [128, D], F32)
            y1_r = c.sb_ring("y1", 3, [128, D], BF16)
            y2_r = c.sb_ring("y2", 3, [128, D], BF16)
            h3_r = c.sb_ring("h3", 3, [128, D], F32)
            o_r = c.sb_ring("og", 2, [128, D], F32)
            junk_r = c.sb_ring("junkG", 1, [128, D], BF16)
            ss_r = c.sb_ring("ssG", 4, [128, 4], F32)
            def g_phase1(t):
                ts_ = slice(t * 128, (t + 1) * 128)
                ht, y1, y2, h3 = hin.next(), y1_r.next(), y2_r.next(), h3_r.next()
                c.dma("sp", ht[:, :], h2_d[ts_, :], [h2_d], [ht])
                for k, yk in ((0, y1), (1, y2)):
                    c.memset("pool", yk[:, :], 0.0, [yk])
                    c.p.dma("pool", lambda h, yk=yk, t=t, k=k: h.indirect_dma_start(
                        out=yk[:, :], out_offset=None, in_=y_d[:, :],
                        in_offset=bass.IndirectOffsetOnAxis(ap=slot_i[:, t, k:k + 1], axis=0),
                        bounds_check=bc_reg, oob_is_err=False), bs([y_d, slot_i]), bs([yk]))
                c.stt(h3[:, :], y1[:, :], wts[:, t, 0:1], ht[:, :], ALU.mult, ALU.add, [y1, wts, ht], [h3])
                c.stt(h3[:, :], y2[:, :], wts[:, t, 1:2], h3[:, :], ALU.mult, ALU.add, [y2, wts, h3], [h3])
                ss, junk = ss_r.next(), junk_r.next()
                c.act(junk[:, :D], h3[:, :D], AF.Square, [h3], [junk, ss], accum=ss[:, 0:1])
                return h3, ss

            def g_phase2(t, h3, ss):
                ts_ = slice(t * 128, (t + 1) * 128)
                og = o_r.next()
                c.ts("dve", ss[:, 1:2], ss[:, 0:1], 1.0 / D, EPS, ALU.mult, ALU.add, [ss], [ss])
                c.p.op("act", lambda h: h.sqrt(ss[:, 3:4], ss[:, 1:2]), bs([ss]), bs([ss]))
                c.recip(ss[:, 2:3], ss[:, 3:4], [ss], [ss])
                c.stt(og[:, :], h3[:, :], ss[:, 2:3], gfin[:, :], ALU.mult, ALU.mult, [h3, ss, gfin], [og])
                c.dma("sp", out_d[ts_, :], og[:, :], [og], [out_d])

            cur = g_phase1(0)
            for t in range(NT):
                nxt = g_phase1(t + 1) if t + 1 < NT else None
                g_phase2(t, *cur)
                cur = nxt
            final_outs.append(out_d.b)
            finish()
        c.stack = outer
    return nc


def _fm(g):
    g = np.asarray(g, np.float32).reshape(-1)
    return np.ascontiguousarray(g.reshape(-1, 128).T)


def host_shared(inputs):
    f32 = np.float32
    bf = ml_dtypes.bfloat16
    m = {}
    for k in ("w_in", "w_o_diff", "w_uq", "w_ukv", "w_o_mla", "w_out", "w_cq", "w_ckv", "w_co",
              "w_expert_gate", "w_expert_up", "w_expert_down"):
        m[k] = np.ascontiguousarray(np.asarray(inputs[k], f32)[0])
    m["w_router"] = np.ascontiguousarray(np.concatenate(
        [np.asarray(inputs["w_router_group"], f32)[0], np.asarray(inputs["w_router_expert"], f32)[0]], axis=1))
    m["b_router"] = np.ascontiguousarray(np.concatenate(
        [np.asarray(inputs["b_router_group"], f32)[0], np.asarray(inputs["b_router_expert"], f32)[0]])[None, :])
    gT = np.concatenate([_fm(inputs["attn_norm_g"][0]), _fm(inputs["cross_norm_g"][0]), _fm(inputs["mem_norm_g"][0]),
                         _fm(inputs["mla_q_norm_g"][0]), _fm(inputs["mla_kv_norm_g"][0]),
                         _fm(inputs["diff_subln_g"][0]), np.zeros((128, 1), f32),
                         _fm(inputs["ffn_norm_g"][0])], axis=1)
    m["gT"] = np.ascontiguousarray(gT, f32)
    m["grow"] = np.ascontiguousarray(np.stack([np.asarray(inputs["ffn_norm_g"], f32)[0],
                                               np.asarray(inputs["final_norm_g"], f32)], axis=0))
    m["lam"] = np.ascontiguousarray(np.stack([np.asarray(inputs[k], f32)[0] for k in
                                              ("diff_lambda_q1", "diff_lambda_k1", "diff_lambda_q2", "diff_lambda_k2")]).reshape(1, 256))
    ident = np.eye(128, dtype=f32)
    ones = np.ones((128, 128), f32)
    Rd = np.zeros((128, 128), f32)
    for blk in range(2):
        for i in range(8):
            Rd[blk * 64 + i + 8, blk * 64 + i] = -1.0
            Rd[blk * 64 + i, blk * 64 + i + 8] = 1.0
    U = np.triu(np.ones((128, 128), f32), k=1)
    m["cb"] = np.ascontiguousarray(np.concatenate([ident, ones, Rd, U], axis=1).astype(bf))
    Rm = np.zeros((128, 128), f32)
    for i in range(32):
        Rm[i + 32, i] = -1.0
        Rm[i, i + 32] = 1.0
    m["rm"] = Rm.astype(bf)
    cf = np.zeros((128, 2 + NE), f32)
    for pp in range(128):
        i = pp % 64
        if i < 16:
            cf[pp, 0] = ROPE_THETA ** (-(i % 8) * 2.0 / 16.0) / (2 * math.pi)
        cf[pp, 1] = ROPE_THETA ** (-(i % 32) * 2.0 / 64.0) / (2 * math.pi)
    cf[:, 2:] = (np.arange(NE, dtype=f32) * CAP)[None, :]
    m["cf"] = cf
    return m


_NC_CACHE = {}
_DEV_CORES = 0


def kernel(**inputs):
    return _run(inputs)


def _run(inputs, stop_after=None, dump=()):
    inputs = {k: np.asarray(v) for k, v in inputs.items()}
    key = (stop_after, tuple(dump))
    if key not in _NC_CACHE:
        _NC_CACHE[key] = build_program(stop_after=stop_after, dump=dump)
    nc = _NC_CACHE[key]
    shared = host_shared(inputs)
    in_maps = []
    ncores = _DEV_CORES or NCORES
    for b in range(ncores):
        m = dict(shared)
        m["x"] = np.ascontiguousarray(inputs["x"][b], np.float32)
        m["mem"] = np.ascontiguousarray(inputs["mem"][b], np.float32)
        m["pos"] = np.ascontiguousarray(inputs["positions"][b], np.int32)[None, :]
        in_maps.append(m)
    res = run_bass_kernel_spmd(nc, in_maps, core_ids=list(range(ncores)))
    out = np.stack([np.asarray(r["out"]) for r in res.results], axis=0).astype(np.float32)
    if dump:
        return out, [{k: np.asarray(r[k]) for k in dump} for r in res.results]
    return out
```

```python
import math
from contextlib import ExitStack

import numpy as np
import ml_dtypes

import concourse.bass as bass
import concourse.mybir as mybir
from concourse.bass_utils import run_bass_kernel_spmd

F32 = mybir.dt.float32
BF16 = mybir.dt.bfloat16
I32 = mybir.dt.int32
ALU = mybir.AluOpType
AF = mybir.ActivationFunctionType
AX = mybir.AxisListType

NCORES = 8
S = 2048
D = 2048
NT = S // 128
DC = D // 128
D_IN = 8000
EPS = 1e-6


class Buf:
    __slots__ = ("name", "last_w", "readers")

    def __init__(self, name):
        self.name = name
        self.last_w = None
        self.readers = {}


class Prog:
    ENGINES = ("pe", "act", "dve", "pool", "sp")

    def __init__(self, nc, stack, n_lanes=12):
        self.nc = nc
        self.handles = {"pe": nc.tensor, "act": nc.scalar, "dve": nc.vector,
                        "pool": nc.gpsimd, "sp": nc.sync}
        self.thunks = {e: [] for e in self.ENGINES}
        self.sems = {}
        self.count = {}
        for e in self.ENGINES:
            self.sems[e] = stack.enter_context(nc.semaphore("c_" + e))
            self.count[e] = 0
        self.lanes = {}
        for q in ("sp", "pool"):
            ls = []
            for i in range(n_lanes):
                key = "l_%s%d" % (q, i)
                self.sems[key] = stack.enter_context(nc.semaphore(key))
                self.count[key] = 0
                ls.append(key)
            self.lanes[q] = ls
        self.lane_rr = {"sp": 0, "pool": 0}
        self.known = {e: {} for e in self.ENGINES}
        self.n_waits = 0
        self.events = {e: [] for e in self.ENGINES}

    def _collect(self, eng, reads, writes):
        need = {}

        def add(tok, same_ok):
            if tok is None:
                return
            k, v = tok
            if k == eng and same_ok:
                return
            if need.get(k, 0) < v:
                need[k] = v

        for b in reads:
            add(b.last_w, same_ok=(eng == "pe"))
        for b in writes:
            add(b.last_w, same_ok=True)
            for k, v in b.readers.items():
                add((k, v), same_ok=True)
        out = []
        kn = self.known[eng]
        for k, v in need.items():
            if kn.get(k, 0) >= v:
                continue
            kn[k] = v
            out.append((k, v))
        return out

    def _emit_waits(self, eng, waits):
        h = self.handles[eng]
        sems = self.sems
        for k, v in waits:
            self.n_waits += 1
            self.events[eng].append(("w", k, v))
            self.thunks[eng].append(lambda h=h, s=sems[k], v=v: h.wait_ge(s, v))

    def _update(self, tok, reads, writes):
        k, v = tok
        for b in reads:
            if b.readers.get(k, 0) < v:
                b.readers[k] = v
        for b in writes:
            b.last_w = tok
            b.readers = {}

    def op(self, eng, fn, reads=(), writes=(), sig=True):
        waits = self._collect(eng, reads, writes)
        self._emit_waits(eng, waits)
        h = self.handles[eng]
        sem = self.sems[eng]
        if sig:
            self.count[eng] += 1
            tok = (eng, self.count[eng])
            self.events[eng].append(("s", eng, 1))
            self.thunks[eng].append(lambda: fn(h).then_inc(sem, 1))
        else:
            tok = (eng, self.count[eng] + 1)
            self.thunks[eng].append(lambda: fn(h))
        self._update(tok, reads, writes)
        return tok

    def dma(self, q, fn, reads=(), writes=()):
        lanes = self.lanes[q]
        lane = lanes[self.lane_rr[q] % len(lanes)]
        self.lane_rr[q] += 1
        waits = self._collect(q, reads, writes)
        prev = self.count[lane]
        if prev > 0 and self.known[q].get(lane, 0) < prev:
            self.known[q][lane] = prev
            waits.append((lane, prev))
        self._emit_waits(q, waits)
        h = self.handles[q]
        sem = self.sems[lane]
        self.count[lane] += 16
        tok = (lane, self.count[lane])
        self.events[q].append(("s", lane, 16))
        self.thunks[q].append(lambda: fn(h).then_inc(sem, 16))
        self._update(tok, reads, writes)
        return tok

    def wait_all(self, eng, bufs):
        waits = self._collect(eng, bufs, ())
        self._emit_waits(eng, waits)

    def check_deadlock(self):
        val = {}
        pos = {e: 0 for e in self.ENGINES}
        progress = True
        while progress:
            progress = False
            for e in self.ENGINES:
                ev = self.events[e]
                i = pos[e]
                while i < len(ev):
                    kind, k, v = ev[i]
                    if kind == "w":
                        if val.get(k, 0) < v:
                            break
                    else:
                        val[k] = val.get(k, 0) + v
                    i += 1
                if i != pos[e]:
                    progress = True
                    pos[e] = i
        stuck = {e: (pos[e], len(self.events[e]), self.events[e][pos[e]]) for e in self.ENGINES
                 if pos[e] < len(self.events[e])}
        if stuck:
            raise RuntimeError("build-time deadlock check failed: %r" % (stuck,))

    def emit(self):
        self.check_deadlock()
        nc = self.nc
        with nc.Block() as block:
            @block.tensor
            def _(e):
                for t in self.thunks["pe"]:
                    t()

            @block.scalar
            def _(e):
                for t in self.thunks["act"]:
                    t()

            @block.vector
            def _(e):
                for t in self.thunks["dve"]:
                    t()

            @block.gpsimd
            def _(e):
                for t in self.thunks["pool"]:
                    t()

            @block.sync
            def _(e):
                for t in self.thunks["sp"]:
                    t()


class Ring:
    def __init__(self, tiles):
        self.tiles = tiles
        self.i = 0

    def next(self):
        t = self.tiles[self.i % len(self.tiles)]
        self.i += 1
        return t


class Tile:
    __slots__ = ("t", "b")

    def __init__(self, t, name):
        self.t = t
        self.b = Buf(name)

    def __getitem__(self, k):
        return self.t[k]


class Ctx:
    def __init__(self, nc, stack):
        self.nc = nc
        self.stack = stack
        self.p = Prog(nc, stack)
        self._n = 0

    def sb(self, name, shape, dt):
        self._n += 1
        return Tile(self.stack.enter_context(self.nc.sbuf_tensor("s%d_%s" % (self._n, name), list(shape), dt)), name)

    def ps(self, name, shape, dt=F32):
        self._n += 1
        return Tile(self.stack.enter_context(self.nc.psum_tensor("p%d_%s" % (self._n, name), list(shape), dt)), name)

    def dram(self, name, shape, dt, kind="Internal"):
        return Tile(self.nc.dram_tensor(name, list(shape), dt, kind=kind), name)

    def sb_ring(self, name, n, shape, dt):
        return Ring([self.sb("%s%d" % (name, i), shape, dt) for i in range(n)])

    def ps_ring(self, name, n, shape, dt=F32):
        return Ring([self.ps("%s%d" % (name, i), shape, dt) for i in range(n)])


H = 8
ROPE_THETA = 500000.0
CAP = 384
NE = 32
NSLOT = NE * CAP
DFF = 512
LAM_INIT = 0.8 - 0.6 * math.exp(-0.3 * 0)
PI = math.pi
NO_SCATTER = False


def bs(tiles):
    return [t.b for t in tiles]


class K(Ctx):
    def mm(self, out, lhsT, rhs, start, stop, R, W, sig=None):
        if sig is None:
            sig = stop
        self.p.op("pe", lambda h: h.matmul(out, lhsT, rhs, start=start, stop=stop), bs(R), bs(W), sig=sig)

    def tr(self, out, in_, ident, R, W, sig=True):
        self.p.op("pe", lambda h: h.transpose(out, in_, ident), bs(R), bs(W), sig=sig)

    def act(self, out, in_, func, R, W, bias=None, scale=None, accum=None):
        kw = {}
        if bias is not None:
            kw["bias"] = bias
        if scale is not None:
            kw["scale"] = scale
        if accum is not None:
            kw["accum_out"] = accum
        self.p.op("act", lambda h: h.activation(out, in_, func, **kw), bs(R), bs(W))

    def ts(self, eng, out, in0, s1, s2, op0, op1, R, W):
        if op1 is None:
            self.p.op(eng, lambda h: h.tensor_scalar(out, in0, s1, None, op0), bs(R), bs(W))
        else:
            self.p.op(eng, lambda h: h.tensor_scalar(out, in0, s1, s2, op0, op1), bs(R), bs(W))

    def tt(self, eng, out, in0, in1, op, R, W):
        self.p.op(eng, lambda h: h.tensor_tensor(out, in0, in1, op), bs(R), bs(W))

    def stt(self, out, in0, scalar, in1, op0, op1, R, W, accum=None):
        if accum is None:
            self.p.op("dve", lambda h: h.scalar_tensor_tensor(out, in0, scalar, in1, op0, op1), bs(R), bs(W))
        else:
            self.p.op("dve", lambda h: h.scalar_tensor_tensor(out, in0, scalar, in1, op0, op1, accum_out=accum),
                      bs(R), bs(W))

    def copy(self, eng, out, in_, R, W):
        if eng == "act":
            self.p.op("act", lambda h: h.copy(out, in_), bs(R), bs(W))
        else:
            self.p.op(eng, lambda h: h.tensor_copy(out, in_), bs(R), bs(W))

    def recip(self, out, in_, R, W):
        self.p.op("dve", lambda h: h.reciprocal(out, in_), bs(R), bs(W))

    def memset(self, eng, out, val, W):
        self.p.op(eng, lambda h: h.memset(out, val), [], bs(W))

    def dma(self, q, out, in_, R, W):
        self.p.dma(q, lambda h: h.dma_start(out=out, in_=in_), bs(R), bs(W))

    def barrier(self):
        p = self.p
        toks = [(e, p.count[e]) for e in p.ENGINES if p.count[e] > 0]
        for q in ("sp", "pool"):
            for lane in p.lanes[q]:
                if p.count[lane] > 0:
                    toks.append((lane, p.count[lane]))
        for e in p.ENGINES:
            w = []
            for k, v in toks:
                if k == e:
                    continue
                if p.known[e].get(k, 0) < v:
                    p.known[e][k] = v
                    w.append((k, v))
            p._emit_waits(e, w)


def rmsnorm_rstd(c, x_t, d, ss, junk, eps):
    c.act(junk[:, :d], x_t[:, :d], AF.Square, [x_t], [junk, ss], accum=ss[:, 0:1])
    c.ts("dve", ss[:, 1:2], ss[:, 0:1], 1.0 / d, eps, ALU.mult, ALU.add, [ss], [ss])
    c.p.op("act", lambda h: h.sqrt(ss[:, 3:4], ss[:, 1:2]), bs([ss]), bs([ss]))
    c.recip(ss[:, 2:3], ss[:, 3:4], [ss], [ss])


def norm_phase1(c, x_t, rg):
    junk = rg["junk"].next()
    ss = rg["ss"].next()
    c.act(junk[:, :D], x_t[:, :D], AF.Square, [x_t], [junk, ss], accum=ss[:, 0:1])
    return ss


def norm_phase2(c, x_t, ss, gT, goff, dstT, tcol, rg, ident):
    xs = rg["xs"].next()
    c.ts("dve", ss[:, 1:2], ss[:, 0:1], 1.0 / D, EPS, ALU.mult, ALU.add, [ss], [ss])
    c.p.op("act", lambda h: h.sqrt(ss[:, 3:4], ss[:, 1:2]), bs([ss]), bs([ss]))
    c.recip(ss[:, 2:3], ss[:, 3:4], [ss], [ss])
    half = D // 2
    c.ts("dve", xs[:, :half], x_t[:, :half], ss[:, 2:3], None, ALU.mult, None, [x_t, ss], [xs])
    c.act(xs[:, half:], x_t[:, half:], AF.Copy, [x_t, ss], [xs], scale=ss[:, 2:3])
    for g4 in range(DC // 4):
        pt = rg["pst"].next()
        for j in range(4):
            ch = g4 * 4 + j
            c.tr(pt[:, j * 128:(j + 1) * 128], xs[:, ch * 128:(ch + 1) * 128], ident[:, :], [xs, ident], [pt],
                 sig=(j == 3))
        for j in range(4):
            ch = g4 * 4 + j
            if j % 2 == 0:
                c.ts("dve", dstT[:, ch, tcol:tcol + 128], pt[:, j * 128:(j + 1) * 128],
                     gT[:, goff + ch:goff + ch + 1], None, ALU.mult, None, [pt, gT], [dstT])
            else:
                c.act(dstT[:, ch, tcol:tcol + 128], pt[:, j * 128:(j + 1) * 128], AF.Copy, [pt, gT], [dstT],
                      scale=gT[:, goff + ch:goff + ch + 1])


def norm_transpose_stream(c, n_tiles, load_fn, gT, goff, dstT, rg, ident, dst_fn=None, after_fn=None):
    cur = load_fn(0)
    ss = norm_phase1(c, cur, rg)
    for t in range(n_tiles):
        nxt = nss = None
        if t + 1 < n_tiles:
            nxt = load_fn(t + 1)
            nss = norm_phase1(c, nxt, rg)
        if dst_fn is None:
            norm_phase2(c, cur, ss, gT, goff, dstT, t * 128, rg, ident)
        else:
            dt_, tcol = dst_fn(t)
            norm_phase2(c, cur, ss, gT, goff, dt_, tcol, rg, ident)
        if after_fn is not None:
            after_fn(t)
        cur, ss = nxt, nss


def norm_transpose_tile(c, x_t, gT, goff, dstT, tcol, rg, ident):
    ss = norm_phase1(c, x_t, rg)
    norm_phase2(c, x_t, ss, gT, goff, dstT, tcol, rg, ident)


def rope_tables(c, pos_d, invf, col, nparts, Ct, St, tmp_i, tmp_f):
    n = nparts
    c.dma("sp", tmp_i[:n, :], pos_d[0:1, :].partition_broadcast(n), [pos_d], [tmp_i])
    c.copy("dve", tmp_f[:n, :], tmp_i[:n, :], [tmp_i], [tmp_f])
    for dst, phase in ((St, 0.0), (Ct, 0.25)):
        c.ts("dve", dst[:n, :], tmp_f[:n, :], invf[:n, col:col + 1], phase, ALU.mult, ALU.add, [tmp_f, invf], [dst])
        c.copy("dve", tmp_i[:n, :], dst[:n, :], [dst], [tmp_i])
        c.copy("dve", Ct[:n, :] if dst is St else tmp_f[:n, :], tmp_i[:n, :], [tmp_i], [Ct if dst is St else tmp_f])
        kf = Ct if dst is St else tmp_f
        c.tt("dve", dst[:n, :], dst[:n, :], kf[:n, :], ALU.subtract, [dst, kf], [dst])
        for _ in range(2):
            c.stt(dst[:n, :], dst[:n, :], 0.5, dst[:n, :], ALU.is_gt, ALU.subtract, [dst], [dst])
        c.act(dst[:n, :], dst[:n, :], AF.Sin, [dst], [dst], scale=2 * PI)


def rope_evac(c, ps, npart, blk, Rm, Ct, St, rg, dst_ap, dst_tile, defer=None, kfull=False):
    qs = rg["qs"].next()
    ncp = 128 if kfull else npart
    c.copy("act", qs[:ncp, :], ps[:ncp, :], [ps], [qs])
    if kfull:
        _rope_rest(c, qs, npart, blk, Rm, Ct, St, rg, dst_ap, dst_tile, kfull=True)
        return
    if defer is not None:
        defer.append(lambda: _rope_rest(c, qs, npart, blk, Rm, Ct, St, rg, dst_ap, dst_tile))
        return
    _rope_rest(c, qs, npart, blk, Rm, Ct, St, rg, dst_ap, dst_tile)


def _rope_rest(c, qs, npart, blk, Rm, Ct, St, rg, dst_ap, dst_tile, kfull=False):
    pr = rg["psr"].next()
    nk = 128 if kfull else npart
    c.mm(pr[:nk, :], Rm[:nk, :nk], qs[:nk, :], True, True, [Rm, qs], [pr])
    t1 = rg["t1"].next()
    t2 = rg["t2"].next()
    cs = slice(blk * 512, (blk + 1) * 512)
    c.tt("dve", t1[:npart, :], qs[:npart, :], Ct[:npart, cs], ALU.mult, [qs, Ct], [t1])
    c.tt("dve", t2[:npart, :], pr[:npart, :], St[:npart, cs], ALU.mult, [pr, St], [t2])
    c.tt("pool", dst_ap, t1[:npart, :], t2[:npart, :], ALU.add, [t1, t2], [dst_tile])


def build_program(stop_after=None, dump=()):
    nc = bass.Bass("TRN2", target_bir_lowering=False)
    outer = ExitStack()
    with outer:
        c = K(nc, outer)
        p = c.p

        def dram(name, shape, dt, kind=None):
            if kind is None:
                kind = "ExternalOutput" if name in dump else "Internal"
            return c.dram(name, shape, dt, kind=kind)

        x_d = dram("x", [S, D], F32, "ExternalInput")
        mem_d = dram("mem", [256, D], F32, "ExternalInput")
        pos_d = dram("pos", [1, S], I32, "ExternalInput")
        w_in_d = dram("w_in", [D, D_IN], F32, "ExternalInput")
        w_od_d = dram("w_o_diff", [1024, D], F32, "ExternalInput")
        w_uq_d = dram("w_uq", [512, 1536], F32, "ExternalInput")
        w_ukv_d = dram("w_ukv", [256, 2048], F32, "ExternalInput")
        w_om_d = dram("w_o_mla", [1024, D], F32, "ExternalInput")
        w_out_d = dram("w_out", [D, D], F32, "ExternalInput")
        w_cq_d = dram("w_cq", [D, 512], F32, "ExternalInput")
        w_ckv_d = dram("w_ckv", [D, 1024], F32, "ExternalInput")
        w_co_d = dram("w_co", [512, D], F32, "ExternalInput")
        w_r_d = dram("w_router", [D, 36], F32, "ExternalInput")
        b_r_d = dram("b_router", [1, 36], F32, "ExternalInput")
        w_eg_d = dram("w_expert_gate", [NE, D, DFF], F32, "ExternalInput")
        w_eu_d = dram("w_expert_up", [NE, D, DFF], F32, "ExternalInput")
        w_ed_d = dram("w_expert_down", [NE, DFF, D], F32, "ExternalInput")
        gT_d = dram("gT", [128, 4 * DC + 8], F32, "ExternalInput")
        grow_d = dram("grow", [2, D], F32, "ExternalInput")
        lam_d = dram("lam", [1, 256], F32, "ExternalInput")
        cb_d = dram("cb", [128, 4 * 128], BF16, "ExternalInput")
        rm_d = dram("rm", [128, 128], BF16, "ExternalInput")
        cf_d = dram("cf", [128, 2 + NE], F32, "ExternalInput")
        out_d = dram("out", [S, D], F32, "ExternalOutput")

        qT_d = dram("qT_s", [H, 128, S], BF16)
        kT_d = dram("kT_s", [H, 128, S], BF16)
        v_d = dram("v_s", [S, 1024], BF16)
        cq_d = dram("cq_s", [4, 128, S], F32)
        ckv_d = dram("ckv_s", [2, 128, S], F32)
        kpe_d = dram("kpe_s", [64, S], BF16)
        ga_d = dram("ga_s", [DC, 128, S], BF16)
        gb_d = dram("gb_s", [DC, 128, S], BF16)
        oa_d = dram("oa_s", [H, 128, S], BF16)
        ob_d = dram("ob_s", [H, 128, S], BF16)
        h1_d = dram("h1_s", [S, D], F32)
        h2_d = dram("h2_s", [S, D], F32)
        xg_d = dram("xg_s", [NSLOT + 128, D], BF16)
        y_d = dram("y_s", [NSLOT + 128, D], BF16)

        cb = c.sb("cb", [128, 4 * 128], BF16)
        rm = c.sb("rm", [128, 128], BF16)
        cf = c.sb("cf", [128, 2 + NE], F32)
        gT = c.sb("gTs", [128, 4 * DC + 8], F32)
        c.dma("sp", cb[:, :], cb_d[:, :], [cb_d], [cb])
        c.dma("sp", rm[:, :], rm_d[:, :], [rm_d], [rm])
        c.dma("sp", cf[:, :], cf_d[:, :], [cf_d], [cf])
        c.dma("sp", gT[:, :], gT_d[:, :], [gT_d], [gT])
        ident = Tile(cb.t[:, 0:128], "ident"); ident.b = cb.b
        ones = Tile(cb.t[:, 128:256], "ones"); ones.b = cb.b
        Rd = Tile(cb.t[:, 256:384], "Rd"); Rd.b = cb.b
        Utri = Tile(cb.t[:, 384:512], "Utri"); Utri.b = cb.b
        slot_i = c.sb("slot_i", [128, NT, 2], I32)
        wts = c.sb("wts", [128, NT, 2], F32)
        zrow = c.sb("zrow", [128, D], BF16)
        c.memset("pool", zrow[:, :], 0.0, [zrow])

        final_outs = []
        bc_reg = nc.gpsimd.alloc_register("bc_reg")
        p.thunks["pool"].insert(0, lambda: nc.gpsimd.reg_mov(bc_reg, NSLOT + 127))

        def finish():
            p.wait_all("sp", [b for b in final_outs])
            p.emit()

        with ExitStack() as sa:
            c.stack = sa
            xnT = c.sb("xnT", [128, DC, S], BF16)
            with ExitStack() as sa1:
                c.stack = sa1
                rg = {"junk": c.sb_ring("junk", 1, [128, D], BF16), "ss": c.sb_ring("ss", 4, [128, 4], F32),
                      "xs": c.sb_ring("xs", 2, [128, D], BF16), "pst": c.ps_ring("pst", 2, [128, 512], BF16)}
                xin = c.sb_ring("xin", 3, [128, D], F32)

                def load_x(t):
                    xt = xin.next()
                    c.dma("sp", xt[:, :], x_d[t * 128:(t + 1) * 128, :], [x_d], [xt])
                    return xt

                norm_transpose_stream(c, NT, load_x, gT, 0, xnT, rg, ident)
                c.barrier()
            c.stack = sa
            if "xnT_dbg" in dump:
                dbg = dram("xnT_dbg", [128, DC, S], BF16)
                c.dma("sp", dbg[:, :, :], xnT[:, :, :], [xnT], [dbg])
                final_outs.append(dbg.b)
            Cd = c.sb("Cd", [128, S], F32)
            Sd = c.sb("Sd", [128, S], F32)
            Cm = c.sb("Cm", [64, S], F32)
            Sm = c.sb("Sm", [64, S], F32)
            with ExitStack() as sa2:
                c.stack = sa2
                tmp_i = c.sb("tmp_i", [128, S], I32)
                tmp_f = c.sb("tmp_f", [128, S], F32)
                rope_tables(c, pos_d, cf, 0, 128, Cd, Sd, tmp_i, tmp_f)
                rope_tables(c, pos_d, cf, 1, 64, Cm, Sm, tmp_i, tmp_f)
                c.barrier()
            c.stack = sa
            slabs = c.sb_ring("wslab", 3, [128, DC, 512], BF16)
            rg = {"qs": c.sb_ring("qs", 3, [128, 512], BF16), "psr": c.ps_ring("psr", 2, [128, 512], F32),
                  "t1": c.sb_ring("t1", 2, [128, 512], F32), "t2": c.sb_ring("t2", 2, [128, 512], F32)}
            psA = c.ps_ring("psA", 4, [128, 512], F32)
            stg = c.sb_ring("stgA", 4, [128, 512], BF16)
            stgf = c.sb_ring("stgAf", 3, [128, 512], F32)

            def load_slab(c0, ncols):
                sl = slabs.next()
                c.dma("pool", sl[:, :, :ncols], w_in_d[:, c0:c0 + ncols].rearrange("(kc p) n -> p kc n", p=128),
                      [w_in_d], [sl])
                return sl

            slab_specs = ([(2048, 512), (2560, 512), (0, 512), (512, 512), (1024, 512), (1536, 512), (3072, 512), (3584, 320)]
                          + [(3904 + w_ * 2048 + s_ * 512, 512) for w_ in range(2) for s_ in range(4)])
            slab_q = []
            slab_i = {"i": 0}

            def issue_slab():
                if slab_i["i"] < len(slab_specs):
                    c0_, n_ = slab_specs[slab_i["i"]]
                    slab_i["i"] += 1
                    slab_q.append(((c0_, n_), load_slab(c0_, n_)))

            def get_slab(c0, ncols):
                if not slab_q:
                    issue_slab()
                spec, sl_ = slab_q.pop(0)
                assert spec == (c0, ncols), (spec, c0, ncols)
                issue_slab()
                return sl_

            def proj_fm(sl, off, width, blk):
                ps = psA.next()
                for kc in range(DC):
                    c.mm(ps[:width, :], sl[:, kc, off:off + width], xnT[:, kc, blk * 512:(blk + 1) * 512],
                         kc == 0, kc == DC - 1, [sl, xnT], [ps])
                return ps

            for sidx in range(2):
                sl = get_slab(2048 + sidx * 512, 512)
                for t in range(NT):
                    ps = psA.next()
                    for kc in range(DC):
                        c.mm(ps[:, :], xnT[:, kc, t * 128:(t + 1) * 128], sl[:, kc, :], kc == 0, kc == DC - 1,
                             [sl, xnT], [ps])
                    st = stg.next()
                    c.copy("act" if t % 2 == 0 else "dve", st[:, :], ps[:, :], [ps], [st])
                    c.dma("sp", v_d[t * 128:(t + 1) * 128, sidx * 512:(sidx + 1) * 512], st[:, :], [st], [v_d])
            pend = []

            def flush_pending():
                while pend:
                    pend.pop(0)()

            for which, dst_d in ((0, qT_d), (1, kT_d)):
                for sidx in range(2):
                    sl = get_slab(which * 1024 + sidx * 512, 512)
                    for j in range(4):
                        hh = sidx * 4 + j
                        for blk in range(4):
                            ps = proj_fm(sl, j * 128, 128, blk)
                            flush_pending()
                            st = stg.next()
                            todo = []
                            rope_evac(c, ps, 128, blk, Rd, Cd, Sd, rg, st[:, :], st, defer=todo)

                            def tail(todo=todo, st=st, dst_d=dst_d, hh=hh, blk=blk):
                                todo[0]()
                                c.dma("sp", dst_d[hh, :, blk * 512:(blk + 1) * 512], st[:, :], [st], [dst_d])
                            pend.append(tail)
            flush_pending()
            sl = get_slab(3072, 512)
            for j in range(4):
                for blk in range(4):
                    ps = proj_fm(sl, j * 128, 128, blk)
                    st = stgf.next()
                    c.copy("act" if blk % 2 == 0 else "dve", st[:, :], ps[:, :], [ps], [st])
                    c.dma("sp", cq_d[j, :, blk * 512:(blk + 1) * 512], st[:, :], [st], [cq_d])
            sl = get_slab(3584, 320)
            for j in range(2):
                for blk in range(4):
                    ps = proj_fm(sl, j * 128, 128, blk)
                    st = stgf.next()
                    c.copy("act" if blk % 2 == 0 else "dve", st[:, :], ps[:, :], [ps], [st])
                    c.dma("sp", ckv_d[j, :, blk * 512:(blk + 1) * 512], st[:, :], [st], [ckv_d])
            for blk in range(4):
                ps = proj_fm(sl, 256, 64, blk)
                st = stg.next()
                rope_evac(c, ps, 64, blk, rm, Cm, Sm, rg, st[:64, :], st)
                c.dma("sp", kpe_d[:, blk * 512:(blk + 1) * 512], st[:64, :], [st], [kpe_d])
            for which, dst_d in ((0, ga_d), (1, gb_d)):
                for sidx in range(4):
                    sl = get_slab(3904 + which * 2048 + sidx * 512, 512)
                    for j in range(4):
                        ch = sidx * 4 + j
                        for blk in range(4):
                            ps = proj_fm(sl, j * 128, 128, blk)
                            st = stg.next()
                            c.act(st[:, :], ps[:, :], AF.Sigmoid, [ps], [st])
                            c.dma("sp", dst_d[ch, :, blk * 512:(blk + 1) * 512], st[:, :], [st], [dst_d])
            c.barrier()
        c.stack = outer
        for nm, tl in (("qT_s", qT_d), ("kT_s", kT_d), ("v_s", v_d), ("cq_s", cq_d), ("ckv_s", ckv_d),
                       ("kpe_s", kpe_d), ("ga_s", ga_d), ("gb_s", gb_d)):
            if nm in dump:
                final_outs.append(tl.b)
        if stop_after == "A":
            final_outs.append(out_d.b)
            c.dma("sp", out_d[0:128, :], x_d[0:128, :], [x_d], [out_d])
            finish()
            return nc

        def stop_here():
            final_outs.append(out_d.b)
            c.dma("sp", out_d[0:128, :], x_d[0:128, :], [x_d], [out_d])
            finish()

        def attn_core(nk_tiles, qblk, score_fn, v_fn, ps_s, e_ring, acc_o, acc_d, scale):
            Es = {}

            def score(kt):
                ps = ps_s.next()
                score_fn(ps, kt)
                E = e_ring.next()
                c.act(E[:, :], ps[:, :], AF.Exp, [ps], [E], scale=scale)
                Es[kt] = E

            score(0)
            for kt in range(nk_tiles):
                if kt + 1 < nk_tiles:
                    score(kt + 1)
                E = Es.pop(kt)
                vap, vt = v_fn(kt)
                last = kt == nk_tiles - 1
                c.mm(acc_o[:, :], vap, E[:, :], kt == 0, last, [vt, E], [acc_o], sig=last)
                c.mm(acc_d[:, :], ones[:, :], E[:, :], kt == 0, last, [ones, E], [acc_d], sig=last)

        def attn_stream(calls, ps_s, e_ring, ps_o, ps_d, scale, look):
            flat = [(ci, kt) for ci, cl in enumerate(calls) for kt in range(0, cl["nk"], 2)]
            Es = {}
            state = {"nxt": 0}

            def score(i):
                ci, kt = flat[i]
                cl = calls[ci]
                if kt == 0 and cl.get("pre_fn") is not None:
                    cl["pre_fn"]()
                ps = ps_s.next()
                cl["score_fn"](ps, 0, kt)
                cl["score_fn"](ps, 512, kt + 1)
                E = e_ring.next()
                c.act(E[:, :], ps[:, :], AF.Exp, [ps], [E], scale=scale)
                Es[i] = E

            def fill():
                if state["nxt"] < len(flat):
                    score(state["nxt"])
                    state["nxt"] += 1

            deferred = []
            for _ in range(look):
                fill()
            for i, (ci, kt) in enumerate(flat):
                fill()
                for dfr in list(deferred):
                    dfr[0] -= 1
                    if dfr[0] <= 0:
                        deferred.remove(dfr)
                        dfr[1]()
                cl = calls[ci]
                if kt == 0:
                    cl["acc_o"] = ps_o.next()
                    cl["acc_d"] = cl["acc_d_tile"] if cl.get("acc_d_tile") is not None else ps_d.next()
                acc_o, acc_d = cl["acc_o"], cl["acc_d"]
                E = Es.pop(i)
                for j in range(2):
                    vap, vt = cl["v_fn"](kt + j)
                    first = (kt + j == 0)
                    last = (kt + j == cl["nk"] - 1)
                    c.mm(acc_o[:, :], vap, E[:, j * 512:(j + 1) * 512], first, last, [vt, E], [acc_o], sig=last)
                    c.mm(acc_d[:, :], ones[:, :], E[:, j * 512:(j + 1) * 512], first, last, [ones, E], [acc_d], sig=last)
                if kt + 2 >= cl["nk"]:
                    tail = cl["post_fn"](acc_o, acc_d)
                    if tail is not None:
                        deferred.append([3, tail])
            for dfr in deferred:
                dfr[1]()

        with ExitStack() as sb_:
            c.stack = sb_
            lamt = c.sb("lamt", [128, 256], F32)
            lams = c.sb("lams", [128, 8], F32)
            ljunk = c.sb("ljunk", [128, 64], F32)
            c.dma("sp", lamt[:, :], lam_d[0:1, :].partition_broadcast(128), [lam_d], [lamt])
            for i in range(2):
                c.tt("dve", ljunk[:, :], lamt[:, i * 128:i * 128 + 64], lamt[:, i * 128 + 64:i * 128 + 128], ALU.mult,
                     [lamt], [ljunk])
                c.p.op("dve", lambda h, i=i: h.reduce_sum(lams[:, i:i + 1], ljunk[:, :], axis=AX.X), bs([ljunk]), bs([lams]))
                c.act(lams[:, 2 + i:3 + i], lams[:, i:i + 1], AF.Exp, [lams], [lams])
            c.tt("dve", lams[:, 4:5], lams[:, 3:4], lams[:, 2:3], ALU.subtract, [lams], [lams])
            c.ts("dve", lams[:, 5:6], lams[:, 4:5], -LAM_INIT, None, ALU.add, None, [lams], [lams])
            neglam = lams[:, 5:6]

            zf = {"i": 0}

            def zero_fill(n):
                for _ in range(n):
                    i = zf["i"]
                    if i < NSLOT // 128:
                        c.dma("sp", xg_d[i * 128:(i + 1) * 128, :], zrow[:, :], [zrow], [xg_d])
                    elif i == NSLOT // 128:
                        c.dma("sp", y_d[NSLOT:NSLOT + 128, :], zrow[:, :], [zrow], [y_d])
                    zf["i"] = i + 1

            qh_r = c.sb_ring("qh", 2, [128, S], BF16)
            kh_r = c.sb_ring("kh", 2, [128, 2, S], BF16)
            for _kt in kh_r.tiles:
                c.memset("pool", _kt[:, :, :], 0.0, [_kt])
            vh_r = c.sb_ring("vh", 2, [128, NT, 128], BF16)
            e_ring = c.sb_ring("E", 4, [128, 1024], BF16)
            ps_s = c.ps_ring("ps_s", 2, [128, 1024], F32)
            ps_o = c.ps_ring("ps_o", 2, [128, 512], F32)
            ps_d = c.ps_ring("ps_d", 2, [128, 512], F32)
            rec_r = c.sb_ring("rec", 3, [128, 512], F32)
            oc_r = c.sb_ring("oc", 6, [128, 512], F32)
            sq_r = c.sb_ring("sq", 3, [128, 512], BF16)
            ost_r = c.sb_ring("ost", 3, [128, 512], BF16)
            heads = {}

            def load_head(hh):
                if hh >= H or hh in heads:
                    return
                qh, kh, vh = qh_r.next(), kh_r.next(), vh_r.next()
                c.dma("sp", qh[:, :], qT_d[hh, :, :], [qT_d], [qh])
                c.dma("sp", kh[0:64, 0, :], kT_d[hh, 0:64, :], [kT_d], [kh])
                c.dma("sp", kh[64:128, 1, :], kT_d[hh, 64:128, :], [kT_d], [kh])
                c.dma("sp", vh[:, :, :], v_d[:, hh * 128:(hh + 1) * 128].rearrange("(kt p) d -> p kt d", p=128),
                      [v_d], [vh])
                heads[hh] = (qh, kh, vh)

            calls = []
            for hh in range(H):
                for qb in range(4):
                    qs_ = slice(qb * 512, (qb + 1) * 512)
                    pair = {}
                    for comp in range(2):
                        r0 = comp * 64

                        def pre_fn(hh=hh, qb=qb, comp=comp):
                            if comp == 0 and qb == 0:
                                load_head(hh)
                            if comp == 0 and qb == 1:
                                load_head(hh + 1)
                            if comp == 0:
                                zero_fill(4)

                        def score_fn(ps, off, kt, comp=comp, hh=hh, qs_=qs_):
                            qh, kh, vh = heads[hh]
                            c.mm(ps[:, off:off + 512], kh[:, comp, kt * 128:(kt + 1) * 128], qh[:, qs_], True, True,
                                 [kh, qh], [ps], sig=(off == 512))

                        def v_fn(kt, hh=hh):
                            vh = heads[hh][2]
                            return vh[:, kt, :], vh

                        def post_fn(acc_o, acc_d, hh=hh, qs_=qs_, comp=comp, pair=pair):
                            rec = rec_r.next()
                            c.recip(rec[:, :], acc_d[:, :], [acc_d], [rec])
                            oc = oc_r.next()
                            c.tt("dve", oc[:, :], acc_o[:, :], rec[:, :], ALU.mult, [acc_o, rec], [oc])
                            pair[comp] = oc
                            if comp == 0:
                                return
                            o = oc_r.next()
                            c.stt(o[:, :], pair[1][:, :], neglam, pair[0][:, :], ALU.mult, ALU.add,
                                  [pair[0], pair[1], lams], [o])
                            sq = sq_r.next()
                            c.tt("pool", sq[:, :], o[:, :], o[:, :], ALU.mult, [o], [sq])
                            return lambda: subln_tail(o, sq, hh, qs_)

                        def subln_tail(o, sq, hh, qs_):
                            pn = ps_d.tiles[1]
                            c.mm(pn[:, :], ones[:, :], sq[:, :], True, True, [ones, sq], [pn])
                            rec = rec_r.next()
                            c.ts("dve", rec[:, :], pn[:, :], 1.0 / 128.0, 1e-5, ALU.mult, ALU.add, [pn], [rec])
                            c.act(rec[:, :], rec[:, :], AF.Ln, [rec], [rec])
                            c.act(rec[:, :], rec[:, :], AF.Exp, [rec], [rec], scale=-0.5)
                            c.tt("pool", o[:, :], o[:, :], rec[:, :], ALU.mult, [o, rec], [o])
                            ost = ost_r.next()
                            c.ts("pool", ost[:, :], o[:, :], gT[:, 54:55], 1.0 - LAM_INIT, ALU.mult, ALU.mult, [o, gT], [ost])
                            c.dma("sp", oa_d[hh, :, qs_], ost[:, :], [ost], [oa_d])

                        calls.append({"nk": NT, "score_fn": score_fn, "v_fn": v_fn, "post_fn": post_fn, "pre_fn": pre_fn,
                                      "acc_d_tile": ps_d.tiles[comp]})
            attn_stream(calls, ps_s, e_ring, ps_o, ps_d, 0.125, 1)
            c.barrier()
        c.stack = outer
        if "oa_s" in dump:
            final_outs.append(oa_d.b)
        if stop_after == "B":
            stop_here()
            return nc

        with ExitStack() as sc_:
            c.stack = sc_
            cqn = c.sb("cqn", [128, 4, S], BF16)
            ckvn = c.sb("ckvn", [128, 2, S], BF16)
            kpe = c.sb("kpe", [128, S], BF16)
            c.memset("pool", kpe[:, :], 0.0, [kpe])
            Cm = c.sb("Cm2", [64, S], F32)
            Sm = c.sb("Sm2", [64, S], F32)
            c.dma("sp", kpe[0:64, :], kpe_d[:, :], [kpe_d], [kpe])
            with ExitStack() as sc1:
                c.stack = sc1
                tmp_i = c.sb("tmp_i2", [64, S], I32)
                tmp_f = c.sb("tmp_f2", [64, S], F32)
                rope_tables(c, pos_d, cf, 1, 64, Cm, Sm, tmp_i, tmp_f)
                c.barrier()
            with ExitStack() as sc2:
                c.stack = sc2
                cf32 = c.sb("cf32", [128, 4, S], F32)
                sq_r = c.sb_ring("sqc", 2, [128, 512], BF16)
                ps_n = c.ps_ring("ps_nc", 2, [128, 512], F32)
                rec_r = c.sb_ring("recc", 2, [128, 512], F32)
                for src_d, nch, dst, goff in ((cq_d, 4, cqn, 48), (ckv_d, 2, ckvn, 52)):
                    for ch in range(nch):
                        c.dma("sp", cf32[:, ch, :], src_d[ch, :, :], [src_d], [cf32])
                    for blk in range(4):
                        cs = slice(blk * 512, (blk + 1) * 512)
                        pn = ps_n.next()
                        for ch in range(nch):
                            sq = sq_r.next()
                            c.act(sq[:, :], cf32[:, ch, cs], AF.Square, [cf32], [sq])
                            c.mm(pn[:, :], ones[:, :], sq[:, :], ch == 0, ch == nch - 1, [ones, sq], [pn], sig=True)
                        rec = rec_r.next()
                        c.ts("dve", rec[:, :], pn[:, :], 1.0 / (nch * 128), EPS, ALU.mult, ALU.add, [pn], [rec])
                        c.p.op("act", lambda h, rec=rec: h.sqrt(rec[:, :], rec[:, :]), bs([rec]), bs([rec]))
                        c.recip(rec[:, :], rec[:, :], [rec], [rec])
                        for ch in range(nch):
                            c.stt(dst[:, ch, cs], cf32[:, ch, cs], gT[:, goff + ch:goff + ch + 1], rec[:, :],
                                  ALU.mult, ALU.mult, [cf32, gT, rec], [dst])
                c.barrier()
            if stop_after == "C1":
                dbg = dram("cqn_dbg", [128, 4, S], BF16)
                c.dma("sp", dbg[:, :, :], cqn[:, :, :], [cqn], [dbg])
                final_outs.append(dbg.b)
                stop_here()
                return nc
            c.stack = sc_
            psA = c.ps_ring("psAC", 2, [128, 512], F32)
            rg = {"qs": c.sb_ring("qsC", 2, [128, 512], BF16), "psr": psA,
                  "t1": c.sb_ring("t1C", 2, [128, 512], F32), "t2": c.sb_ring("t2C", 2, [128, 512], F32)}
            slq_r = c.sb_ring("slq", 2, [128, 4, 256], BF16)
            for _st in slq_r.tiles:
                c.memset("pool", _st[:, :, :], 0.0, [_st])
            slkv_r = c.sb_ring("slkv", 2, [128, 2, 256], BF16)
            qn_r = c.sb_ring("qn", 2, [128, S], BF16)
            qp_r = c.sb_ring("qp", 2, [128, S], BF16)
            for _qt in qp_r.tiles:
                c.memset("pool", _qt[:, :], 0.0, [_qt])
            kn_r = c.sb_ring("kn", 2, [128, S], BF16)
            vh_r = c.sb_ring("vhC", 2, [128, NT, 128], BF16)
            ps_s = c.ps_ring("ps_sC", 2, [128, 1024], F32)
            ps_o = c.ps_ring("ps_oC", 1, [128, 512], F32)
            ps_d = c.ps_ring("ps_dC", 1, [128, 512], F32)
            rec_r = c.sb_ring("recC", 2, [128, 512], F32)
            ost_r = c.sb_ring("ostC", 3, [128, 512], BF16)
            e_ring = c.sb_ring("EC", 4, [128, 1024], BF16)
            heads = {}

            def prep_head(hh):
                if hh >= H or hh in heads:
                    return
                slq, slkv = slq_r.next(), slkv_r.next()
                c.dma("pool", slq[:, :, 0:192], w_uq_d[:, hh * 192:(hh + 1) * 192].rearrange("(kc p) n -> p kc n", p=128),
                      [w_uq_d], [slq])
                c.dma("pool", slkv[:, :, :], w_ukv_d[:, hh * 256:(hh + 1) * 256].rearrange("(kc p) n -> p kc n", p=128),
                      [w_ukv_d], [slkv])
                qn, qp, kn, vh = qn_r.next(), qp_r.next(), kn_r.next(), vh_r.next()
                heads[hh] = (qn, qp, kn, vh)
                for blk in range(4):
                    cs = slice(blk * 512, (blk + 1) * 512)
                    ps = psA.next()
                    for kc in range(4):
                        c.mm(ps[:, :], slq[:, kc, 0:128], cqn[:, kc, cs], kc == 0, kc == 3, [slq, cqn], [ps])
                    c.copy("dve", qn[:, cs], ps[:, :], [ps], [qn])
                    ps = psA.next()
                    for kc in range(4):
                        c.mm(ps[:, :], slq[:, kc, 128:256], cqn[:, kc, cs], kc == 0, kc == 3, [slq, cqn], [ps])
                    rope_evac(c, ps, 64, blk, rm, Cm, Sm, rg, qp[:64, cs], qp, kfull=True)
                    ps = psA.next()
                    for kc in range(2):
                        c.mm(ps[:, :], slkv[:, kc, 0:128], ckvn[:, kc, cs], kc == 0, kc == 1, [slkv, ckvn], [ps])
                    c.copy("dve", kn[:, cs], ps[:, :], [ps], [kn])
                for t4 in range(NT // 4):
                    ps = psA.next()
                    for j in range(4):
                        t = t4 * 4 + j
                        for kc in range(2):
                            c.mm(ps[:, j * 128:(j + 1) * 128], ckvn[:, kc, t * 128:(t + 1) * 128], slkv[:, kc, 128:256],
                                 kc == 0, kc == 1, [slkv, ckvn], [ps], sig=(kc == 1 and j == 3))
                    c.copy("dve", vh[:, t4 * 4:(t4 + 1) * 4, :],
                           ps[:, :].rearrange("p (a b) -> p a b", a=4), [ps], [vh])

            calls = []
            for hh in range(H):
                for qb in range(4):
                    qs_ = slice(qb * 512, (qb + 1) * 512)

                    def pre_fn(hh=hh, qb=qb):
                        if qb == 0:
                            prep_head(hh)
                        if qb == 2:
                            prep_head(hh + 1)

                    def score_fn(ps, off, kt, hh=hh, qs_=qs_):
                        qn, qp, kn, vh = heads[hh]
                        ks = slice(kt * 128, (kt + 1) * 128)
                        c.mm(ps[:, off:off + 512], kn[:, ks], qn[:, qs_], True, False, [kn, qn], [ps], sig=False)
                        c.mm(ps[:, off:off + 512], kpe[:, ks], qp[:, qs_], False, True, [kpe, qp], [ps], sig=(off == 512))

                    def v_fn(kt, hh=hh):
                        vh = heads[hh][3]
                        return vh[:, kt, :], vh

                    def post_fn(acc_o, acc_d, hh=hh, qs_=qs_):
                        rec = rec_r.next()
                        c.recip(rec[:, :], acc_d[:, :], [acc_d], [rec])
                        ost = ost_r.next()
                        c.tt("dve", ost[:, :], acc_o[:, :], rec[:, :], ALU.mult, [acc_o, rec], [ost])
                        c.dma("sp", ob_d[hh, :, qs_], ost[:, :], [ost], [ob_d])

                    calls.append({"nk": NT, "score_fn": score_fn, "v_fn": v_fn, "post_fn": post_fn, "pre_fn": pre_fn})
            attn_stream(calls, ps_s, e_ring, ps_o, ps_d, 192.0 ** -0.5, 1)
            c.barrier()
        c.stack = outer
        if "ob_s" in dump:
            final_outs.append(ob_d.b)
        if stop_after == "C":
            stop_here()
            return nc

        with ExitStack() as sd_:
            c.stack = sd_
            mergedT = c.sb("mergedT", [128, DC, S], BF16)
            with ExitStack() as sd1:
                c.stack = sd1
                oaT = c.sb("oaT", [128, H, S], BF16)
                obT = c.sb("obT", [128, H, S], BF16)
                for hh in range(H):
                    c.dma("sp", oaT[:, hh, :], oa_d[hh, :, :], [oa_d], [oaT])
                    c.dma("sp", obT[:, hh, :], ob_d[hh, :, :], [ob_d], [obT])
                sla_r = c.sb_ring("sla", 2, [128, H, 512], BF16)
                slb_r = c.sb_ring("slb", 2, [128, H, 512], BF16)
                ga_r = c.sb_ring("gaT", 2, [128, S], BF16)
                gb_r = c.sb_ring("gbT", 2, [128, S], BF16)
                ps_a = c.ps_ring("ps_a", 3, [128, 512], F32)
                ps_b = c.ps_ring("ps_b", 3, [128, 512], F32)
                t1_r = c.sb_ring("t1D", 2, [128, 512], F32)
                t2_r = c.sb_ring("t2D", 2, [128, 512], F32)
                slabs_d = {}
                gates_d = {}

                def load_slabs_d(c4):
                    if c4 >= 4 or c4 in slabs_d:
                        return
                    sla, slb = sla_r.next(), slb_r.next()
                    c.dma("pool", sla[:, :, :], w_od_d[:, c4 * 512:(c4 + 1) * 512].rearrange("(kc p) n -> p kc n", p=128),
                          [w_od_d], [sla])
                    c.dma("pool", slb[:, :, :], w_om_d[:, c4 * 512:(c4 + 1) * 512].rearrange("(kc p) n -> p kc n", p=128),
                          [w_om_d], [slb])
                    slabs_d[c4] = (sla, slb)

                def load_gates_d(ch):
                    if ch >= DC or ch in gates_d:
                        return
                    ga, gb = ga_r.next(), gb_r.next()
                    c.dma("sp", ga[:, :], ga_d[ch, :, :], [ga_d], [ga])
                    c.dma("sp", gb[:, :], gb_d[ch, :, :], [gb_d], [gb])
                    gates_d[ch] = (ga, gb)

                load_slabs_d(0)
                load_gates_d(0)
                for c4 in range(4):
                    sla, slb = slabs_d.pop(c4)
                    for j in range(4):
                        ch = c4 * 4 + j
                        ga, gb = gates_d.pop(ch)
                        load_gates_d(ch + 1)
                        if j == 1:
                            load_slabs_d(c4 + 1)
                        for blk in range(4):
                            cs = slice(blk * 512, (blk + 1) * 512)
                            pa, pb = ps_a.next(), ps_b.next()
                            for kc in range(H):
                                c.mm(pa[:, :], sla[:, kc, j * 128:(j + 1) * 128], oaT[:, kc, cs], kc == 0, kc == H - 1,
                                     [sla, oaT], [pa])
                            for kc in range(H):
                                c.mm(pb[:, :], slb[:, kc, j * 128:(j + 1) * 128], obT[:, kc, cs], kc == 0, kc == H - 1,
                                     [slb, obT], [pb])
                            t1, t2 = t1_r.next(), t2_r.next()
                            c.tt("dve", t1[:, :], pa[:, :], ga[:, cs], ALU.mult, [pa, ga], [t1])
                            c.tt("dve", t2[:, :], pb[:, :], gb[:, cs], ALU.mult, [pb, gb], [t2])
                            c.tt("pool", mergedT[:, ch, cs], t1[:, :], t2[:, :], ALU.add, [t1, t2], [mergedT])
                c.barrier()
            c.stack = sd_
            wout = c.sb("wout", [128, DC, D], BF16)
            for nb in range(4):
                c.dma("pool", wout[:, :, nb * 512:(nb + 1) * 512],
                      w_out_d[:, nb * 512:(nb + 1) * 512].rearrange("(kc p) n -> p kc n", p=128), [w_out_d], [wout])
            xin = c.sb_ring("xinD", 2, [128, D], F32)
            hout = c.sb_ring("houtD", 2, [128, D], F32)
            ps_h = c.ps_ring("ps_h", 4, [128, 512], F32)
            for t in range(NT):
                ts_ = slice(t * 128, (t + 1) * 128)
                xt, ht = xin.next(), hout.next()
                c.dma("sp", xt[:, :], x_d[ts_, :], [x_d], [xt])
                for nb in range(4):
                    ns = slice(nb * 512, (nb + 1) * 512)
                    ps = ps_h.next()
                    for kc in range(DC):
                        c.mm(ps[:, :], mergedT[:, kc, ts_], wout[:, kc, ns], kc == 0, kc == DC - 1, [mergedT, wout], [ps])
                    c.tt("dve", ht[:, ns], ps[:, :], xt[:, ns], ALU.add, [ps, xt], [ht])
                c.dma("sp", h1_d[ts_, :], ht[:, :], [ht], [h1_d])
            c.barrier()
        c.stack = outer
        if "h1_s" in dump:
            final_outs.append(h1_d.b)
        if stop_after == "D":
            stop_here()
            return nc

        with ExitStack() as se_:
            c.stack = se_
            qcT = c.sb("qcT", [128, 4, S], BF16)
            kcT = c.sb("kcT", [128, 4, 256], BF16)
            vc = c.sb("vc", [128, 2, 512], BF16)
            ocT = c.sb("ocT", [128, 4, S], BF16)
            with ExitStack() as se1:
                c.stack = se1
                hn1T_b = [c.sb("hn1T%d" % i, [128, DC, 512], BF16) for i in range(4)]
                memnT = c.sb("memnT", [128, DC, 256], BF16)
                rg = {"junk": c.sb_ring("junkE", 1, [128, D], BF16), "ss": c.sb_ring("ssE", 4, [128, 4], F32),
                      "xs": c.sb_ring("xsE", 2, [128, D], BF16), "pst": c.ps_ring("pstE", 2, [128, 512], BF16)}
                xin = c.sb_ring("xinE", 3, [128, D], F32)
                slab_r = c.sb_ring("slabE", 2, [128, DC, 512], BF16)
                psA = c.ps_ring("psAE", 4, [128, 512], F32)
                for mt in range(2):
                    xt = xin.next()
                    c.dma("sp", xt[:, :], mem_d[mt * 128:(mt + 1) * 128, :], [mem_d], [xt])
                    norm_transpose_tile(c, xt, gT, 32, memnT, mt * 128, rg, ident)
                slq_ = slab_r.next()
                c.dma("pool", slq_[:, :, :], w_cq_d[:, :].rearrange("(kc p) n -> p kc n", p=128), [w_cq_d], [slq_])

                def load_h1(t):
                    xt = xin.next()
                    c.dma("sp", xt[:, :], h1_d[t * 128:(t + 1) * 128, :], [h1_d], [xt])
                    return xt

                def q_proj_block(t):
                    if t % 4 != 3:
                        return
                    blk = t // 4
                    cs = slice(blk * 512, (blk + 1) * 512)
                    for hh in range(4):
                        ps = psA.next()
                        for kc in range(DC):
                            c.mm(ps[:, :], slq_[:, kc, hh * 128:(hh + 1) * 128], hn1T_b[blk][:, kc, :], kc == 0, kc == DC - 1,
                                 [slq_, hn1T_b[blk]], [ps])
                        c.copy("act" if hh % 2 == 0 else "dve", qcT[:, hh, cs], ps[:, :], [ps], [qcT])

                norm_transpose_stream(c, NT, load_h1, gT, 16, None, rg, ident,
                                      dst_fn=lambda t: (hn1T_b[t // 4], (t % 4) * 128), after_fn=q_proj_block)
                sl = slab_r.next()
                c.dma("pool", sl[:, :, :], w_ckv_d[:, 0:512].rearrange("(kc p) n -> p kc n", p=128), [w_ckv_d], [sl])
                for hh in range(4):
                    ps = psA.next()
                    for kc in range(DC):
                        c.mm(ps[:, 0:256], sl[:, kc, hh * 128:(hh + 1) * 128], memnT[:, kc, :], kc == 0, kc == DC - 1,
                             [sl, memnT], [ps])
                    c.copy("act", kcT[:, hh, :], ps[:, 0:256], [ps], [kcT])
                sl = slab_r.next()
                c.dma("pool", sl[:, :, :], w_ckv_d[:, 512:1024].rearrange("(kc p) n -> p kc n", p=128), [w_ckv_d], [sl])
                for mt in range(2):
                    ps = psA.next()
                    for kc in range(DC):
                        c.mm(ps[:, :], memnT[:, kc, mt * 128:(mt + 1) * 128], sl[:, kc, :], kc == 0, kc == DC - 1,
                             [sl, memnT], [ps])
                    c.copy("dve", vc[:, mt, :], ps[:, :], [ps], [vc])
                c.barrier()
            with ExitStack() as se2:
                c.stack = se2
                e_ring = c.sb_ring("EE", 4, [128, 512], BF16)
                ps_s = c.ps_ring("ps_sE", 3, [128, 512], F32)
                ps_o = c.ps_ring("ps_oE", 2, [128, 512], F32)
                ps_d = c.ps_ring("ps_dE", 2, [128, 512], F32)
                rec_r = c.sb_ring("recE", 2, [128, 512], F32)
                for hh in range(4):
                    for qb in range(4):
                        qs_ = slice(qb * 512, (qb + 1) * 512)
                        acc_o, acc_d = ps_o.next(), ps_d.next()

                        def score_fn(ps, kt, hh=hh, qs_=qs_):
                            c.mm(ps[:, :], kcT[:, hh, kt * 128:(kt + 1) * 128], qcT[:, hh, qs_], True, True,
                                 [kcT, qcT], [ps])

                        def v_fn(kt, hh=hh):
                            return vc[:, kt, hh * 128:(hh + 1) * 128], vc

                        attn_core(2, qb, score_fn, v_fn, ps_s, e_ring, acc_o, acc_d, 128.0 ** -0.5)
                        rec = rec_r.next()
                        c.recip(rec[:, :], acc_d[:, :], [acc_d], [rec])
                        c.tt("dve", ocT[:, hh, qs_], acc_o[:, :], rec[:, :], ALU.mult, [acc_o, rec], [ocT])
                c.barrier()
            with ExitStack() as se3:
                c.stack = se3
                wco = c.sb("wco", [128, 4, D], BF16)
                c.dma("pool", wco[:, :, :], w_co_d[:, :].rearrange("(kc p) n -> p kc n", p=128), [w_co_d], [wco])
                gff = c.sb("gff", [128, D], F32)
                c.dma("sp", gff[:, :], grow_d[0:1, :].partition_broadcast(128), [grow_d], [gff])
                wr = c.sb("wr", [128, DC, 36], BF16)
                c.dma("pool", wr[:, :, :], w_r_d[:, :].rearrange("(kc p) n -> p kc n", p=128), [w_r_d], [wr])
                br = c.sb("br", [128, 36], F32)
                c.dma("sp", br[:, :], b_r_d[0:1, :].partition_broadcast(128), [b_r_d], [br])
                A_all = c.sb("A_all", [128, NT, 32], BF16)
                hin = c.sb_ring("hinE", 2, [128, D], F32)
                hout = c.sb_ring("houtE", 2, [128, D], F32)
                ttok_r = c.sb_ring("ttok", 3, [128, D], BF16)
                tTs_r = c.sb_ring("tTs", 2, [128, DC, 128], BF16)
                junk_r = c.sb_ring("junkE3", 1, [128, D], BF16)
                ss_r = c.sb_ring("ssE3", 4, [128, 4], F32)
                rt_r = c.sb_ring("rt", 3, [128, 320], F32)
                ps_h = c.ps_ring("ps_hE", 3, [128, 512], F32)
                pst_r = c.ps_ring("pstE3", 2, [128, 512], BF16)
                ps_l = c.ps_ring("ps_l", 2, [128, 64], F32)
                ps_r = c.ps_ring("ps_r", 1, [128, 64], F32)
                pend_tail = []
                for t in range(NT):
                    ts_ = slice(t * 128, (t + 1) * 128)
                    xt, ht = hin.next(), hout.next()
                    c.dma("sp", xt[:, :], h1_d[ts_, :], [h1_d], [xt])
                    for nb in range(4):
                        ns = slice(nb * 512, (nb + 1) * 512)
                        ps = ps_h.next()
                        for kc in range(4):
                            c.mm(ps[:, :], ocT[:, kc, ts_], wco[:, kc, ns], kc == 0, kc == 3, [ocT, wco], [ps])
                        c.tt("dve", ht[:, ns], ps[:, :], xt[:, ns], ALU.add, [ps, xt], [ht])
                    c.dma("sp", h2_d[ts_, :], ht[:, :], [ht], [h2_d])
                    ss, junk, ttok = ss_r.next(), junk_r.next(), ttok_r.next()
                    rmsnorm_rstd(c, ht, D, ss, junk, EPS)
                    c.stt(ttok[:, :], ht[:, :], ss[:, 2:3], gff[:, :], ALU.mult, ALU.mult, [ht, ss, gff], [ttok])
                    tTs = tTs_r.next()
                    for g4 in range(DC // 4):
                        pt = pst_r.next()
                        for j in range(4):
                            ch = g4 * 4 + j
                            c.tr(pt[:, j * 128:(j + 1) * 128], ttok[:, ch * 128:(ch + 1) * 128], ident[:, :],
                                 [ttok, ident], [pt], sig=(j == 3))
                        c.copy("act" if g4 % 2 == 0 else "dve", tTs[:, g4 * 4:(g4 + 1) * 4, :],
                               pt[:, :].rearrange("p (a b) -> p a b", a=4), [pt], [tTs])
                    pl = ps_l.next()
                    for kc in range(DC):
                        c.mm(pl[:, 0:36], tTs[:, kc, :], wr[:, kc, :], kc == 0, kc == DC - 1, [tTs, wr], [pl])
                    rt = rt_r.next()
                    R_ = [rt]
                    L = rt[:, 0:36]
                    c.tt("dve", L, pl[:, 0:36], br[:, :], ALU.add, [pl, br], R_)
                    gmax, ngmax, gsum, gp = rt[:, 40:41], rt[:, 41:42], rt[:, 42:43], rt[:, 43:44]
                    c.p.op("dve", lambda h, rt=rt: h.reduce_max(rt[:, 40:41], rt[:, 0:4], axis=AX.X), bs(R_), bs(R_))
                    c.ts("pool", ngmax, gmax, -1.0, None, ALU.mult, None, R_, R_)
                    c.act(rt[:, 44:48], rt[:, 0:4], AF.Exp, R_, R_, bias=ngmax, scale=1.0, accum=gsum)
                    c.recip(gp, gsum, R_, R_)
                    c.ts("pool", rt[:, 48:52], rt[:, 0:4], gmax, None, ALU.is_ge, None, R_, R_)
                    c.ts("pool", rt[:, 52:56], rt[:, 48:52], -1.0, 1e30, ALU.add, ALU.mult, R_, R_)
                    for g in range(4):
                        c.ts("pool", rt[:, 64 + g * 8:72 + g * 8], rt[:, 4 + g * 8:12 + g * 8], rt[:, 52 + g:53 + g], None,
                             ALU.add, None, R_, R_)
                    Lm = rt[:, 64:96]
                    c.p.op("dve", lambda h, rt=rt: h.max(rt[:, 96:104], rt[:, 64:96]), bs(R_), bs(R_))
                    c.ts("pool", rt[:, 104:136], Lm, rt[:, 96:97], None, ALU.is_equal, None, R_, R_)
                    c.ts("pool", rt[:, 136:168], Lm, rt[:, 97:98], None, ALU.is_equal, None, R_, R_)
                    c.tt("pool", rt[:, 56:57], rt[:, 97:98], rt[:, 96:97], ALU.subtract, R_, R_)
                    c.act(rt[:, 57:58], rt[:, 56:57], AF.Exp, R_, R_)
                    c.ts("pool", rt[:, 58:59], rt[:, 57:58], 1.0, None, ALU.add, None, R_, R_)
                    c.recip(rt[:, 58:59], rt[:, 58:59], R_, R_)
                    c.tt("pool", wts[:, t, 0:1], rt[:, 58:59], gp, ALU.mult, R_, [wts])
                    c.stt(wts[:, t, 1:2], rt[:, 57:58], rt[:, 58:59], gp, ALU.mult, ALU.mult, R_, [wts])
                    c.tt("pool", A_all[:, t, :], rt[:, 104:136], rt[:, 136:168], ALU.add, R_, [A_all])
                    def tail(t=t, rt=rt, R_=R_, ttok=ttok):
                        pr = ps_r.next()
                        c.mm(pr[:, 0:32], Utri[:, :], A_all[:, t, :], True, t == 0, [Utri, A_all], [pr], sig=(t == 0))
                        for tp in range(t):
                            c.mm(pr[:, 0:32], ones[:, :], A_all[:, tp, :], False, tp == t - 1, [ones, A_all], [pr],
                                 sig=(tp == t - 1))
                        c.ts("dve", rt[:, 200:232], pr[:, 0:32], float(CAP), 1e6, ALU.is_ge, ALU.mult, [pr], R_)
                        c.tt("dve", rt[:, 168:200], pr[:, 0:32], cf[:, 2:2 + NE], ALU.add, [pr, cf], R_)
                        c.tt("pool", rt[:, 168:200], rt[:, 168:200], rt[:, 200:232], ALU.add, R_, R_)
                        for k, oh0 in ((0, 104), (1, 136)):
                            c.tt("pool", rt[:, 232 + 32 * k:264 + 32 * k], rt[:, oh0:oh0 + 32], rt[:, 168:200], ALU.mult, R_, R_)
                            c.p.op("dve", lambda h, rt=rt, k=k: h.reduce_sum(rt[:, 59 + k:60 + k], rt[:, 232 + 32 * k:264 + 32 * k],
                                                                           axis=AX.X), bs(R_), bs(R_))
                            c.ts("pool", rt[:, 59 + k:60 + k], rt[:, 59 + k:60 + k], float(NSLOT), None, ALU.min, None, R_, R_)
                            c.copy("pool", slot_i[:, t, k:k + 1], rt[:, 59 + k:60 + k], R_, [slot_i])
                        for k in range(0 if NO_SCATTER else 2):
                            c.p.dma("pool", lambda h, ttok=ttok, t=t, k=k: h.indirect_dma_start(
                                out=xg_d[:, :], out_offset=bass.IndirectOffsetOnAxis(ap=slot_i[:, t, k:k + 1], axis=0),
                                in_=ttok[:, :], in_offset=None, bounds_check=bc_reg, oob_is_err=False),
                                bs([ttok, slot_i]), bs([xg_d]))

                    if pend_tail:
                        pend_tail.pop(0)()
                    pend_tail.append(tail)
                while pend_tail:
                    pend_tail.pop(0)()
                c.barrier()
        c.stack = outer
        for nm, tl in (("h2_s", h2_d), ("xg_s", xg_d)):
            if nm in dump:
                final_outs.append(tl.b)
        if "route" in dump:
            rdump = dram("route", [128, NT, 4], F32)
            c.dma("sp", rdump[:, :, 2:4], wts[:, :, :], [wts], [rdump])
            sfl = c.sb("sfl", [128, NT, 2], F32)
            c.copy("dve", sfl[:, :, :], slot_i[:, :, :], [slot_i], [sfl])
            c.dma("sp", rdump[:, :, 0:2], sfl[:, :, :], [sfl], [rdump])
            final_outs.append(rdump.b)
        if stop_after == "E":
            stop_here()
            return nc

        with ExitStack() as sf_:
            c.stack = sf_
            wg_r = c.sb_ring("wg", 2, [128, DC, DFF], BF16)
            wu_r = c.sb_ring("wu", 2, [128, DC, DFF], BF16)
            wd_r = c.sb_ring("wd", 2, [128, 4, D], BF16)
            xgT_r = c.sb_ring("xgT", 2, [128, DC, CAP], BF16)
            hid_r = c.sb_ring("hid", 2, [128, 4, CAP], BF16)
            sg_r = c.sb_ring("sg", 2, [128, CAP], F32)
            y_r = c.sb_ring("yt", 3, [128, D], BF16)
            pst_r = c.ps_ring("pstF", 2, [128, 512], BF16)
            ps_g = c.ps_ring("ps_g", 2, [128, CAP], F32)
            ps_u = c.ps_ring("ps_u", 2, [128, CAP], F32)
            ps_y = c.ps_ring("ps_y", 2, [128, 512], F32)
            NSB = CAP // 128
            xg_r6 = c.sb_ring("xgt6", 2 * NSB, [128, D], BF16)
            W = {}
            XT = {}
            TG = {}

            def load_weights(e):
                if e >= NE:
                    return
                wg, wu, wd = wg_r.next(), wu_r.next(), wd_r.next()
                c.dma("pool", wg[:, :, :], w_eg_d[e, :, :].rearrange("(p kc) n -> p kc n", p=128), [w_eg_d], [wg])
                c.dma("pool", wu[:, :, :], w_eu_d[e, :, :].rearrange("(p kc) n -> p kc n", p=128), [w_eu_d], [wu])
                c.dma("pool", wd[:, :, :], w_ed_d[e, :, :].rearrange("(p kc) n -> p kc n", p=128), [w_ed_d], [wd])
                W[e] = (wg, wu, wd)

            def load_xg(e):
                if e >= NE:
                    return
                xgT = xgT_r.next()
                XT[e] = xgT
                groups = []
                for sb in range(NSB):
                    xg = xg_r6.next()
                    r0 = e * CAP + sb * 128
                    c.dma("sp", xg[:, :], xg_d[r0:r0 + 128, :], [xg_d], [xg])
                    for g4 in range(DC // 4):
                        groups.append((xg, sb, g4))
                TG[e] = groups

            def transpose_groups(e, n):
                if e >= NE:
                    return
                xgT = XT[e]
                for _ in range(n):
                    if not TG[e]:
                        return
                    xg, sb, g4 = TG[e].pop(0)
                    pt = pst_r.next()
                    for j in range(4):
                        ch = g4 * 4 + j
                        c.tr(pt[:, j * 128:(j + 1) * 128], xg[:, ch:D:DC], ident[:, :], [xg, ident], [pt], sig=(j == 3))
                    c.copy("act" if g4 % 2 == 0 else "dve", xgT[:, g4 * 4:(g4 + 1) * 4, sb * 128:(sb + 1) * 128],
                           pt[:, :].rearrange("p (a b) -> p a b", a=4), [pt], [xgT])

            load_xg(0)
            load_xg(1)
            load_weights(0)
            transpose_groups(0, 4 * NSB)
            for e in range(NE):
                load_xg(e + 2)
                load_weights(e + 1)
                wg, wu, wd = W.pop(e)
                xgT = XT.pop(e)
                hid = hid_r.next()
                for dc in range(4):
                    pg, pu = ps_g.next(), ps_u.next()
                    for kc in range(DC):
                        c.mm(pg[:, :], wg[:, kc, dc:DFF:4], xgT[:, kc, :], kc == 0, kc == DC - 1,
                             [wg, xgT], [pg])
                    for kc in range(DC):
                        c.mm(pu[:, :], wu[:, kc, dc:DFF:4], xgT[:, kc, :], kc == 0, kc == DC - 1,
                             [wu, xgT], [pu])
                    transpose_groups(e + 1, 2)
                    sg = sg_r.next()
                    c.act(sg[:, :], pg[:, :], AF.Silu, [pg], [sg])
                    c.tt("dve", hid[:, dc, :], sg[:, :], pu[:, :], ALU.mult, [sg, pu], [hid])
                for sb in range(NSB):
                    yt = y_r.next()
                    for nb in range(4):
                        ns = slice(nb * 512, (nb + 1) * 512)
                        py = ps_y.next()
                        for dc in range(4):
                            c.mm(py[:, :], hid[:, dc, sb * 128:(sb + 1) * 128], wd[:, dc, ns], dc == 0, dc == 3,
                                 [hid, wd], [py])
                        c.copy("act" if nb % 2 == 0 else "dve", yt[:, ns], py[:, :], [py], [yt])
                    transpose_groups(e + 1, 2)
                    r0 = e * CAP + sb * 128
                    c.dma("sp", y_d[r0:r0 + 128, :], yt[:, :], [yt], [y_d])
                transpose_groups(e + 1, 4 * NSB)
            c.barrier()
        c.stack = outer
        if "y_s" in dump:
            final_outs.append(y_d.b)
        if stop_after == "F":
            stop_here()
            return nc

        with ExitStack() as sg_:
            c.stack = sg_
            gfin = c.sb("gfin", [128, D], F32)
            c.dma("sp", gfin[:, :], grow_d[1:2, :].partition_broadcast(128), [grow_d], [gfin])
            hin = c.sb_ring("hinG", 3, [128, D], F32)
            y1_r = c.sb_ring("y1", 3, [128, D], BF16)
            y2_r = c.sb_ring("y2", 3, [128, D], BF16)
            h3_r = c.sb_ring("h3", 3, [128, D], F32)
            o_r = c.sb_ring("og", 2, [128, D], F32)
            junk_r = c.sb_ring("junkG", 1, [128, D], BF16)
            ss_r = c.sb_ring("ssG", 4, [128, 4], F32)
            def g_phase1(t):
                ts_ = slice(t * 128, (t + 1) * 128)
                ht, y1, y2, h3 = hin.next(), y1_r.next(), y2_r.next(), h3_r.next()
                c.dma("sp", ht[:, :], h2_d[ts_, :], [h2_d], [ht])
                for k, yk in ((0, y1), (1, y2)):
                    c.memset("pool", yk[:, :], 0.0, [yk])
                    c.p.dma("pool", lambda h, yk=yk, t=t, k=k: h.indirect_dma_start(
                        out=yk[:, :], out_offset=None, in_=y_d[:, :],
                        in_offset=bass.IndirectOffsetOnAxis(ap=slot_i[:, t, k:k + 1], axis=0),
                        bounds_check=bc_reg, oob_is_err=False), bs([y_d, slot_i]), bs([yk]))
                c.stt(h3[:, :], y1[:, :], wts[:, t, 0:1], ht[:, :], ALU.mult, ALU.add, [y1, wts, ht], [h3])
                c.stt(h3[:, :], y2[:, :], wts[:, t, 1:2], h3[:, :], ALU.mult, ALU.add, [y2, wts, h3], [h3])
                ss, junk = ss_r.next(), junk_r.next()
                c.act(junk[:, :D], h3[:, :D], AF.Square, [h3], [junk, ss], accum=ss[:, 0:1])
                return h3, ss

            def g_phase2(t, h3, ss):
                ts_ = slice(t * 128, (t + 1) * 128)
                og = o_r.next()
                c.ts("dve", ss[:, 1:2], ss[:, 0:1], 1.0 / D, EPS, ALU.mult, ALU.add, [ss], [ss])
                c.p.op("act", lambda h: h.sqrt(ss[:, 3:4], ss[:, 1:2]), bs([ss]), bs([ss]))
                c.recip(ss[:, 2:3], ss[:, 3:4], [ss], [ss])
                c.stt(og[:, :], h3[:, :], ss[:, 2:3], gfin[:, :], ALU.mult, ALU.mult, [h3, ss, gfin], [og])
                c.dma("sp", out_d[ts_, :], og[:, :], [og], [out_d])

            cur = g_phase1(0)
            for t in range(NT):
                nxt = g_phase1(t + 1) if t + 1 < NT else None
                g_phase2(t, *cur)
                cur = nxt
            final_outs.append(out_d.b)
            finish()
        c.stack = outer
    return nc


def _fm(g):
    g = np.asarray(g, np.float32).reshape(-1)
    return np.ascontiguousarray(g.reshape(-1, 128).T)


def host_shared(inputs):
    f32 = np.float32
    bf = ml_dtypes.bfloat16
    m = {}
    for k in ("w_in", "w_o_diff", "w_uq", "w_ukv", "w_o_mla", "w_out", "w_cq", "w_ckv", "w_co",
              "w_expert_gate", "w_expert_up", "w_expert_down"):
        m[k] = np.ascontiguousarray(np.asarray(inputs[k], f32)[0])
    m["w_router"] = np.ascontiguousarray(np.concatenate(
        [np.asarray(inputs["w_router_group"], f32)[0], np.asarray(inputs["w_router_expert"], f32)[0]], axis=1))
    m["b_router"] = np.ascontiguousarray(np.concatenate(
        [np.asarray(inputs["b_router_group"], f32)[0], np.asarray(inputs["b_router_expert"], f32)[0]])[None, :])
    gT = np.concatenate([_fm(inputs["attn_norm_g"][0]), _fm(inputs["cross_norm_g"][0]), _fm(inputs["mem_norm_g"][0]),
                         _fm(inputs["mla_q_norm_g"][0]), _fm(inputs["mla_kv_norm_g"][0]),
                         _fm(inputs["diff_subln_g"][0]), np.zeros((128, 1), f32),
                         _fm(inputs["ffn_norm_g"][0])], axis=1)
    m["gT"] = np.ascontiguousarray(gT, f32)
    m["grow"] = np.ascontiguousarray(np.stack([np.asarray(inputs["ffn_norm_g"], f32)[0],
                                               np.asarray(inputs["final_norm_g"], f32)], axis=0))
    m["lam"] = np.ascontiguousarray(np.stack([np.asarray(inputs[k], f32)[0] for k in
                                              ("diff_lambda_q1", "diff_lambda_k1", "diff_lambda_q2", "diff_lambda_k2")]).reshape(1, 256))
    ident = np.eye(128, dtype=f32)
    ones = np.ones((128, 128), f32)
    Rd = np.zeros((128, 128), f32)
    for blk in range(2):
        for i in range(8):
            Rd[blk * 64 + i + 8, blk * 64 + i] = -1.0
            Rd[blk * 64 + i, blk * 64 + i + 8] = 1.0
    U = np.triu(np.ones((128, 128), f32), k=1)
    m["cb"] = np.ascontiguousarray(np.concatenate([ident, ones, Rd, U], axis=1).astype(bf))
    Rm = np.zeros((128, 128), f32)
    for i in range(32):
        Rm[i + 32, i] = -1.0
        Rm[i, i + 32] = 1.0
    m["rm"] = Rm.astype(bf)
    cf = np.zeros((128, 2 + NE), f32)
    for pp in range(128):
        i = pp % 64
        if i < 16:
            cf[pp, 0] = ROPE_THETA ** (-(i % 8) * 2.0 / 16.0) / (2 * math.pi)
        cf[pp, 1] = ROPE_THETA ** (-(i % 32) * 2.0 / 64.0) / (2 * math.pi)
    cf[:, 2:] = (np.arange(NE, dtype=f32) * CAP)[None, :]
    m["cf"] = cf
    return m


_NC_CACHE = {}
_DEV_CORES = 0


def kernel(**inputs):
    return _run(inputs)


def _run(inputs, stop_after=None, dump=()):
    inputs = {k: np.asarray(v) for k, v in inputs.items()}
    key = (stop_after, tuple(dump))
    if key not in _NC_CACHE:
        _NC_CACHE[key] = build_program(stop_after=stop_after, dump=dump)
    nc = _NC_CACHE[key]
    shared = host_shared(inputs)
    in_maps = []
    ncores = _DEV_CORES or NCORES
    for b in range(ncores):
        m = dict(shared)
        m["x"] = np.ascontiguousarray(inputs["x"][b], np.float32)
        m["mem"] = np.ascontiguousarray(inputs["mem"][b], np.float32)
        m["pos"] = np.ascontiguousarray(inputs["positions"][b], np.int32)[None, :]
        in_maps.append(m)
    res = run_bass_kernel_spmd(nc, in_maps, core_ids=list(range(ncores)))
    out = np.stack([np.asarray(r["out"]) for r in res.results], axis=0).astype(np.float32)
    if dump:
        return out, [{k: np.asarray(r[k]) for k in dump} for r in res.results]
    return out
```
